# Optimizing a Trainium2 kernel written in Bass

```python
import math, functools
import jax, jax.numpy as jnp
from jax import lax
import numpy as np

D_MODEL = 2048
BATCH = 4
SEQ = 2048
DEPTH = 1
DEC_BATCH = 32
DEC_SEQ = 8
PAST_LEN = 16384
PAGE_SIZE = 128

N_HEADS = 8
HEAD_DIM = 128
N_KV_HEADS = 2
GQA_GROUP = N_HEADS // N_KV_HEADS
IDX_HEADS = 16
IDX_DIM = 64
TOPK = 256
Q_BLOCK = 128
N_BUCKETS = 32
MAX_DISTANCE = 128
SSD_HEADS = 16
SSD_HEAD_DIM = 64
SSD_GROUPS = 2
D_STATE = 128
CONV_WIDTH = 4
CHUNK = 128
DT_MIN = 0.001
DT_MAX = 0.1
D_ATTN = N_HEADS * HEAD_DIM
D_SSD = SSD_HEADS * SSD_HEAD_DIM
D_MIX = D_ATTN + D_SSD
CONV_DIM = D_SSD + 2 * SSD_GROUPS * D_STATE
D_FF = 5632
EPS = 1e-6
IN_SPLITS = (D_ATTN, N_KV_HEADS * HEAD_DIM, N_KV_HEADS * HEAD_DIM, IDX_HEADS * IDX_DIM, IDX_DIM, IDX_HEADS, D_SSD, CONV_DIM, SSD_HEADS)
D_IN = sum(IN_SPLITS)

kernel_name = 'hymba_dsa_ssd_macaron_step'


def split_cols(t, sizes):
    offsets = [int(o) for o in np.cumsum(sizes)[:-1]]
    return jnp.split(t, offsets, axis=-1)


def rms_norm(x, g):
    xf = x.astype(jnp.float32)
    xf = xf * lax.rsqrt(jnp.mean(xf * xf, axis=-1, keepdims=True) + EPS)
    return (xf * g.astype(jnp.float32)).astype(x.dtype)


def swiglu(x, w1, w3, w2):
    return (jax.nn.silu(x @ w1) * (x @ w3)) @ w2


def t5_bucket(n):
    max_exact = N_BUCKETS // 2
    nf = jnp.maximum(n, 1).astype(jnp.float32)
    large = max_exact + (jnp.log(nf / max_exact) / math.log(MAX_DISTANCE / max_exact) * (N_BUCKETS - max_exact)).astype(jnp.int32)
    return jnp.where(n < max_exact, n, jnp.minimum(large, N_BUCKETS - 1))


def sparse_attend(q, qi, wi, pos_q, ki_all, gather_kv, top, rel_bias):
    b, nq = q.shape[:2]
    n_keys = ki_all.shape[1]
    rel = jnp.einsum('bqhd,bsd->bqhs', qi, ki_all)
    score = jnp.einsum('bqhs,bqh->bqs', jax.nn.relu(rel), wi).astype(jnp.float32)
    admissible = jnp.arange(n_keys)[None, :] <= pos_q[:, None]
    score = jnp.where(admissible[None], score, -jnp.inf)
    _, idx = lax.top_k(score, top)
    valid = idx <= pos_q[None, :, None]
    k_sel, v_sel = gather_kv(idx)
    qg = q.reshape(b, nq, N_KV_HEADS, GQA_GROUP, HEAD_DIM)
    logits = jnp.einsum('bqkgd,bqskd->bqkgs', qg, k_sel).astype(jnp.float32) * HEAD_DIM ** -0.5
    bias = rel_bias[t5_bucket(jnp.maximum(pos_q[None, :, None] - idx, 0))]
    bias = bias.reshape(b, nq, top, N_KV_HEADS, GQA_GROUP).transpose(0, 1, 3, 4, 2)
    logits = jnp.where(valid[:, :, None, None, :], logits + bias.astype(jnp.float32), -jnp.inf)
    p = jax.nn.softmax(logits, axis=-1).astype(v_sel.dtype)
    out = jnp.einsum('bqkgs,bqskd->bqkgd', p, v_sel)
    return out.reshape(b, nq, D_ATTN)


def prompt_attend(q, k, v, qi, ki, wi, rel_bias):
    b, t = q.shape[:2]
    top = min(TOPK, t // 4)
    qb = min(Q_BLOCK, t)
    nb = t // qb
    take = jax.vmap(lambda rows, ii: rows[ii])

    def gather_kv(idx):
        return take(k, idx), take(v, idx)

    def blocks(a):
        return jnp.moveaxis(a.reshape(b, nb, qb, *a.shape[2:]), 1, 0)

    def one_block(args):
        i, qq, qqi, wwi = args
        pos = i * qb + jnp.arange(qb)
        return sparse_attend(qq, qqi, wwi, pos, ki, gather_kv, top, rel_bias)

    out = lax.map(one_block, (jnp.arange(nb), blocks(q), blocks(qi), blocks(wi)))
    return jnp.moveaxis(out, 0, 1).reshape(b, t, D_ATTN)


def sample_attend(q, k, v, qi, ki, wi, rel_bias, pool_k, pool_v, pool_kidx, page_table):
    b, t = q.shape[:2]
    past = page_table.shape[1] * PAGE_SIZE
    ki_past = pool_kidx[page_table].reshape(b, past, IDX_DIM)
    ki_all = jnp.concatenate([ki_past, ki.astype(ki_past.dtype)], axis=1)
    top = min(TOPK, (past + t) // 4)
    take = jax.vmap(lambda rows, ii: rows[ii])

    def gather_kv(idx):
        in_past = (idx < past)[..., None, None]
        pidx = jnp.minimum(idx, past - 1)
        phys = jax.vmap(lambda pt, pg: pt[pg])(page_table, pidx // PAGE_SIZE)
        off = pidx % PAGE_SIZE
        nidx = jnp.clip(idx - past, 0, t - 1)
        k_sel = jnp.where(in_past, pool_k[phys, off], take(k, nidx))
        v_sel = jnp.where(in_past, pool_v[phys, off], take(v, nidx))
        return k_sel, v_sel

    pos = past + jnp.arange(t)
    return sparse_attend(q, qi, wi, pos, ki_all, gather_kv, top, rel_bias)


def ssd_scan(xh, dt, a, bm, cm, h0):
    b, l, H, P = xh.shape
    G, N = bm.shape[2], bm.shape[3]
    E = H // G
    q = min(CHUNK, l)
    nc = -(-l // q)
    pad = nc * q - l
    if pad:
        padf = lambda t: jnp.pad(t, [(0, 0), (0, pad)] + [(0, 0)] * (t.ndim - 2))
        xh, dt, bm, cm = padf(xh), padf(dt), padf(bm), padf(cm)
    xd = (xh * dt[..., None]).reshape(b, nc, q, G, E, P)
    acum = jnp.cumsum((dt * a).reshape(b, nc, q, G, E), axis=2)
    bc = bm.reshape(b, nc, q, G, N)
    cc = cm.reshape(b, nc, q, G, N)
    causal = jnp.tril(jnp.ones((q, q), bool))[:, :, None, None]
    seg = acum[:, :, :, None] - acum[:, :, None, :]
    lmat = jnp.exp(jnp.where(causal, seg, -jnp.inf))
    cb = jnp.einsum('bcign,bcjgn->bcijg', cc, bc)
    y_diag = jnp.einsum('bcijg,bcijge,bcjgep->bcigep', cb, lmat, xd)
    decay = jnp.exp(acum[:, :, -1:] - acum)
    st = jnp.einsum('bcjgn,bcjge,bcjgep->bcgepn', bc, decay, xd)
    cdecay = jnp.exp(acum[:, :, -1])

    def step(h, inp):
        s_c, d_c = inp
        return h * d_c[..., None, None] + s_c, h

    h_last, h_start = lax.scan(step, h0.reshape(b, G, E, P, N), (jnp.moveaxis(st, 1, 0), jnp.moveaxis(cdecay, 1, 0)))
    h_start = jnp.moveaxis(h_start, 0, 1)
    y_off = jnp.einsum('bcign,bcgepn,bcige->bcigep', cc, h_start, jnp.exp(acum))
    y = (y_diag + y_off).reshape(b, nc * q, H, P)[:, :l]
    return y, h_last.reshape(b, H, P, N)


def ssd_mixer(z, xbc, dtr, conv_prev, ssm_prev, conv_w, conv_b, a_log, dt_bias, d_skip, g_ssd):
    b, L = xbc.shape[:2]
    xpad = jnp.concatenate([conv_prev.astype(xbc.dtype), xbc], axis=1)
    conv = sum(xpad[:, k:k + L] * conv_w[k] for k in range(CONV_WIDTH)) + conv_b
    xbc_c = jax.nn.silu(conv)
    xs, bm, cm = split_cols(xbc_c, (D_SSD, SSD_GROUPS * D_STATE, SSD_GROUPS * D_STATE))
    f32 = lambda t: t.astype(jnp.float32)
    xh = f32(xs).reshape(b, L, SSD_HEADS, SSD_HEAD_DIM)
    dt = jax.nn.softplus(f32(dtr) + f32(dt_bias))
    a = -jnp.exp(f32(a_log))
    y, h_new = ssd_scan(xh, dt, a, f32(bm).reshape(b, L, SSD_GROUPS, D_STATE), f32(cm).reshape(b, L, SSD_GROUPS, D_STATE), f32(ssm_prev))
    y = y + f32(d_skip)[:, None] * xh
    y = y.reshape(b, L, D_SSD) * jax.nn.silu(f32(z))
    y = rms_norm(y.reshape(b, L, SSD_GROUPS, D_SSD // SSD_GROUPS), g_ssd.reshape(SSD_GROUPS, D_SSD // SSD_GROUPS))
    return y.reshape(b, L, D_SSD).astype(z.dtype), xpad[:, L:], h_new.astype(z.dtype)


def setup_inputs(seed: int = 0) -> dict:
    key = jax.random.key(seed)
    ks = jax.random.split(key, 32)
    n_pages = PAST_LEN // PAGE_SIZE
    n_used = DEC_BATCH * n_pages
    n_pool = (n_used * 5) // 4
    nrm = lambda k, shape, scale=1.0: jax.random.normal(k, shape, jnp.float32) * scale
    gain = lambda k, shape: 1.0 + 0.01 * jax.random.normal(k, shape, jnp.float32)
    page_table = jax.random.permutation(ks[7], n_pool)[:n_used].reshape(DEC_BATCH, n_pages).astype(jnp.int32)
    dt0 = jnp.exp(jax.random.uniform(ks[17], (DEPTH, SSD_HEADS)) * (math.log(DT_MAX) - math.log(DT_MIN)) + math.log(DT_MIN))
    return {
        'x_prompt': nrm(ks[0], (BATCH, SEQ, D_MODEL)),
        'x_sample': nrm(ks[1], (DEC_BATCH, DEC_SEQ, D_MODEL)),
        'cache_k': nrm(ks[2], (DEPTH, n_pool, PAGE_SIZE, N_KV_HEADS, HEAD_DIM)),
        'cache_v': nrm(ks[3], (DEPTH, n_pool, PAGE_SIZE, N_KV_HEADS, HEAD_DIM)),
        'cache_kidx': nrm(ks[4], (DEPTH, n_pool, PAGE_SIZE, IDX_DIM)),
        'state_conv': nrm(ks[5], (DEPTH, DEC_BATCH, CONV_WIDTH - 1, CONV_DIM)),
        'state_ssm': nrm(ks[6], (DEPTH, DEC_BATCH, SSD_HEADS, SSD_HEAD_DIM, D_STATE), 0.1),
        'page_table': page_table,
        'rel_bias': nrm(ks[8], (N_BUCKETS, N_HEADS), 0.5),
        'g_ffn1': gain(ks[9], (DEPTH, D_MODEL)),
        'w1_ffn1': nrm(ks[10], (DEPTH, D_MODEL, D_FF), D_MODEL ** -0.5),
        'w3_ffn1': nrm(ks[11], (DEPTH, D_MODEL, D_FF), D_MODEL ** -0.5),
        'w2_ffn1': nrm(ks[12], (DEPTH, D_FF, D_MODEL), D_FF ** -0.5),
        'g_mix': gain(ks[13], (DEPTH, D_MODEL)),
        'w_in': nrm(ks[14], (DEPTH, D_MODEL, D_IN), D_MODEL ** -0.5),
        'conv_w': nrm(ks[15], (DEPTH, CONV_WIDTH, CONV_DIM), CONV_WIDTH ** -0.5),
        'conv_b': nrm(ks[16], (DEPTH, CONV_DIM), 0.01),
        'a_log': jnp.log(jax.random.uniform(ks[18], (DEPTH, SSD_HEADS), minval=1.0, maxval=16.0)),
        'dt_bias': dt0 + jnp.log(-jnp.expm1(-dt0)),
        'd_skip': 1.0 + 0.1 * nrm(ks[19], (DEPTH, SSD_HEADS)),
        'g_ssd': gain(ks[20], (DEPTH, D_SSD)),
        'w_out': nrm(ks[21], (DEPTH, D_MIX, D_MODEL), D_MIX ** -0.5),
        'g_ffn2': gain(ks[22], (DEPTH, D_MODEL)),
        'w1_ffn2': nrm(ks[23], (DEPTH, D_MODEL, D_FF), D_MODEL ** -0.5),
        'w3_ffn2': nrm(ks[24], (DEPTH, D_MODEL, D_FF), D_MODEL ** -0.5),
        'w2_ffn2': nrm(ks[25], (DEPTH, D_FF, D_MODEL), D_FF ** -0.5),
        'g_final': gain(ks[26], (D_MODEL,)),
    }


def reference(x_prompt, x_sample, cache_k, cache_v, cache_kidx, state_conv, state_ssm, page_table, rel_bias, g_ffn1, w1_ffn1, w3_ffn1, w2_ffn1, g_mix, w_in, conv_w, conv_b, a_log, dt_bias, d_skip, g_ssd, w_out, g_ffn2, w1_ffn2, w3_ffn2, w2_ffn2, g_final):
    def layer(l, x, attend, conv_prev, ssm_prev):
        b, L, _ = x.shape
        x = x + 0.5 * swiglu(rms_norm(x, g_ffn1[l]), w1_ffn1[l], w3_ffn1[l], w2_ffn1[l])
        h = rms_norm(x, g_mix[l])
        q, k, v, qi, ki, wi, z, xbc, dtr = split_cols(h @ w_in[l], IN_SPLITS)
        q = q.reshape(b, L, N_HEADS, HEAD_DIM)
        k = k.reshape(b, L, N_KV_HEADS, HEAD_DIM)
        v = v.reshape(b, L, N_KV_HEADS, HEAD_DIM)
        qi = qi.reshape(b, L, IDX_HEADS, IDX_DIM)
        wi = wi * (IDX_HEADS * IDX_DIM) ** -0.5
        a_out = attend(q, k, v, qi, ki, wi)
        s_out, conv_new, ssm_new = ssd_mixer(z, xbc, dtr, conv_prev, ssm_prev, conv_w[l], conv_b[l], a_log[l], dt_bias[l], d_skip[l], g_ssd[l])
        x = x + jnp.concatenate([a_out, s_out], axis=-1) @ w_out[l]
        x = x + 0.5 * swiglu(rms_norm(x, g_ffn2[l]), w1_ffn2[l], w3_ffn2[l], w2_ffn2[l])
        return x, k, v, ki, conv_new, ssm_new

    bp = x_prompt.shape[0]
    xp = x_prompt
    kp, vp, kip, cp, sp = [], [], [], [], []
    for l in range(DEPTH):
        zero_conv = jnp.zeros((bp, CONV_WIDTH - 1, CONV_DIM), x_prompt.dtype)
        zero_ssm = jnp.zeros((bp, SSD_HEADS, SSD_HEAD_DIM, D_STATE), jnp.float32)
        attend = functools.partial(prompt_attend, rel_bias=rel_bias)
        xp, k, v, ki, cn, sn = layer(l, xp, attend, zero_conv, zero_ssm)
        kp.append(k); vp.append(v); kip.append(ki); cp.append(cn); sp.append(sn)
    y_prompt = rms_norm(xp, g_final)

    xs = x_sample
    ks_, vs_, kis_, cs_, ss_ = [], [], [], [], []
    for l in range(DEPTH):
        attend = functools.partial(sample_attend, rel_bias=rel_bias, pool_k=cache_k[l], pool_v=cache_v[l], pool_kidx=cache_kidx[l], page_table=page_table)
        xs, k, v, ki, cn, sn = layer(l, xs, attend, state_conv[l], state_ssm[l])
        ks_.append(k); vs_.append(v); kis_.append(ki); cs_.append(cn); ss_.append(sn)
    y_sample = rms_norm(xs, g_final)

    k_prompt = jnp.stack(kp)
    v_prompt = jnp.stack(vp)
    kidx_prompt = jnp.stack(kip)
    conv_prompt = jnp.stack(cp)
    ssm_prompt = jnp.stack(sp)
    k_sample = jnp.stack(ks_)
    v_sample = jnp.stack(vs_)
    kidx_sample = jnp.stack(kis_)
    conv_sample = jnp.stack(cs_)
    ssm_sample = jnp.stack(ss_)
    return (y_prompt, y_sample, k_prompt, v_prompt, kidx_prompt, conv_prompt, ssm_prompt, k_sample, v_sample, kidx_sample, conv_sample, ssm_sample)
```

```python
import numpy as np
import concourse.bass as bass
import concourse.mybir as mybir
from concourse.bass_utils import run_bass_kernel_spmd
from contextlib import ExitStack

F32 = mybir.dt.float32
BF16 = mybir.dt.bfloat16
I32 = mybir.dt.int32
AF = mybir.ActivationFunctionType
ALU = mybir.AluOpType
AX = mybir.AxisListType

D = 2048
DFF = 5632
NFF = DFF // 128
NPT = 8
NTT = 9
NTOK = 1024 + 35
TB = [(0, 512), (512, 512), (1024, 35)]
EPS = 1e-6
G = 4
NIN = 43


_DBG = {}


class Sched:
    ENGS = ("pe", "act", "dve", "pool", "sp")

    def __init__(self, nc):
        self.nc = nc
        self.ops = []
        self.last_w = {}
        self.readers = {}
        self.dma_cnt = {}
        self.last_eng = {}
        self.last_dma = {}
        self.last_bar = None
        self.batch_ends = {}

    def dma_batch_end(self, group):
        self.batch_ends.setdefault(group, []).append(self.dma_cnt.get(group, 0))

    def barrier(self, fn):
        deps = set(self.last_eng.values()) | set(self.last_dma.values())
        i = self.op("pool", fn, _extra=deps)
        self.last_bar = i
        return i

    def op(self, eng, fn, r=(), w=(), dma=None, _extra=()):
        i = len(self.ops)
        deps = set(_extra)
        if self.last_bar is not None:
            deps.add(self.last_bar)
        for k in list(r) + list(w):
            if k in self.last_w:
                deps.add(self.last_w[k])
        for k in w:
            lastc = {}
            for j in self.readers.get(k, ()):
                oj = self.ops[j]
                if oj["dma"] is None:
                    lastc[oj["eng"]] = max(lastc.get(oj["eng"], -1), j)
                else:
                    deps.add(j)
            deps.update(lastc.values())
        deps.discard(i)
        o = dict(eng=eng, fn=fn, deps=deps, dma=dma, sig=False, i=i)
        if dma is not None:
            self.dma_cnt[dma] = self.dma_cnt.get(dma, 0) + 1
            o["dcount"] = self.dma_cnt[dma]
            self.last_dma[dma] = i
        else:
            self.last_eng[eng] = i
        self.ops.append(o)
        for k in w:
            self.last_w[k] = i
            self.readers[k] = []
        for k in r:
            self.readers.setdefault(k, []).append(i)
        return i

    def emit(self, final_dma_groups=()):
        nc = self.nc
        ops = self.ops
        for o in ops:
            for d in o["deps"]:
                od = ops[d]
                if od["dma"] is None and od["eng"] == "pe" and o["eng"] == "pe" and o["dma"] is None:
                    continue
                od["sig"] = True
        cnt = {e: 0 for e in self.ENGS}
        for o in ops:
            if o["dma"] is None and o["sig"]:
                cnt[o["eng"]] += 1
                o["sval"] = cnt[o["eng"]]
        groups = sorted(self.dma_cnt.keys())
        with ExitStack() as es:
            esem = {e: es.enter_context(nc.semaphore("s_" + e)) for e in self.ENGS}
            dsem = {g: es.enter_context(nc.semaphore("d_" + str(g))) for g in groups}
            block = es.enter_context(nc.Block())
            per_eng = {e: [o for o in ops if o["eng"] == e] for e in self.ENGS}

            def run(engobj, ename):
                waited = {}
                for o in per_eng[ename]:
                    need = {}
                    for d in o["deps"]:
                        od = ops[d]
                        if od["dma"] is not None:
                            key = ("d", od["dma"])
                            dc = od["dcount"]
                            ends = [b for b in self.batch_ends.get(od["dma"], ()) if b >= dc]
                            val = 16 * (min(ends) if ends else dc)
                        else:
                            if od["eng"] == "pe" and ename == "pe" and o["dma"] is None:
                                continue
                            key = ("e", od["eng"])
                            val = od["sval"]
                        if need.get(key, 0) < val:
                            need[key] = val
                    for key, val in need.items():
                        if waited.get(key, 0) >= val:
                            continue
                        waited[key] = val
                        sem = dsem[key[1]] if key[0] == "d" else esem[key[1]]
                        engobj.wait_ge(sem, val)
                    ins = o["fn"](engobj)
                    if o["dma"] is not None:
                        ins.then_inc(dsem[o["dma"]], 16)
                    elif o["sig"]:
                        ins.then_inc(esem[ename], 1)
                if ename == "sp":
                    for g in final_dma_groups:
                        if g in dsem:
                            engobj.wait_ge(dsem[g], 16 * self.dma_cnt[g])

            block.tensor(lambda e: run(e, "pe"))
            block.scalar(lambda e: run(e, "act"))
            block.vector(lambda e: run(e, "dve"))
            block.gpsimd(lambda e: run(e, "pool"))
            block.sync(lambda e: run(e, "sp"))


class Arena:
    def __init__(self, nc, base=16512, top=229344):
        self.nc = nc
        self.base = base
        self.top = top
        self.n = 0

    def at(self, off, shape, dtype, name=None):
        self.n += 1
        nbytes = int(np.prod(shape[1:])) * (2 if dtype == BF16 else 4)
        assert self.base + off + nbytes <= self.top, (name, off, nbytes)
        return self.nc.alloc_sbuf_tensor_at(name or f"t{self.n}", list(shape), dtype, offset=self.base + off)


def build():
    nc = bass.Bass("TRN2", target_bir_lowering=False)
    S = Sched(nc)
    A = Arena(nc)

    def din(name, shape, dt=F32):
        return nc.dram_tensor(name, list(shape), dt, kind="ExternalInput").ap()

    def dout(name, shape, dt=F32):
        return nc.dram_tensor(name, list(shape), dt, kind="ExternalOutput").ap()

    xin = din("xin", [NTT, 128, D])
    xpre = din("xpre", [NPT, 128, D])
    flagc = din("flagc", [128, 2])
    gvec = din("gvec", [4, D])
    wf1 = [din("w1a", [NFF, 128, 16, 128]), din("w3a", [NFF, 128, 16, 128]), din("w2a", [NFF, 128, D])]
    wf2 = [din("w1b", [NFF, 128, 16, 128]), din("w3b", [NFF, 128, 16, 128]), din("w2b", [NFF, 128, D])]
    win = din("win", [NIN, 128, 16, 128])
    wout = din("wout", [4, 4, 128, 4, 512])
    sconvT = din("sconvT", [128, 12, 4, 3])
    sssmT = din("sssmT", [4, 128, 1024])
    cwT = din("cwT", [128, 12, 4])
    cbT = din("cbT", [128, 12])
    dcol = din("dcol", [128, 8])
    gssd = din("gssd", [128, 8])
    alog = din("alog", [1, 16])
    relb = din("relb", [32, 8])
    kidx_tab = din("kidx_tab", [5120 * 128, 64])
    kv_tab = din("kv_tab", [5120 * 128, 512])
    pt4 = din("pt4", [4, 128], I32)
    bm4 = din("bm4", [32, 4])
    dq8 = din("dq8", [32, 8])
    pen32 = din("pen32", [32, 4, 8])
    oh_d = din("oh_d", [32, 256])
    dtb = din("dtb", [1, 16])
    y_out = dout("y_out", [NTT, 128, D])
    kvs_out = dout("kvs_out", [NTT, 128, 640])
    conv_out = dout("conv_out", [5, 3, 1536])
    ssm_out = dout("ssm_out", [5, 128, 1024])
    xsp = nc.dram_tensor("xsp", [NTT, 128, D], F32).ap()
    pre_k = nc.dram_tensor("pre_k", [128, 2, 1024], BF16).ap()
    pre_v = nc.dram_tensor("pre_v", [128, 8, 258], BF16).ap()
    pre_ki = nc.dram_tensor("pre_ki", [128, 1024], BF16).ap()
    pre_h = nc.dram_tensor("pre_h", [128, 1024], F32).ap()
    pre_halo = nc.dram_tensor("pre_halo", [128, 36], F32).ap()
    bsc_t = nc.dram_tensor("bsc", [8, 128, 256], F32)
    bsc = bsc_t.ap()

    OX = 0
    OH = 73728
    OW = OH + 16 * NTOK * 2
    O_STAGE = OW
    O_WUP = O_STAGE + 3 * 8192
    O_W2G = O_WUP + 4 * 4096
    O_GT = O_W2G + 2 * G * D * 2
    GTB = G * NTOK * 2 + 8
    O_SIL = O_GT + 2 * GTB
    O_MISC = O_SIL + 2 * 2048
    xres = A.at(OX, [128, NTT, D], F32, "xres")
    hT = A.at(OH, [128, 16, NTOK], BF16, "hT")
    stage = [A.at(O_STAGE + i * 8192, [128, 16, 128], F32, f"stage{i}") for i in range(3)]
    wup = [A.at(O_WUP + i * 4096, [128, 16, 128], BF16, f"wup{i}") for i in range(4)]
    w2g = [A.at(O_W2G + i * G * D * 2, [128, G, D], BF16, f"w2g{i}") for i in range(2)]
    gT = [A.at(O_GT + i * GTB, [128, G, NTOK], BF16, f"gT{i}") for i in range(2)]
    sil = [A.at(O_SIL + i * 2048, [128, 512], F32, f"sil{i}") for i in range(2)]
    al = lambda v: (v + 31) // 32 * 32
    o = O_MISC
    ident_b = A.at(o, [128, 128], BF16, "ident_b"); o = al(o + 256)
    ident_f = A.at(o, [128, 128], F32, "ident_f"); o = al(o + 512)
    tri_f = A.at(o, [128, 128], F32, "tri_f"); o = al(o + 512)
    ones_f = A.at(o, [128, 128], F32, "ones_f"); o = al(o + 512)
    ones_b = A.at(o, [128, 128], BF16, "ones_b"); o = al(o + 256)
    stat = A.at(o, [128, 8], F32, "stat"); o = al(o + 32)
    flag_t = A.at(o, [128, 2], F32, "flag_t"); o = al(o + 8)
    cw_t = A.at(o, [128, 12, 4], F32, "cw_t"); o = al(o + 192)
    cb_t = A.at(o, [128, 12], F32, "cb_t"); o = al(o + 48)
    dcol_t = A.at(o, [128, 8], F32, "dcol_t"); o = al(o + 32)
    gssd_t = A.at(o, [128, 8], F32, "gssd_t"); o = al(o + 32)
    dtb_t = A.at(o, [128, 16], F32, "dtb_t"); o = al(o + 64)
    a_t = A.at(o, [128, 16], F32, "a_t"); o = al(o + 64)
    halo = A.at(o, [128, 12, 3], F32, "halo"); o = al(o + 144)
    sct = A.at(o, [128, 12, 4, 3], F32, "sct"); o = al(o + 576)
    tokst = [A.at(O_MISC - 2048 + i * 512, [128, 128], F32, f"tokst{i}") for i in range(2)]
    tails = [A.at(o + i * 2560, [3, 5, 128], F32, f"tails{i}") for i in range(1)]; o = al(o + 2560)
    gb = A.at(O_STAGE, [128, D], F32, "gb")
    xs = A.at(O_STAGE + 8192, [128, D], BF16, "xs")
    junk = A.at(O_STAGE + 8192 + 4096, [128, D], BF16, "junk")
    NTP = 1072
    NB = NTP * 2
    qT = A.at(OX, [128, 8, NTP], BF16, "qT")
    qiT = A.at(OX + 8 * NB, [128, 8, NTP], BF16, "qiT")
    szT = A.at(OX + 16 * NB, [128, 8, NTP], BF16, "szT")
    ssdT = szT
    attT = A.at(OX + 24 * NB, [128, 8, NTP], BF16, "attT")
    kT = A.at(OX + 32 * NB, [128, 2, NTP], BF16, "kT")
    o = O_W2G
    xcT = A.at(o, [128, 12, NTP], BF16, "xcT"); o = al(o + 12 * NB)
    vb1 = A.at(o, [128, NTT, 2, 129], BF16, "vb1"); o = al(o + NTT * 2 * 129 * 2 + 4)
    sm_tok = A.at(o, [128, NTT, 128], F32, "sm_tok"); o = al(o + NTT * 512)
    kiT2 = A.at(o, [128, NTOK + 1], BF16, "kiT2"); o = al(o + NB + 2)
    dtT = A.at(o, [16, NTOK], F32, "dtT"); o = al(o + NTOK * 4)
    rawx = A.at(o, [128, NTOK + 3], F32, "rawx"); o = al(o + (NTOK + 3) * 4)
    cacc = A.at(o, [128, 1024], F32, "cacc"); o = al(o + 4096)
    exts = A.at(o, [128, 4, 11], F32, "exts"); o = al(o + 176)
    assert o <= O_MISC - 2048, o - O_MISC
    o = OH
    kT_pre = A.at(o, [128, 2, 1024], BF16, "kT_pre"); o = al(o + 4096)
    vb1_pre = A.at(o, [128, 8, 2, 129], BF16, "vb1_pre"); o = al(o + 4128)
    kiT2_pre = A.at(o, [128, 1024], BF16, "kiT2_pre"); o = al(o + 2048)
    o = al(o + 16384)
    Hst = A.at(o, [128, 1024], F32, "Hst"); o = al(o + 4096)
    Hb = A.at(o, [128, 1024], BF16, "Hb"); o = al(o + 2048)
    assert o <= OW
    o = O_STAGE
    ysb = A.at(o, [128, 1024], F32, "ysb"); o = al(o + 4096)
    xd = A.at(o, [128, 1024], BF16, "xd"); o = al(o + 2048)
    xdd = A.at(o, [128, 1024], BF16, "xdd"); o = al(o + 2048)
    Btok = A.at(o, [128, 256], BF16, "Btok"); o = al(o + 512)
    Dm = A.at(o, [128, 8, 128], F32, "Dm"); o = al(o + 4096)
    CBm = A.at(o, [128, 2, 128], F32, "CBm"); o = al(o + 1024)
    t1 = A.at(o, [128, 128], F32, "t1"); o = al(o + 512)
    t2 = A.at(o, [128, 128], F32, "t2"); o = al(o + 512)
    Mh = A.at(o, [128, 128], BF16, "Mh"); o = al(o + 256)
    sm16 = [A.at(o + i * 64, [128, 16], F32, f"sm16_{i}") for i in range(8)]; o = al(o + 512)
    gbuf = A.at(o, [128, 8, 128], F32, "gbuf"); o = al(o + 4096)
    sq = A.at(o, [128, 8, 128], BF16, "sq"); o = al(o + 2048)
    rs = A.at(o, [128, 2, 128], F32, "rs"); o = al(o + 1024)
    assert o <= O_W2G

    psall = nc.alloc_psum_tensor("psall", [128, 4096], F32)
    ps = [psall[:, i * 512:(i + 1) * 512] for i in range(8)]

    import os
    KSTOP = int(os.environ.get("KSTOP", "99"))
    KSKIP = os.environ.get("KSKIP", "")
    nbar = [0]

    class _Stop(Exception):
        pass

    def bar():
        S.barrier(lambda e: e.memset(stat[:, 7:8], 0.0))
        nbar[0] += 1
        if nbar[0] >= KSTOP:
            raise _Stop()

    for t, nm in ((ident_b, "ident_b"), (ident_f, "ident_f")):
        S.op("pool", lambda e, t=t: e.memset(t[:], 1.0), w=[nm])
        S.op("pool", lambda e, t=t: e.affine_select(out=t[:], in_=t[:], pattern=[[-1, 128]], compare_op=ALU.is_equal,
                                                     fill=0.0, base=0, channel_multiplier=1), r=[nm], w=[nm])
    S.op("pool", lambda e: e.memset(tri_f[:], 1.0), w=["tri_f"])
    S.op("pool", lambda e: e.affine_select(out=tri_f[:], in_=tri_f[:], pattern=[[1, 128]], compare_op=ALU.is_ge,
                                           fill=0.0, base=0, channel_multiplier=-1), r=["tri_f"], w=["tri_f"])
    S.op("pool", lambda e: e.memset(ones_f[:], 1.0), w=["ones_f"])
    S.op("pool", lambda e: e.memset(ones_b[:], 1.0), w=["ones_b"])
    for i, (dst, src) in enumerate(((flag_t[:], flagc[:, :]), (cw_t[:], cwT[:, :, :]), (cb_t[:], cbT[:, :]), (dcol_t[:], dcol[:, :]),
                                    (gssd_t[:], gssd[:, :]), (sct[:], sconvT[:, :, :, :]),
                                    (dtb_t[:], dtb[0:1, :].partition_broadcast(128)), (a_t[:], alog[0:1, :].partition_broadcast(128)))):
        S.op("sp", lambda e, dst=dst, src=src: e.dma_start(out=dst, in_=src), w=[("cst", i)], dma="cst")
    S.dma_batch_end("cst")
    S.op("act", lambda e: e.activation(out=a_t[:], in_=a_t[:], func=AF.Exp), r=[("cst", 7)], w=[("cst", 7)])
    S.op("dve", lambda e: e.tensor_scalar(out=a_t[:], in0=a_t[:], scalar1=-1.0, scalar2=None, op0=ALU.mult), r=[("cst", 7)], w=[("cst", 7)])

    def load_x(src, ntiles):
        for t in range(ntiles):
            S.op("sp", lambda e, t=t: e.dma_start(out=xres[:, t, :], in_=src[t, :, :]), w=[("x", t)], dma="xld")
        S.dma_batch_end("xld")

    def norm_to_hT(gi, ntiles):
        S.op("sp", lambda e: e.dma_start(out=gb[:], in_=gvec[gi:gi + 1, :].partition_broadcast(128)),
             r=["stage0"], w=["gb", "stage0"], dma="gld")
        for t in range(ntiles):
            ncol = 128 if t < NPT else 35
            S.op("act", lambda e, t=t: e.activation(out=junk[:], in_=xres[:, t, :], func=AF.Square, accum_out=stat[:, 0:1]),
                 r=[("x", t)], w=["junk", "ss", "stage1"])
            S.op("dve", lambda e: e.tensor_scalar(out=stat[:, 1:2], in0=stat[:, 0:1], scalar1=1.0 / D, scalar2=EPS,
                                                  op0=ALU.mult, op1=ALU.add), r=["ss"], w=["ms"])
            S.op("act", lambda e: e.activation(out=stat[:, 2:3], in_=stat[:, 1:2], func=AF.Sqrt), r=["ms"], w=["sd"])
            S.op("dve", lambda e: e.reciprocal(out=stat[:, 3:4], in_=stat[:, 2:3]), r=["sd"], w=["rstd"])
            S.op("dve", lambda e, t=t: e.scalar_tensor_tensor(out=xs[:], in0=xres[:, t, :], scalar=stat[:, 3:4], in1=gb[:],
                                                              op0=ALU.mult, op1=ALU.mult),
                 r=[("x", t), "rstd", "gb", "stage0"], w=["xs", "stage1"])
            for half in range(2):
                pb = ps[2 * (t % 2) + half]
                pbv = pb.bitcast(BF16)
                pk = ("ps", 2 * (t % 2) + half)
                for j in range(8):
                    kc = half * 8 + j
                    S.op("pe", lambda e, pbv=pbv, j=j, kc=kc: e.transpose(out=pbv[:, j * 128:(j + 1) * 128],
                                                                          in_=xs[:, kc * 128:(kc + 1) * 128], identity=ident_b[:]),
                         r=["xs", "stage1", "ident_b"], w=[pk])
                tbk = ("hT", 0 if t < 4 else (1 if t < 8 else 2))
                c0 = t * 128
                if half == 0:
                    S.op("act", lambda e, pbv=pbv, half=half, c0=c0, ncol=ncol: e.activation(
                        out=hT[:, half * 8:(half + 1) * 8, c0:c0 + ncol],
                        in_=pbv.rearrange("p (j c) -> p j c", j=8)[:, :, 0:ncol], func=AF.Copy), r=[pk], w=[tbk])
                else:
                    S.op("dve", lambda e, pbv=pbv, half=half, c0=c0, ncol=ncol: e.tensor_copy(
                        out=hT[:, half * 8:(half + 1) * 8, c0:c0 + ncol],
                        in_=pbv.rearrange("p (j c) -> p j c", j=8)[:, :, 0:ncol]), r=[pk], w=[tbk])

    wctr = [0]
    suse = [0, 0, 0]

    def stage_slot():
        i = wctr[0]
        wctr[0] += 1
        k = i % 3
        suse[k] += 1
        return stage[k], f"stage{k}", f"stage{k}_{suse[k] // 100}", i

    def load_up_tile(src_ap, cast_eng):
        st, sk, sg, i = stage_slot()
        wb = wup[i % 4]
        wk = f"wup{i % 4}"
        S.op("sp", lambda e: e.dma_start(out=st[:], in_=src_ap), w=[sk], dma=sg)
        if cast_eng == "act":
            S.op("act", lambda e: e.activation(out=wb[:], in_=st[:], func=AF.Copy), r=[sk], w=[wk])
        else:
            S.op(cast_eng, lambda e: e.tensor_copy(out=wb[:], in_=st[:]), r=[sk], w=[wk])
        return wb, wk

    def up_mm(wb, wk, tb, pbank, pkey):
        c0, n = TB[tb]
        for kc in range(16):
            S.op("pe", lambda e, kc=kc: e.matmul(pbank[:, 0:n], lhsT=wb[:, kc, :], rhs=hT[:, kc, c0:c0 + n],
                                                 start=(kc == 0), stop=(kc == 15)),
                 r=[wk, ("hT", tb)], w=[pkey])

    def ffn(w, ntiles):
        w1, w3, w2 = w
        ntb = 3 if ntiles == NTT else 2
        pctr = 0
        for grp in range(NFF // G):
            gt = gT[grp % 2]
            w2t = w2g[grp % 2]
            for j in range(G):
                fc = grp * G + j
                wb1, wk1 = load_up_tile(w1[fc], "pool")
                wb3, wk3 = load_up_tile(w3[fc], "dve")
                st, sk, sg, i = stage_slot()
                S.op("sp", lambda e, st=st, fc=fc: e.dma_start(out=st[:].rearrange("p a b -> p (a b)"), in_=w2[fc]),
                     w=[sk], dma=sg)
                S.op("act", lambda e, st=st, w2t=w2t, j=j: e.activation(out=w2t[:, j, :], in_=st[:].rearrange("p a b -> p (a b)"),
                                                                        func=AF.Copy), r=[sk], w=[("w2g", grp % 2, j)])
                for tb in range(ntb):
                    c0, n = TB[tb]
                    pa, pbk = 2 * (pctr % 3), 2 * (pctr % 3) + 1
                    pctr += 1
                    up_mm(wb1, wk1, tb, ps[pa], ("ps", pa))
                    up_mm(wb3, wk3, tb, ps[pbk], ("ps", pbk))
                    sl = sil[pctr % 2]
                    slk = ("sil", pctr % 2)
                    S.op("act", lambda e, sl=sl, pa=pa, n=n: e.activation(out=sl[:, 0:n], in_=ps[pa][:, 0:n], func=AF.Silu),
                         r=[("ps", pa)], w=[slk])
                    S.op("dve", lambda e, sl=sl, pbk=pbk, n=n, c0=c0, gt=gt, j=j: e.tensor_tensor(
                        out=gt[:, j, c0:c0 + n], in0=sl[:, 0:n], in1=ps[pbk][:, 0:n], op=ALU.mult),
                         r=[slk, ("ps", pbk)], w=[("gT", grp % 2, j, tb)])
            for t in range(ntiles):
                m = 128 if t < NPT else 35
                tb = 0 if t < 4 else (1 if t < 8 else 2)
                for nb in range(4):
                    pd = 6 + (t * 4 + nb) % 2
                    for j in range(G):
                        S.op("pe", lambda e, t=t, j=j, nb=nb, pd=pd, m=m, gt=gt, w2t=w2t: e.matmul(
                            ps[pd][0:m, :], lhsT=gt[:, j, t * 128:t * 128 + m], rhs=w2t[:, j, nb * 512:(nb + 1) * 512],
                            start=(j == 0), stop=(j == G - 1)),
                             r=[("gT", grp % 2, j, tb), ("w2g", grp % 2, j)], w=[("ps", pd)])
                    S.op("dve", lambda e, t=t, nb=nb, pd=pd, m=m: e.scalar_tensor_tensor(
                        out=xres[0:m, t, nb * 512:(nb + 1) * 512], in0=ps[pd][0:m, :], scalar=0.5,
                        in1=xres[0:m, t, nb * 512:(nb + 1) * 512], op0=ALU.mult, op1=ALU.add),
                         r=[("ps", pd), ("x", t)], w=[("x", t)])

    QSCALE = 128.0 ** -0.5

    def inproj(main):
        ntiles = NTT if main else NPT
        ntb = 3 if main else 2
        chunks = list(range(NIN)) if main else [8, 9, 10, 11] + list(range(28, 40)) + [41, 42]
        if main and "q" in KSKIP:
            chunks = [c for c in chunks if not (c < 8 or 12 <= c < 28)]
        pctr = 0
        tctr = [0]
        if main:
            S.op("sp", lambda e: e.dma_start(out=halo[:], in_=pre_halo.rearrange("p (a b) -> p a b", b=3)),
                 w=["halo"], dma="pre")
            S.dma_batch_end("pre")
        else:
            S.op("pool", lambda e: e.memset(halo[:], 0.0), w=["halo"])
        S.op("pool", lambda e: e.memset(vb1[:, :, :, 128:129], 1.0), w=["vb1ones"])
        for c in chunks:
            wb, wk = load_up_tile(win[c], "pool" if c % 2 == 0 else "dve")
            is_q, is_k, is_v = c < 8, 8 <= c < 10, 10 <= c < 12
            is_qi, is_z, is_xbc = 12 <= c < 20, 20 <= c < 28, 28 <= c < 40
            is_sm, is_ki2, is_aux = c == 40, c == 41, c == 42
            if not is_v and not is_sm:
                for tb in range(ntb):
                    c0, n = TB[tb]
                    pa = pctr % 6
                    pctr += 1
                    up_mm(wb, wk, tb, ps[pa], ("ps", pa))
                    src = ps[pa][:, 0:n]
                    if is_q:
                        S.op("act", lambda e, src=src, c=c, c0=c0, n=n: e.activation(out=qT[:, c, c0:c0 + n], in_=src, func=AF.Copy, scale=QSCALE),
                             r=[("ps", pa)], w=[("qT", c, tb)])
                    elif is_k:
                        S.op("act", lambda e, src=src, c=c, c0=c0, n=n: e.activation(out=kT[:, c - 8, c0:c0 + n], in_=src, func=AF.Copy),
                             r=[("ps", pa)], w=[("kT", c - 8, tb)])
                    elif is_qi:
                        S.op("act", lambda e, src=src, c=c, c0=c0, n=n: e.activation(out=qiT[:, c - 12, c0:c0 + n], in_=src, func=AF.Copy),
                             r=[("ps", pa)], w=[("qiT", c - 12, tb)])
                    elif is_z:
                        S.op("act", lambda e, src=src, c=c, c0=c0, n=n: e.activation(out=szT[:, c - 20, c0:c0 + n], in_=src, func=AF.Silu),
                             r=[("ps", pa)], w=[("szT", c - 20, tb)])
                    elif is_ki2:
                        S.op("act", lambda e, src=src, c0=c0, n=n: e.activation(out=kiT2[:, c0:c0 + n], in_=src, func=AF.Copy),
                             r=[("ps", pa)], w=[("kiT2", tb)])
                    elif is_aux:
                        S.op("act", lambda e, pa=pa, c0=c0, n=n: e.activation(out=dtT[0:16, c0:c0 + n], in_=ps[pa][0:16, 0:n], func=AF.Copy),
                             r=[("ps", pa)], w=[("dtT", tb)])
                    elif is_xbc:
                        S.op("act", lambda e, src=src, c0=c0, n=n: e.activation(out=rawx[:, 3 + c0:3 + c0 + n], in_=src, func=AF.Copy),
                             r=[("ps", pa)], w=[("rawx", tb)])
            if is_xbc:
                xc = c - 28
                rk = [("rawx", tb) for tb in range(ntb)]
                S.op("dve", lambda e, xc=xc: e.tensor_copy(out=rawx[:, 0:3], in_=halo[:, xc, :]), r=["halo"], w=["rawxh"])
                if not main:
                    S.op("dve", lambda e, xc=xc: e.tensor_copy(out=halo[:, xc, :], in_=rawx[:, 3 + 1021:3 + 1024]),
                         r=rk + ["rawxh"], w=["halo"])
                S.op("dve", lambda e, xc=xc: e.tensor_scalar(out=cacc[:], in0=rawx[:, 0:1024], scalar1=cw_t[:, xc, 0:1], scalar2=None,
                                                             op0=ALU.mult), r=rk + ["rawxh", ("cst", 1)], w=["cacc"])
                for k in range(1, 4):
                    S.op("dve", lambda e, xc=xc, k=k: e.scalar_tensor_tensor(out=cacc[:], in0=rawx[:, k:k + 1024], scalar=cw_t[:, xc, k:k + 1],
                                                                            in1=cacc[:], op0=ALU.mult, op1=ALU.add),
                         r=rk + ["rawxh"], w=["cacc"])
                S.op("act", lambda e, xc=xc: e.activation(out=xcT[:, xc, 0:1024], in_=cacc[:], func=AF.Silu, bias=cb_t[:, xc:xc + 1]),
                     r=["cacc", ("cst", 2)], w=[("xcT", xc)])
                if main and "s" not in KSKIP:
                    S.op("dve", lambda e, xc=xc: e.tensor_copy(out=exts[:, :, 0:3], in_=sct[:, xc, :, :]), r=[("cst", 5)], w=["exts"])
                    S.op("dve", lambda e: e.tensor_copy(out=exts[:, :, 3:11], in_=rawx[:, 3 + 1024:3 + 1056].rearrange("p (b t) -> p b t", t=8)),
                         r=rk, w=["exts"])
                    S.op("dve", lambda e, xc=xc: e.tensor_scalar(out=cacc[:, 0:32].rearrange("p (b t) -> p b t", t=8), in0=exts[:, :, 0:8],
                                                                 scalar1=cw_t[:, xc, 0:1], scalar2=None, op0=ALU.mult),
                         r=["exts", ("xcT", xc)], w=["cacc"])
                    for k in range(1, 4):
                        S.op("dve", lambda e, xc=xc, k=k: e.scalar_tensor_tensor(
                            out=cacc[:, 0:32].rearrange("p (b t) -> p b t", t=8), in0=exts[:, :, k:k + 8], scalar=cw_t[:, xc, k:k + 1],
                            in1=cacc[:, 0:32].rearrange("p (b t) -> p b t", t=8), op0=ALU.mult, op1=ALU.add), r=["exts"], w=["cacc"])
                    S.op("act", lambda e, xc=xc: e.activation(out=xcT[:, xc, 1024:1056], in_=cacc[:, 0:32], func=AF.Silu, bias=cb_t[:, xc:xc + 1]),
                         r=["cacc"], w=[("xcT", xc)])
                if main and "t" not in KSKIP:
                    srcs = [1021] + [1024 + b * 8 + 5 for b in range(4)]
                    for si, s0 in enumerate(srcs):
                        pb, off = (7, si * 128) if si < 4 else (6, 0)
                        S.op("pe", lambda e, s0=s0, pb=pb, off=off: e.transpose(out=ps[pb][0:3, off:off + 128],
                                                                              in_=rawx[:, 3 + s0:3 + s0 + 3], identity=ident_f[:]),
                             r=rk + ["ident_f"], w=[("ps", pb)])
                    tl = tails[0]
                    tlk = ("tails", 0)
                    S.op("act", lambda e, tl=tl: e.activation(out=tl[0:3, 0:4, :], in_=ps[7][0:3, 0:512].rearrange("p (s c) -> p s c", s=4),
                                                              func=AF.Copy), r=[("ps", 7)], w=[tlk])
                    S.op("act", lambda e, tl=tl: e.activation(out=tl[0:3, 4, :], in_=ps[6][0:3, 0:128], func=AF.Copy), r=[("ps", 6)], w=[tlk])
                    S.op("sp", lambda e, tl=tl, xc=xc: e.dma_start(out=conv_out[:, :, xc * 128:(xc + 1) * 128].rearrange("s r c -> r s c"),
                                                                   in_=tl[0:3, :, :]), r=[tlk], w=[("conv_out", xc)], dma="tl0")
            if is_k or is_v or is_sm:
                if is_k and not main:
                    continue
                col = (c - 8) * 128 if (is_k or is_v) else 512
                for t in range(ntiles):
                    m = 128 if t < NPT else 35
                    tb = 0 if t < 4 else (1 if t < 8 else 2)
                    pd = 6 + t % 2
                    for kc in range(16):
                        S.op("pe", lambda e, t=t, kc=kc, pd=pd, m=m, wb=wb: e.matmul(
                            ps[pd][0:m, 0:128], lhsT=hT[:, kc, t * 128:t * 128 + m], rhs=wb[:, kc, :],
                            start=(kc == 0), stop=(kc == 15)), r=[wk, ("hT", tb)], w=[("ps", pd)])
                    if is_v:
                        S.op("dve", lambda e, t=t, pd=pd, c=c: e.tensor_copy(out=vb1[:, t, c - 10, 0:128], in_=ps[pd][:, 0:128]),
                             r=[("ps", pd)], w=[("vb1", t)])
                    if is_sm:
                        S.op("dve", lambda e, t=t, pd=pd: e.tensor_copy(out=sm_tok[:, t, :], in_=ps[pd][:, 0:128]),
                             r=[("ps", pd)], w=[("sm_tok", t)])
                    if main and "o" not in KSKIP:
                        ti = tctr[0] % 2
                        tctr[0] += 1
                        if "e" not in KSKIP:
                            S.op("dve", lambda e, ti=ti, pd=pd: e.tensor_copy(out=tokst[ti][:], in_=ps[pd][:, 0:128]),
                                 r=[("ps", pd)], w=[("tokst", ti)])
                        if "d" not in KSKIP:
                          S.op(os.environ.get("KTOKQ", "sp"), lambda e, t=t, ti=ti, col=col: e.dma_start(out=kvs_out[t, :, col:col + 128], in_=tokst[ti][:]),
                             r=[("tokst", ti)], w=[("kvs_out", t, col)], dma=f"tok{ti}")

    dtk, dta, acum, ea, dec, eal, scd, tmp16 = sm16

    def ssd_chunk(c0, L, full):
        cs0, cs1 = c0, c0 + L
        K = ["ssd"]

        def Q(eng, fn):
            S.op(eng, fn, w=K)

        Q("pe", lambda e: e.transpose(out=ps[6][0:L, 0:16], in_=dtT[0:16, cs0:cs1], identity=ident_f[0:16, 0:16]))
        Q("dve", lambda e: e.tensor_tensor(out=tmp16[0:L, :], in0=ps[6][0:L, 0:16], in1=dtb_t[0:L, :], op=ALU.add))
        Q("act", lambda e: e.activation(out=tmp16[0:L, :], in_=tmp16[0:L, :], func=AF.Exp))
        Q("act", lambda e: e.activation(out=dtk[0:L, :], in_=tmp16[0:L, :], func=AF.Ln, bias=1.0))
        Q("dve", lambda e: e.tensor_tensor(out=dta[0:L, :], in0=dtk[0:L, :], in1=a_t[0:L, :], op=ALU.mult))
        Q("pe", lambda e: e.matmul(ps[6][0:L, 16:32], lhsT=tri_f[0:L, 0:L], rhs=dta[0:L, :], start=True, stop=True))
        Q("pe", lambda e: e.matmul(ps[6][:, 32:48], lhsT=ones_f[0:L, :], rhs=dta[0:L, :], start=True, stop=True))
        Q("dve", lambda e: e.tensor_copy(out=acum[0:L, :], in_=ps[6][0:L, 16:32]))
        Q("act", lambda e: e.activation(out=ea[0:L, :], in_=acum[0:L, :], func=AF.Exp))
        Q("dve", lambda e: e.tensor_tensor(out=dec[0:L, :], in0=ps[6][0:L, 32:48], in1=acum[0:L, :], op=ALU.subtract))
        Q("act", lambda e: e.activation(out=dec[0:L, :], in_=dec[0:L, :], func=AF.Exp))
        Q("act", lambda e: e.activation(out=eal[:, :], in_=ps[6][:, 32:48], func=AF.Exp))
        Q("dve", lambda e: e.tensor_tensor(out=scd[0:L, :], in0=dtk[0:L, :], in1=dec[0:L, :], op=ALU.mult))
        pv = [ps[0].bitcast(BF16), ps[1].bitcast(BF16)]
        for j in range(10):
            dst = pv[0][0:L, j * 128:(j + 1) * 128] if j < 8 else pv[1][0:L, (j - 8) * 128:(j - 7) * 128]
            Q("pe", lambda e, j=j, dst=dst: e.transpose(out=dst, in_=xcT[:, j, cs0:cs1], identity=ident_b[:]))
        xv = pv[0][0:L, 0:1024].rearrange("p (h q) -> p h q", q=64)
        Q("dve", lambda e: e.tensor_tensor(out=xd[0:L, :].rearrange("p (h q) -> p h q", q=64), in0=xv,
                                           in1=dtk[0:L, :].rearrange("p (h o) -> p h o", o=1).to_broadcast([L, 16, 64]), op=ALU.mult))
        Q("dve", lambda e: e.tensor_tensor(out=xdd[0:L, :].rearrange("p (h q) -> p h q", q=64), in0=xv,
                                           in1=scd[0:L, :].rearrange("p (h o) -> p h o", o=1).to_broadcast([L, 16, 64]), op=ALU.mult))
        Q("act", lambda e: e.activation(out=Btok[0:L, :], in_=pv[1][0:L, 0:256], func=AF.Copy))
        if full:
            for g in range(2):
                Q("pe", lambda e, g=g: e.matmul(ps[6][0:L, 128 + g * 128:128 + g * 128 + L], lhsT=xcT[:, 8 + g, cs0:cs1],
                                               rhs=xcT[:, 10 + g, cs0:cs1], start=True, stop=True))
                Q("dve", lambda e, g=g: e.tensor_tensor(out=CBm[0:L, g, 0:L], in0=ps[6][0:L, 128 + g * 128:128 + g * 128 + L],
                                                        in1=tri_f[0:L, 0:L], op=ALU.mult))
            for half in range(2):
                Q("dve", lambda e, half=half: e.tensor_tensor(
                    out=Dm[0:L, :, 0:L], in0=ident_f[0:L, 0:L].rearrange("p (o i) -> p o i", o=1).to_broadcast([L, 8, L]),
                    in1=acum[0:L, half * 8:(half + 1) * 8].rearrange("p (h o) -> p h o", o=1).to_broadcast([L, 8, L]), op=ALU.mult))
                for q4 in range(2):
                    Q("pe", lambda e, q4=q4: e.matmul(ps[2 + q4][0:L, 0:4 * L].rearrange("p (h i) -> p h i", h=4), lhsT=ones_f[0:L, 0:L],
                                                     rhs=Dm[0:L, 4 * q4:4 * q4 + 4, 0:L], start=True, stop=True))
                for hh in range(8):
                    h = half * 8 + hh
                    bsrc = ps[2 + hh // 4][0:L, (hh % 4) * L:(hh % 4 + 1) * L]
                    Q("dve", lambda e, bsrc=bsrc, h=h: e.tensor_scalar(out=t1[0:L, 0:L], in0=bsrc, scalar1=acum[0:L, h:h + 1], scalar2=0.0,
                                                                       op0=ALU.subtract, op1=ALU.min))
                    Q("act", lambda e: e.activation(out=t2[0:L, 0:L], in_=t1[0:L, 0:L], func=AF.Exp))
                    Q("dve", lambda e, h=h: e.tensor_tensor(out=Mh[0:L, 0:L], in0=t2[0:L, 0:L], in1=CBm[0:L, h // 8, 0:L], op=ALU.mult))
                    Q("pe", lambda e, h=h: e.matmul(ps[h // 8][0:L, (h % 8) * 64:(h % 8 + 1) * 64], lhsT=Mh[0:L, 0:L],
                                                   rhs=xd[0:L, h * 64:(h + 1) * 64], start=True, stop=True))
            for g in range(2):
                Q("pe", lambda e, g=g: e.matmul(ps[4 + g][0:L, :], lhsT=xcT[:, 10 + g, cs0:cs1], rhs=Hb[:, g * 512:(g + 1) * 512],
                                               start=True, stop=True))
                Q("act", lambda e, g=g: e.activation(out=ysb[0:L, g * 512:(g + 1) * 512], in_=ps[g][0:L, :], func=AF.Copy))
            for h in range(16):
                Q("dve", lambda e, h=h: e.scalar_tensor_tensor(out=ysb[0:L, h * 64:(h + 1) * 64], in0=ps[4 + h // 8][0:L, (h % 8) * 64:(h % 8 + 1) * 64],
                                                               scalar=ea[0:L, h:h + 1], in1=ysb[0:L, h * 64:(h + 1) * 64], op0=ALU.mult, op1=ALU.add))
        for g in range(2):
            Q("pe", lambda e, g=g: e.matmul(ps[2 + g][:, :], lhsT=Btok[0:L, g * 128:(g + 1) * 128], rhs=xdd[0:L, g * 512:(g + 1) * 512],
                                           start=True, stop=True))
        Q("dve", lambda e: e.tensor_tensor(out=Hst[:].rearrange("p (h q) -> p h q", q=64), in0=Hst[:].rearrange("p (h q) -> p h q", q=64),
                                           in1=eal[:, :].rearrange("p (h o) -> p h o", o=1).to_broadcast([128, 16, 64]), op=ALU.mult))
        for g in range(2):
            Q("dve", lambda e, g=g: e.tensor_tensor(out=Hst[:, g * 512:(g + 1) * 512], in0=Hst[:, g * 512:(g + 1) * 512], in1=ps[2 + g][:, :],
                                                    op=ALU.add))
        Q("act", lambda e: e.activation(out=Hb[:], in_=Hst[:], func=AF.Copy))
        if full:
            for j in range(8):
                Q("pe", lambda e, j=j: e.transpose(out=ps[4 + j // 4][:, (j % 4) * L:(j % 4 + 1) * L], in_=ysb[0:L, j * 128:(j + 1) * 128],
                                                  identity=ident_f[0:L, 0:L]))
            for j in range(8):
                ysrc = ps[4 + j // 4][:, (j % 4) * L:(j % 4 + 1) * L]
                Q("dve", lambda e, j=j, ysrc=ysrc: e.scalar_tensor_tensor(out=gbuf[:, j, 0:L], in0=xcT[:, j, cs0:cs1], scalar=dcol_t[:, j:j + 1],
                                                                          in1=ysrc, op0=ALU.mult, op1=ALU.add))
                Q("dve", lambda e, j=j: e.tensor_tensor(out=gbuf[:, j, 0:L], in0=gbuf[:, j, 0:L], in1=szT[:, j, cs0:cs1], op=ALU.mult))
                Q("act", lambda e, j=j: e.activation(out=sq[:, j, 0:L], in_=gbuf[:, j, 0:L], func=AF.Square))
            for g in range(2):
                for jj in range(4):
                    Q("pe", lambda e, g=g, jj=jj: e.matmul(ps[7][:, g * 128:g * 128 + L], lhsT=ones_b[:, :], rhs=sq[:, 4 * g + jj, 0:L],
                                                          start=(jj == 0), stop=(jj == 3)))
                Q("dve", lambda e, g=g: e.tensor_scalar(out=rs[:, g, 0:L], in0=ps[7][:, g * 128:g * 128 + L], scalar1=1.0 / 512, scalar2=EPS,
                                                        op0=ALU.mult, op1=ALU.add))
                Q("act", lambda e, g=g: e.activation(out=rs[:, g, 0:L], in_=rs[:, g, 0:L], func=AF.Sqrt))
                Q("dve", lambda e, g=g: e.reciprocal(out=rs[:, g, 0:L], in_=rs[:, g, 0:L]))
            for j in range(8):
                Q("dve", lambda e, j=j: e.scalar_tensor_tensor(out=ssdT[:, j, cs0:cs1], in0=gbuf[:, j, 0:L], scalar=gssd_t[:, j:j + 1],
                                                               in1=rs[:, j // 4, 0:L], op0=ALU.mult, op1=ALU.mult))


    sc = A.at(OH + 10272, [128, 2048], F32, "sc")
    mk = A.at(OH + 10272 + 8192, [128, 2048], BF16, "mk")
    mkT = A.at(OH + 10272 + 12288, [128, 16, 128], BF16, "mkT")
    o = O_STAGE
    bT = A.at(o, [128, 2, 8, 128], F32, "bT"); o += 8192
    rtmp = A.at(o, [128, 2048], F32, "rtmp"); o += 8192
    pexp = A.at(o, [128, 512], F32, "pexp"); o += 2048
    pm = A.at(o, [128, 512], BF16, "pm"); o += 1024
    atok = A.at(o, [128, 1024], BF16, "atok"); o += 2048
    hw = A.at(o, [128, 32], F32, "hw"); o += 128
    p2 = A.at(o, [128, 32], F32, "p2"); o += 128
    wis = A.at(o, [128, 16], F32, "wis"); o += 64
    cfar = A.at(o, [128, 8], F32, "cfar"); o += 32
    rden = A.at(o, [128, 8], F32, "rden"); o += 32
    a1 = A.at(o, [128, 8], F32, "a1"); o += 32
    rb_t = A.at(o, [32, 8], F32, "rb_t"); o += 32
    oh_t = A.at(o, [32, 256], F32, "oh_t"); o += 1024
    rbb = A.at(o, [32, 8, 128], F32, "rbb"); o += 4096
    assert o <= O_W2G
    NIT = 30

    atn = [0]

    def AQ(eng, fn, dma=None):
        if dma is not None:
            atn[0] += 1
            dma = f"at{atn[0] // 100}"
        S.op(eng, fn, w=["att"], dma=dma)

    def att_setup():
        AQ("sp", lambda e: e.dma_start(out=rb_t[:], in_=relb[:, :]), dma="at")
        AQ("sp", lambda e: e.dma_start(out=oh_t[:], in_=oh_d[:, :]), dma="at")
        AQ("sp", lambda e: e.dma_start(out=cfar[:], in_=relb[31:32, :].partition_broadcast(128)), dma="at")
        AQ("dve", lambda e: e.tensor_copy(out=rbb[:], in_=rb_t[:].rearrange("p (h o) -> p h o", o=1).to_broadcast([32, 8, 128])))
        for h in range(8):
            AQ("pe", lambda e, h=h: e.matmul(ps[0][:, 0:256], lhsT=rbb[:, h, :], rhs=oh_t[:, :], start=True, stop=True))
            AQ("dve", lambda e: e.tensor_copy(out=rtmp[:, 0:256], in_=ps[0][:, 0:256]))
            AQ("sp", lambda e, h=h: e.dma_start(out=bsc[h, :, :], in_=rtmp[:, 0:256]), dma="at")
        for h in range(8):
            for ty in range(2):
                src = bass.AP(tensor=bsc_t, offset=h * 128 * 256 + 128 * ty, ap=[[255, 128], [1, 128]])
                AQ("sp", lambda e, h=h, ty=ty, src=src: e.dma_start(out=bT[:, ty, h, :], in_=src), dma="at")
        for k in range(NIT):
            AQ("pool", lambda e, k=k: e.memset(p2[:, k:k + 1], 2.0 ** -(k + 1)))

    KATT = int(os.environ.get("KATT", "9"))
    KQT = int(os.environ.get("KQT", "8"))

    def att_prompt_tile(qt):
        if KATT < 2 or qt >= KQT:
            return
        q0 = qt * 128
        nb = 9 + qt
        Sx = nb * 128
        segs = [(kiT2_pre, 0, 512), (kiT2_pre, 512, 512)]
        own = 128 * (qt + 1)
        c = 0
        while c < own:
            n = min(512, own - c)
            segs.append((kiT2, c, n))
            c += n
        AQ("dve", lambda e: e.tensor_scalar(out=wis[:], in0=sm_tok[:, qt, 64:80], scalar1=1024.0 ** -0.5, scalar2=None, op0=ALU.mult))
        for hi in range(16):
            pb = 64 * (hi % 2)
            for si, (src, c0, n) in enumerate(segs):
                AQ("pe", lambda e, si=si, src=src, c0=c0, n=n, pb=pb, hi=hi: e.matmul(
                    ps[si][:, 0:n], lhsT=qiT[pb:pb + 64, hi // 2, q0:q0 + 128], rhs=src[pb:pb + 64, c0:c0 + n], start=True, stop=True))
            AQ("act", lambda e: e.activation(out=rtmp[:, 0:Sx], in_=psall[:, 0:Sx], func=AF.Relu))
            if hi == 0:
                AQ("dve", lambda e: e.tensor_scalar(out=sc[:, 0:Sx], in0=rtmp[:, 0:Sx], scalar1=wis[:, 0:1], scalar2=None, op0=ALU.mult))
            else:
                AQ("dve", lambda e, hi=hi: e.scalar_tensor_tensor(out=sc[:, 0:Sx], in0=rtmp[:, 0:Sx], scalar=wis[:, hi:hi + 1], in1=sc[:, 0:Sx],
                                                                  op0=ALU.mult, op1=ALU.add))
        if KATT < 3:
            return
        absm, lo, mid, cnt, tt, Wd = [a1[:, i:i + 1] for i in range(6)]
        AQ("dve", lambda e: e.tensor_reduce(out=absm, in_=sc[:, 0:Sx], axis=AX.X, op=ALU.max, apply_absolute_value=True))
        AQ("dve", lambda e: e.tensor_scalar(out=sc[:, 0:1024], in0=sc[:, 0:1024], scalar1=flag_t[:, 1:2], scalar2=None, op0=ALU.add))
        AQ("pool", lambda e: e.affine_select(out=sc[:, Sx - 128:Sx], in_=sc[:, Sx - 128:Sx], pattern=[[-1, 128]], compare_op=ALU.is_ge,
                                             fill=-1e30, base=0, channel_multiplier=1))
        AQ("dve", lambda e: e.tensor_scalar(out=lo, in0=absm, scalar1=-1.0, scalar2=-1.0, op0=ALU.mult, op1=ALU.add))
        AQ("dve", lambda e: e.tensor_scalar(out=Wd, in0=absm, scalar1=2.0, scalar2=2.0, op0=ALU.mult, op1=ALU.add))
        AQ("dve", lambda e: e.tensor_scalar(out=hw[:, 0:NIT], in0=p2[:, 0:NIT], scalar1=Wd, scalar2=None, op0=ALU.mult))
        for k in range(NIT):
            AQ("dve", lambda e, k=k: e.tensor_tensor(out=mid, in0=lo, in1=hw[:, k:k + 1], op=ALU.add))
            AQ("dve", lambda e: e.tensor_scalar(out=mk[:, 0:Sx], in0=sc[:, 0:Sx], scalar1=mid, scalar2=0.0, op0=ALU.is_ge, op1=ALU.add, accum_out=cnt))
            AQ("dve", lambda e, k=k: e.tensor_scalar(out=tt, in0=cnt, scalar1=256.0, scalar2=hw[:, k:k + 1], op0=ALU.is_ge, op1=ALU.mult))
            AQ("dve", lambda e: e.tensor_tensor(out=lo, in0=lo, in1=tt, op=ALU.add))
        AQ("dve", lambda e: e.tensor_scalar(out=mk[:, 0:Sx], in0=sc[:, 0:Sx], scalar1=lo, scalar2=None, op0=ALU.is_ge))
        if KATT < 4:
            return
        pv0, pv1 = ps[0].bitcast(BF16), ps[1].bitcast(BF16)
        for kb in range(nb):
            dst = pv0[:, kb * 128:(kb + 1) * 128] if kb < 8 else pv1[:, (kb - 8) * 128:(kb - 7) * 128]
            AQ("pe", lambda e, kb=kb, dst=dst: e.transpose(out=dst, in_=mk[:, kb * 128:(kb + 1) * 128], identity=ident_b[:]))
        AQ("dve", lambda e: e.tensor_copy(out=mkT[:, 0:8, :], in_=pv0.rearrange("p (k c) -> p k c", c=128)))
        AQ("dve", lambda e: e.tensor_copy(out=mkT[:, 8:nb, :], in_=pv1[:, 0:(nb - 8) * 128].rearrange("p (k c) -> p k c", c=128)))

        if KATT < 5:
            return

        def pso(h):
            return ps[5 + h // 3][:, (h % 3) * 160:(h % 3) * 160 + 129]

        for kb in range(nb):
            pre = kb < 8
            j = kb if pre else kb - 8
            ksrc = kT_pre if pre else kT
            vsrc = vb1_pre if pre else vb1
            diff = (8 + qt) - kb
            for kv in range(2):
                AQ("pe", lambda e, kv=kv, ksrc=ksrc, j=j: e.matmul(ps[2 + kv].rearrange("p (h c) -> p h c", c=128), lhsT=ksrc[:, kv, j * 128:(j + 1) * 128],
                                                                  rhs=qT[:, 4 * kv:4 * kv + 4, q0:q0 + 128], start=True, stop=True))
                if diff >= 2:
                    for hh in range(4):
                        AQ("act", lambda e, kv=kv, hh=hh: e.activation(out=pexp[:, hh * 128:(hh + 1) * 128], in_=ps[2 + kv][:, hh * 128:(hh + 1) * 128],
                                                                       func=AF.Exp, bias=cfar[:, 4 * kv + hh:4 * kv + hh + 1]))
                else:
                    AQ("dve", lambda e, kv=kv, diff=diff: e.tensor_tensor(out=pexp[:].rearrange("p (h c) -> p h c", c=128),
                                                                          in0=ps[2 + kv].rearrange("p (h c) -> p h c", c=128),
                                                                          in1=bT[:, diff, 4 * kv:4 * kv + 4, :], op=ALU.add))
                    AQ("act", lambda e: e.activation(out=pexp[:], in_=pexp[:], func=AF.Exp))
                AQ("dve", lambda e, kb=kb: e.tensor_tensor(out=pm[:].rearrange("p (h c) -> p h c", c=128), in0=pexp[:].rearrange("p (h c) -> p h c", c=128),
                                                           in1=mkT[:, kb:kb + 1, :].to_broadcast([128, 4, 128]), op=ALU.mult))
                for hh in range(4):
                    h = 4 * kv + hh
                    AQ("pe", lambda e, h=h, hh=hh, vsrc=vsrc, j=j, kv=kv, kb=kb: e.matmul(pso(h), lhsT=pm[:, hh * 128:(hh + 1) * 128], rhs=vsrc[:, j, kv, :],
                                                                                       start=(kb == 0), stop=(kb == nb - 1)))
        if KATT < 6:
            return
        for b3 in range(3):
            nh = 3 if b3 < 2 else 2
            AQ("dve", lambda e, b3=b3, nh=nh: e.reciprocal(out=rden[:, 3 * b3:3 * b3 + nh],
                                                          in_=ps[5 + b3][:, 0:480].rearrange("p (h c) -> p h c", c=160)[:, 0:nh, 128]))
        for h in range(8):
            AQ("dve", lambda e, h=h: e.tensor_scalar(out=atok[:, h * 128:(h + 1) * 128], in0=pso(h)[:, 0:128], scalar1=rden[:, h:h + 1], scalar2=None,
                                                     op0=ALU.mult))
        for h in range(8):
            AQ("pe", lambda e, h=h: e.transpose(out=pv0[:, h * 128:(h + 1) * 128], in_=atok[:, h * 128:(h + 1) * 128], identity=ident_b[:]))
        AQ("dve", lambda e: e.tensor_copy(out=attT[:, :, q0:q0 + 128], in_=pv0.rearrange("p (k c) -> p k c", c=128)))
        S.op("pool", lambda e: e.memset(stat[:, 6:7], 0.0), r=["att"], w=["mixT"])


    def att_sample():
        o = O_W2G
        def T(shape, dt, nm):
            nonlocal o
            nb_ = int(np.prod(shape[1:])) * (2 if dt == BF16 else 4)
            t_ = A.at(o, shape, dt, nm)
            o = al(o + nb_)
            return t_
        pti = T([128, 128], I32, "s_pti"); ptf = T([128, 128], F32, "s_ptf"); idx_i = T([128, 128], I32, "s_idx")
        iota_c = T([128, 8], F32, "s_iota")
        kig = T([128, 4, 64], F32, "s_kig"); kiTs = T([64, 512], BF16, "s_kiTs")
        qiS = T([64, 16, 8], BF16, "s_qiS")
        wis32 = T([32, 16], F32, "s_wis32"); wd32 = T([32, 16, 8], F32, "s_wd32"); wdb = T([32, 128], F32, "s_wdb")
        wrow = T([128, 128], F32, "s_wrow")
        bm_t = T([32, 4], F32, "s_bm"); dq_t = T([32, 8], F32, "s_dq"); pen_t = T([32, 4, 8], F32, "s_pen")
        scS = T([128, 132, 8], F32, "s_scS"); ind = T([128, 132, 8], BF16, "s_ind"); mS = T([128, 132, 8], BF16, "s_mS")
        KVg = T([128, 4, 512], F32, "s_KVg")
        kTs = T([128, 4, 2, 128], BF16, "s_kTs"); Vb = T([128, 4, 2, 129], BF16, "s_Vb")
        pS = T([128, 256], F32, "s_pS"); pmS = T([128, 256], BF16, "s_pmS")
        cfS = T([128, 8, 8], F32, "s_cfS"); bSl = T([128, 8, 8], F32, "s_bSl"); bSn = T([32, 8, 8], F32, "s_bSn")
        rw = [T([128, 8], F32, f"s_rw{i}") for i in range(8)]
        dg = T([8, 8], F32, "s_dg"); m2 = T([8, 8], F32, "s_m2")
        atS = T([32, 2, 128], BF16, "s_atS"); rdS = T([32, 8], F32, "s_rdS")
        assert o <= O_MISC - 2048, o
        absr, lor, midr, cntr, ttr, Wr, pc, hwr = rw
        NPG = 128

        AQ("pool", lambda e: e.iota(iota_c[:, 0:1], pattern=[[0, 1]], base=0, channel_multiplier=1, allow_small_or_imprecise_dtypes=True))
        AQ("sp", lambda e: e.dma_start(out=bm_t[:], in_=bm4[:, :]), dma="at")
        AQ("sp", lambda e: e.dma_start(out=dq_t[:], in_=dq8[:, :]), dma="at")
        AQ("sp", lambda e: e.dma_start(out=pen_t[:], in_=pen32[:, :, :]), dma="at")
        AQ("dve", lambda e: e.tensor_scalar(out=wis32[:], in0=sm_tok[0:32, 8, 64:80], scalar1=1024.0 ** -0.5, scalar2=None, op0=ALU.mult))
        AQ("dve", lambda e: e.tensor_tensor(out=wd32[:], in0=wis32[:].rearrange("p (h o) -> p h o", o=1).to_broadcast([32, 16, 8]),
                                            in1=dq_t[:].rearrange("p (o q) -> p o q", o=1).to_broadcast([32, 16, 8]), op=ALU.mult))
        AQ("dve", lambda e: e.tensor_copy(out=cfS[:], in_=cfar[:].rearrange("p (h o) -> p h o", o=1).to_broadcast([128, 8, 8])))
        for h in range(8):
            src = bass.AP(tensor=bsc_t, offset=h * 128 * 256 + 128, ap=[[255, 128], [1, 8]])
            AQ("sp", lambda e, h=h, src=src: e.dma_start(out=bSl[:, h, :], in_=src), dma="at")
            for tb in range(4):
                src2 = bass.AP(tensor=bsc_t, offset=h * 128 * 256, ap=[[255, 8], [1, 8]])
                AQ("sp", lambda e, h=h, tb=tb, src2=src2: e.dma_start(out=bSn[8 * tb:8 * tb + 8, h, :], in_=src2), dma="at")
        AQ("pool", lambda e: e.memset(Vb[:, :, :, 128:129], 1.0))

        for b in range(4):
            cb = 1024 + 8 * b
            AQ("sp", lambda e, b=b: e.dma_start(out=pti[:], in_=pt4[b:b + 1, :].partition_broadcast(128)), dma="at")
            AQ("dve", lambda e: e.tensor_copy(out=ptf[:], in_=pti[:]))
            AQ("dve", lambda e: e.tensor_scalar(out=ptf[:], in0=ptf[:], scalar1=128.0, scalar2=iota_c[:, 0:1], op0=ALU.mult, op1=ALU.add))
            AQ("dve", lambda e: e.tensor_copy(out=idx_i[:], in_=ptf[:]))
            qv = qiS[:].rearrange("d (hc two) q -> d hc two q", two=2)
            AQ("sp", lambda e, cb=cb, qv=qv: e.dma_start(out=qv[:, :, 0, :], in_=qiT[0:64, :, cb:cb + 8]), dma="at")
            AQ("sp", lambda e, cb=cb, qv=qv: e.dma_start(out=qv[:, :, 1, :], in_=qiT[64:128, :, cb:cb + 8]), dma="at")
            qflat = qiS[:].rearrange("d h q -> d (h q)")
            AQ("dve", lambda e, b=b: e.tensor_scalar(out=wdb[:], in0=wd32[:].rearrange("p h q -> p (h q)"), scalar1=bm_t[:, b:b + 1], scalar2=None, op0=ALU.mult))
            AQ("pe", lambda e: e.matmul(ps[0][:, 0:128], lhsT=ones_f[0:32, :], rhs=wdb[:], start=True, stop=True))
            AQ("dve", lambda e: e.tensor_copy(out=wrow[:], in_=ps[0][:, 0:128]))
            AQ("pool", lambda e: e.memset(scS[:, 128, :], -1e30))
            AQ("pe", lambda e, qflat=qflat: e.matmul(ps[0][0:32, 0:128], lhsT=kiT2[0:64, 1024:1056], rhs=qflat, start=True, stop=True))
            AQ("act", lambda e: e.activation(out=rtmp[0:32, 0:128], in_=ps[0][0:32, 0:128], func=AF.Relu))
            AQ("dve", lambda e: e.tensor_tensor(out=rtmp[0:32, 0:128], in0=rtmp[0:32, 0:128], in1=wrow[0:32, :], op=ALU.mult))
            AQ("dve", lambda e: e.tensor_reduce(out=scS[0:32, 128, :], in_=rtmp[0:32, 0:128].rearrange("p (h q) -> p q h", q=8), axis=AX.X, op=ALU.add))
            AQ("dve", lambda e, b=b: e.tensor_tensor(out=scS[0:32, 128, :], in0=scS[0:32, 128, :], in1=pen_t[:, b, :], op=ALU.add))
            for st in range(NPG // 4):
                j0 = 4 * st
                for pg in range(4):
                    AQ("pool", lambda e, pg=pg, j0=j0: e.indirect_dma_start(out=kig[:, pg, :], out_offset=None, in_=kidx_tab[:, :],
                                                                          in_offset=bass.IndirectOffsetOnAxis(ap=idx_i[:, j0 + pg:j0 + pg + 1], axis=0)), dma="at")
                for pg in range(4):
                    AQ("pe", lambda e, pg=pg: e.transpose(out=ps[0][0:64, pg * 128:(pg + 1) * 128], in_=kig[:, pg, :], identity=ident_f[:]))
                AQ("dve", lambda e: e.tensor_copy(out=kiTs[:], in_=ps[0][0:64, :]))
                for pg in range(4):
                    AQ("pe", lambda e, pg=pg, qflat=qflat: e.matmul(ps[1][:, pg * 128:(pg + 1) * 128], lhsT=kiTs[:, pg * 128:(pg + 1) * 128], rhs=qflat,
                                                                   start=True, stop=True))
                AQ("act", lambda e: e.activation(out=rtmp[:, 0:512], in_=ps[1][:, :], func=AF.Relu))
                AQ("dve", lambda e: e.tensor_tensor(out=rtmp[:, 0:512].rearrange("p (g c) -> p g c", g=4), in0=rtmp[:, 0:512].rearrange("p (g c) -> p g c", g=4),
                                                    in1=wrow[:].rearrange("p (o c) -> p o c", o=1).to_broadcast([128, 4, 128]), op=ALU.mult))
                AQ("dve", lambda e, j0=j0: e.tensor_reduce(out=scS[:, j0:j0 + 4, :], in_=rtmp[:, 0:512].rearrange("p (g h q) -> p g q h", g=4, q=8),
                                                           axis=AX.X, op=ALU.add))
            AQ("dve", lambda e: e.tensor_reduce(out=pc[:], in_=scS[:, 0:128, :].rearrange("p k q -> p q k"), axis=AX.X, op=ALU.max, apply_absolute_value=True))
            AQ("pe", lambda e: e.transpose(out=ps[0][0:8, 0:128], in_=pc[:], identity=ident_f[:]))
            AQ("dve", lambda e: e.tensor_reduce(out=m2[:, 0:1], in_=ps[0][0:8, 0:128], axis=AX.X, op=ALU.max))
            AQ("dve", lambda e: e.tensor_scalar(out=dg[:], in0=ident_f[0:8, 0:8], scalar1=m2[:, 0:1], scalar2=None, op0=ALU.mult))
            AQ("pe", lambda e: e.matmul(ps[0][:, 0:8], lhsT=ones_f[0:8, :], rhs=dg[:], start=True, stop=True))
            AQ("dve", lambda e: e.tensor_copy(out=absr[:], in_=ps[0][:, 0:8]))
            AQ("dve", lambda e: e.tensor_scalar(out=lor[:], in0=absr[:], scalar1=-1.0, scalar2=-1.0, op0=ALU.mult, op1=ALU.add))
            AQ("dve", lambda e: e.tensor_scalar(out=Wr[:], in0=absr[:], scalar1=2.0, scalar2=2.0, op0=ALU.mult, op1=ALU.add))
            for k in range(NIT):
                AQ("dve", lambda e, k=k: e.tensor_scalar(out=hwr[:], in0=Wr[:], scalar1=2.0 ** -(k + 1), scalar2=None, op0=ALU.mult))
                AQ("dve", lambda e: e.tensor_tensor(out=midr[:], in0=lor[:], in1=hwr[:], op=ALU.add))
                AQ("dve", lambda e: e.tensor_tensor(out=ind[:, 0:129, :], in0=scS[:, 0:129, :],
                                                    in1=midr[:].rearrange("p (o q) -> p o q", o=1).to_broadcast([128, 129, 8]), op=ALU.is_ge))
                AQ("dve", lambda e: e.tensor_reduce(out=pc[:], in_=ind[:, 0:129, :].rearrange("p k q -> p q k"), axis=AX.X, op=ALU.add))
                AQ("pe", lambda e: e.matmul(ps[0][:, 0:8], lhsT=ones_f[:, :], rhs=pc[:], start=True, stop=True))
                AQ("dve", lambda e: e.tensor_scalar(out=ttr[:], in0=ps[0][:, 0:8], scalar1=256.0, scalar2=None, op0=ALU.is_ge))
                AQ("dve", lambda e: e.tensor_tensor(out=ttr[:], in0=ttr[:], in1=hwr[:], op=ALU.mult))
                AQ("dve", lambda e: e.tensor_tensor(out=lor[:], in0=lor[:], in1=ttr[:], op=ALU.add))
            AQ("dve", lambda e: e.tensor_tensor(out=mS[:, 0:129, :], in0=scS[:, 0:129, :],
                                                in1=lor[:].rearrange("p (o q) -> p o q", o=1).to_broadcast([128, 129, 8]), op=ALU.is_ge))
            def pso(kv):
                return ps[4][0:32, kv * 160:kv * 160 + 129]
            for st in range(NPG // 4):
                j0 = 4 * st
                for pg in range(4):
                    AQ("pool", lambda e, pg=pg, j0=j0: e.indirect_dma_start(out=KVg[:, pg, :], out_offset=None, in_=kv_tab[:, :],
                                                                          in_offset=bass.IndirectOffsetOnAxis(ap=idx_i[:, j0 + pg:j0 + pg + 1], axis=0)), dma="at")
                for pg in range(4):
                    for kv in range(2):
                        r_ = pg * 2 + kv
                        AQ("pe", lambda e, pg=pg, kv=kv, r_=r_: e.transpose(out=ps[r_ // 4][:, (r_ % 4) * 128:(r_ % 4 + 1) * 128],
                                                                          in_=KVg[:, pg, kv * 128:(kv + 1) * 128], identity=ident_f[:]))
                AQ("dve", lambda e: e.tensor_copy(out=kTs[:, 0:2, :, :].rearrange("p g k s -> p (g k s)"), in_=ps[0][:, :]))
                AQ("act", lambda e: e.activation(out=kTs[:, 2:4, :, :].rearrange("p g k s -> p (g k s)"), in_=ps[1][:, :], func=AF.Copy))
                AQ("dve", lambda e: e.tensor_copy(out=Vb[:, :, :, 0:128], in_=KVg[:, :, 256:512].rearrange("p g (k d) -> p g k d", k=2)))
                for pg in range(4):
                    for kv in range(2):
                        r_ = pg * 2 + kv
                        AQ("pe", lambda e, pg=pg, kv=kv, r_=r_, cb=cb: e.matmul(ps[2][:, r_ * 32:(r_ + 1) * 32].rearrange("p (h q) -> p h q", q=8),
                                                                              lhsT=kTs[:, pg, kv, :], rhs=qT[:, 4 * kv:4 * kv + 4, cb:cb + 8], start=True, stop=True))
                AQ("dve", lambda e: e.tensor_tensor(out=pS[:].rearrange("p (g c) -> p g c", g=4), in0=ps[2][:, 0:256].rearrange("p (g c) -> p g c", g=4),
                                                    in1=cfS[:].rearrange("p h q -> p (h q)").rearrange("p (o c) -> p o c", o=1).to_broadcast([128, 4, 64]), op=ALU.add))
                if st == NPG // 4 - 1:
                    AQ("dve", lambda e: e.tensor_tensor(out=pS[:, 192:256], in0=ps[2][:, 192:256], in1=bSl[:].rearrange("p h q -> p (h q)"), op=ALU.add))
                AQ("act", lambda e: e.activation(out=pS[:], in_=pS[:], func=AF.Exp))
                AQ("dve", lambda e, j0=j0: e.tensor_tensor(out=pmS[:].rearrange("p (g h q) -> p g h q", g=4, q=8), in0=pS[:].rearrange("p (g h q) -> p g h q", g=4, q=8),
                                                           in1=mS[:, j0:j0 + 4, :].rearrange("p g (o q) -> p g o q", o=1).to_broadcast([128, 4, 8, 8]), op=ALU.mult))
                for pg in range(4):
                    for kv in range(2):
                        r_ = pg * 2 + kv
                        AQ("pe", lambda e, pg=pg, kv=kv, r_=r_, st=st: e.matmul(pso(kv), lhsT=pmS[:, r_ * 32:(r_ + 1) * 32], rhs=Vb[:, pg, kv, :],
                                                                              start=(st == 0 and pg == 0), stop=False))
            for kv in range(2):
                AQ("pe", lambda e, kv=kv, cb=cb: e.matmul(ps[2][0:32, kv * 32:(kv + 1) * 32].rearrange("p (h q) -> p h q", q=8), lhsT=kT[:, kv, 1024:1056],
                                                         rhs=qT[:, 4 * kv:4 * kv + 4, cb:cb + 8], start=True, stop=True))
            AQ("dve", lambda e: e.tensor_tensor(out=pS[0:32, 0:64], in0=ps[2][0:32, 0:64], in1=bSn[:].rearrange("p h q -> p (h q)"), op=ALU.add))
            AQ("act", lambda e: e.activation(out=pS[0:32, 0:64], in_=pS[0:32, 0:64], func=AF.Exp))
            AQ("dve", lambda e: e.tensor_tensor(out=pmS[0:32, 0:64].rearrange("p (h q) -> p h q", q=8), in0=pS[0:32, 0:64].rearrange("p (h q) -> p h q", q=8),
                                                in1=mS[0:32, 128:129, :].to_broadcast([32, 8, 8]), op=ALU.mult))
            for kv in range(2):
                AQ("pe", lambda e, kv=kv: e.matmul(pso(kv), lhsT=pmS[0:32, kv * 32:(kv + 1) * 32], rhs=vb1[0:32, 8, kv, :], start=False, stop=True))
            for kv in range(2):
                AQ("dve", lambda e, kv=kv: e.reciprocal(out=rdS[:, kv:kv + 1], in_=pso(kv)[:, 128:129]))
                AQ("dve", lambda e, kv=kv: e.tensor_scalar(out=atS[:, kv, :], in0=pso(kv)[:, 0:128], scalar1=rdS[:, kv:kv + 1], scalar2=None, op0=ALU.mult))
            pvb = ps[0].bitcast(BF16)
            for kv in range(2):
                AQ("pe", lambda e, kv=kv: e.transpose(out=pvb[:, kv * 32:(kv + 1) * 32], in_=atS[:, kv, :], identity=ident_b[0:32, 0:32]))
            AQ("dve", lambda e, cb=cb: e.tensor_copy(out=attT[:, :, cb:cb + 8], in_=pvb[:, 0:64].rearrange("p (h q) -> p h q", q=8)))
        S.op("pool", lambda e: e.memset(stat[:, 6:7], 0.0), r=["att"], w=["mixT"])

    xst = [A.at(OH + 10272 + i * 2048, [128, 512], F32, f"xst{i}") for i in range(4)]
    xctr = [0]

    def outproj():
        for nb in range(4):
            wt = w2g[nb % 2]
            wv = wt[:].rearrange("p g d -> p (g d)").rearrange("p (k c) -> p k c", c=512)
            for kg in range(4):
                st, sk, sg, i = stage_slot()
                S.op("sp", lambda e, st=st, nb=nb, kg=kg: e.dma_start(out=st[:].rearrange("p a b -> p (a b)"),
                                                                      in_=wout[nb, kg].rearrange("p k c -> p (k c)")), w=[sk], dma=sg)
                S.op("act", lambda e, st=st, wv=wv, kg=kg: e.activation(out=wv[:, kg * 4:(kg + 1) * 4, :],
                                                                        in_=st[:].rearrange("p a b -> p (a b)").rearrange("p (k c) -> p k c", c=512),
                                                                        func=AF.Copy), r=[sk], w=[("wo", nb % 2, kg)])
            for t in range(NTT):
                m = 128 if t < NPT else 35
                pd = 6 + t % 2
                xi = xctr[0] % 2
                xctr[0] += 1
                S.op("sp", lambda e, t=t, nb=nb, xi=xi: e.dma_start(out=xst[xi][:], in_=xsp[t, :, nb * 512:(nb + 1) * 512]),
                     r=[("xsp", t, nb)], w=[("xst", xi)], dma=f"xsi{xi}")
                for kc in range(16):
                    src = attT if kc < 8 else ssdT
                    S.op("pe", lambda e, t=t, kc=kc, pd=pd, m=m, src=src, wv=wv: e.matmul(
                        ps[pd][0:m, :], lhsT=src[:, kc % 8, t * 128:t * 128 + m], rhs=wv[:, kc, :], start=(kc == 0), stop=(kc == 15)),
                         r=[("wo", nb % 2, kc // 4), "mixT"], w=[("ps", pd)])
                S.op("dve", lambda e, xi=xi, pd=pd, m=m: e.tensor_tensor(out=xst[xi][0:m, :], in0=ps[pd][0:m, :], in1=xst[xi][0:m, :], op=ALU.add),
                     r=[("ps", pd)], w=[("xst", xi)])
                S.op("sp", lambda e, t=t, nb=nb, xi=xi: e.dma_start(out=xsp[t, :, nb * 512:(nb + 1) * 512], in_=xst[xi][:]),
                     r=[("xst", xi)], w=[("xsp", t, nb)], dma=f"xso{xi}")

    def final_norm():
        S.op("sp", lambda e: e.dma_start(out=gb[:], in_=gvec[3:4, :].partition_broadcast(128)),
             r=["stage0"], w=["gb", "stage0"], dma="gld")
        for t in range(NTT):
            S.op("act", lambda e, t=t: e.activation(out=junk[:], in_=xres[:, t, :], func=AF.Square, accum_out=stat[:, 0:1]),
                 r=[("x", t)], w=["junk", "ss", "stage1"])
            S.op("dve", lambda e: e.tensor_scalar(out=stat[:, 1:2], in0=stat[:, 0:1], scalar1=1.0 / D, scalar2=EPS,
                                                  op0=ALU.mult, op1=ALU.add), r=["ss"], w=["ms"])
            S.op("act", lambda e: e.activation(out=stat[:, 2:3], in_=stat[:, 1:2], func=AF.Sqrt), r=["ms"], w=["sd"])
            S.op("dve", lambda e: e.reciprocal(out=stat[:, 3:4], in_=stat[:, 2:3]), r=["sd"], w=["rstd"])
            S.op("dve", lambda e, t=t: e.scalar_tensor_tensor(out=xres[:, t, :], in0=xres[:, t, :], scalar=stat[:, 3:4], in1=gb[:],
                                                              op0=ALU.mult, op1=ALU.mult),
                 r=[("x", t), "rstd", "gb", "stage0"], w=[("x", t)])
            S.op("sp", lambda e, t=t: e.dma_start(out=y_out[t, :, :], in_=xres[:, t, :]), r=[("x", t)], w=[("y_out", t)], dma="out")

    try:
        load_x(xpre, NPT)
        norm_to_hT(0, NPT)
        ffn(wf1, NPT)
        norm_to_hT(1, NPT)
        bar()
        inproj(False)
        bar()
        S.op("pool", lambda e: e.memset(Hst[:], 0.0), w=["ssd"])
        for t in range(NPT):
            ssd_chunk(t * 128, 128, False)
        S.op("dve", lambda e: e.tensor_scalar(out=Hst[:], in0=Hst[:], scalar1=flag_t[:, 0:1], scalar2=None, op0=ALU.mult), r=[("cst", 0)], w=["ssd"])
        S.op("sp", lambda e: e.dma_start(out=pre_k[:, :, :], in_=kT[:, :, 0:1024]), w=["pre0"], dma="pre")
        S.op("sp", lambda e: e.dma_start(out=pre_v[:, :, :], in_=vb1[:, 0:8, :, :].rearrange("p t k d -> p t (k d)")),
             r=["vb1ones"], w=["pre1"], dma="pre")
        S.op("sp", lambda e: e.dma_start(out=pre_ki[:, :], in_=kiT2[:, 0:1024]), w=["pre2"], dma="pre")
        S.op("sp", lambda e: e.dma_start(out=pre_h[:, :], in_=Hst[:]), r=["ssd"], w=["pre3"], dma="pre")
        S.op("sp", lambda e: e.dma_start(out=pre_halo[:, :], in_=halo[:].rearrange("p a b -> p (a b)")), r=["halo"], w=["pre4"], dma="pre")
        S.dma_batch_end("pre")
        bar()
        load_x(xin, NTT)
        norm_to_hT(0, NTT)
        ffn(wf1, NTT)
        norm_to_hT(1, NTT)
        for t in range(NTT):
            S.op("sp", lambda e, t=t: e.dma_start(out=xsp[t, :, :], in_=xres[:, t, :]), r=[("x", t)], w=[("xsp", t, nb) for nb in range(4)], dma="xsp")
        bar()
        inproj(True)
        bar()
        S.op("sp", lambda e: e.dma_start(out=kT_pre[:], in_=pre_k[:, :, :]), w=["kT_pre"], dma="pre")
        S.op("sp", lambda e: e.dma_start(out=vb1_pre[:].rearrange("p t k d -> p t (k d)"), in_=pre_v[:, :, :]),
             w=["vb1_pre"], dma="pre")
        S.op("sp", lambda e: e.dma_start(out=kiT2_pre[:], in_=pre_ki[:, :]), w=["kiT2_pre"], dma="pre")
        S.op("sp", lambda e: e.dma_start(out=Hst[:], in_=pre_h[:, :]), w=["ssd"], dma="pre")
        S.dma_batch_end("pre")
        S.op("act", lambda e: e.activation(out=Hb[:], in_=Hst[:], func=AF.Copy), w=["ssd"])
        for t in range(NPT):
            ssd_chunk(t * 128, 128, True)
        S.op("sp", lambda e: e.dma_start(out=ssm_out[0, :, :], in_=Hst[:]), r=["ssd"], w=[("ssm_out", 0)], dma="hs")
        for b in range(4):
            S.op("sp", lambda e, b=b: e.dma_start(out=Hst[:], in_=sssmT[b]), r=[("ssm_out", b)], w=["ssd"], dma="hs")
            S.op("act", lambda e: e.activation(out=Hb[:], in_=Hst[:], func=AF.Copy), w=["ssd"])
            ssd_chunk(1024 + 8 * b, 8, True)
            S.op("sp", lambda e, b=b: e.dma_start(out=ssm_out[1 + b, :, :], in_=Hst[:]), r=["ssd"], w=[("ssm_out", 1 + b)], dma="hs")
        bar()
        S.op("pool", lambda e: e.memset(attT[:], 0.0), w=["mixT", "att"])
        att_setup()
        for qt in range(NPT):
            att_prompt_tile(qt)
        if "S" not in KSKIP:
            att_sample()
        S.op("pool", lambda e: e.memset(ssdT[:, :, 1056:NTOK], 0.0), r=["ssd"], w=["mixT", "ssd"])
        bar()
        outproj()
        bar()
        load_x(xsp, NTT)
        if "h" not in KSKIP:
            norm_to_hT(2, NTT)
        if "f" not in KSKIP:
            ffn(wf2, NTT)
        if "n" not in KSKIP:
            final_norm()
    except _Stop:
        pass
    fin = ["out"] + [f"tok{i}" for i in range(2)] + ["tl0"] + ["hs"]
    S.emit(final_dma_groups=fin)
    return nc


def _up_layout(W):
    K, N = W.shape
    assert K == D and N % 128 == 0
    return np.ascontiguousarray(W.reshape(16, 128, N // 128, 128).transpose(2, 1, 0, 3))


def _t5_onehot():
    n = np.arange(256)
    nf = np.maximum(n, 1).astype(np.float32)
    large = 16 + (np.log(nf / np.float32(16)) / np.float32(np.log(128 / 16)) * np.float32(16)).astype(np.int32)
    bucket = np.where(n < 16, n, np.minimum(large, 31))
    oh = np.zeros((32, 256), np.float32)
    oh[bucket, n] = 1.0
    return oh


_IN_OFF = dict(q=(0, 1024), k=(1024, 256), v=(1280, 256), qi=(1536, 1024), ki=(2560, 64), wi=(2624, 16),
               z=(2640, 1024), xbc=(3664, 1536), dt=(5200, 16))


def kernel(x_prompt, x_sample, cache_k, cache_v, cache_kidx, state_conv, state_ssm, page_table, rel_bias,
           g_ffn1, w1_ffn1, w3_ffn1, w2_ffn1, g_mix, w_in, conv_w, conv_b, a_log, dt_bias, d_skip, g_ssd,
           w_out, g_ffn2, w1_ffn2, w3_ffn2, w2_ffn2, g_final):
    f = lambda a: np.asarray(a, dtype=np.float32)
    xp, xs_ = f(x_prompt), f(x_sample)
    win_full = f(w_in)[0]
    cols = []
    for nm in ("q", "k", "v", "qi", "z", "xbc", "ki", "wi", "dt"):
        o, n = _IN_OFF[nm]
        cols.append(win_full[:, o:o + n])
    cols.append(np.zeros((D, 32), np.float32))
    ko, kn = _IN_OFF["ki"]
    cols += [win_full[:, ko:ko + kn], win_full[:, ko:ko + kn]]
    do, dn = _IN_OFF["dt"]
    cols += [win_full[:, do:do + dn], np.zeros((D, 112), np.float32)]
    win_r = _up_layout(np.concatenate(cols, axis=1))
    shared = dict(
        gvec=np.stack([f(g_ffn1)[0], f(g_mix)[0], f(g_ffn2)[0], f(g_final)]),
        w1a=_up_layout(f(w1_ffn1)[0]), w3a=_up_layout(f(w3_ffn1)[0]), w2a=np.ascontiguousarray(f(w2_ffn1)[0].reshape(NFF, 128, D)),
        w1b=_up_layout(f(w1_ffn2)[0]), w3b=_up_layout(f(w3_ffn2)[0]), w2b=np.ascontiguousarray(f(w2_ffn2)[0].reshape(NFF, 128, D)),
        win=win_r,
        wout=np.ascontiguousarray(f(w_out)[0].reshape(4, 4, 128, 4, 512).transpose(3, 0, 2, 1, 4)),
        cwT=np.ascontiguousarray(f(conv_w)[0].reshape(4, 12, 128).transpose(2, 1, 0)),
        cbT=np.ascontiguousarray(f(conv_b)[0].reshape(12, 128).T),
        dcol=np.ascontiguousarray(np.repeat(f(d_skip)[0], 64).reshape(8, 128).T),
        gssd=np.ascontiguousarray(f(g_ssd)[0].reshape(8, 128).T),
        alog=f(a_log), dtb=f(dt_bias), relb=f(rel_bias), oh_d=_t5_onehot(),
    )
    ck = np.ascontiguousarray(f(cache_k)[0].reshape(5120 * 128, 256))
    cv = np.ascontiguousarray(f(cache_v)[0].reshape(5120 * 128, 256))
    cki = np.ascontiguousarray(f(cache_kidx)[0].reshape(5120 * 128, 64))
    ptab = np.asarray(page_table, dtype=np.int32)
    tt_ = np.arange(32)
    bm4 = (tt_[:, None] // 8 == np.arange(4)[None]).astype(np.float32)
    dq8 = (tt_[:, None] % 8 == np.arange(8)[None]).astype(np.float32)
    pen32 = np.where((tt_[:, None, None] // 8 == np.arange(4)[None, :, None]) & (tt_[:, None, None] % 8 <= np.arange(8)[None, None, :]), 0.0, -1e30).astype(np.float32)
    shared.update(kidx_tab=cki, kv_tab=np.concatenate([ck, cv], axis=1), bm4=bm4, dq8=dq8, pen32=pen32)
    sconv_all = f(state_conv)[0]
    sssm_all = f(state_ssm)[0]
    in_maps = []
    for c in range(8):
        b, half = c // 2, c % 2
        xin = np.zeros((NTT, 128, D), np.float32)
        xin[:NPT] = xp[b, half * 1024:(half + 1) * 1024].reshape(NPT, 128, D)
        xin[8, 0:32] = xs_[4 * c:4 * c + 4].reshape(32, D)
        if half == 1:
            xin[8, 32:35] = xp[b, 1021:1024]
        m = dict(shared)
        m["xin"] = xin
        m["pt4"] = np.ascontiguousarray(ptab[4 * c:4 * c + 4])
        m["xpre"] = np.ascontiguousarray(xp[b, 0:1024].reshape(NPT, 128, D)) if half == 1 else np.zeros((NPT, 128, D), np.float32)
        fl = np.zeros((128, 2), np.float32)
        fl[:, 0] = float(half)
        fl[:, 1] = (float(half) - 1.0) * 1e30
        m["flagc"] = fl
        m["sconvT"] = np.ascontiguousarray(sconv_all[4 * c:4 * c + 4].reshape(4, 3, 12, 128).transpose(3, 2, 0, 1))
        m["sssmT"] = np.ascontiguousarray(sssm_all[4 * c:4 * c + 4].reshape(4, 1024, 128).transpose(0, 2, 1))
        in_maps.append(m)
    nc = build()
    res = run_bass_kernel_spmd(nc, in_maps, core_ids=list(range(8))).results
    _DBG["res"] = res

    y_prompt = np.zeros((4, 2048, D), np.float32)
    y_sample = np.zeros((32, 8, D), np.float32)
    k_prompt = np.zeros((1, 4, 2048, 2, 128), np.float32)
    v_prompt = np.zeros_like(k_prompt)
    kidx_prompt = np.zeros((1, 4, 2048, 64), np.float32)
    conv_prompt = np.zeros((1, 4, 3, 1536), np.float32)
    ssm_prompt = np.zeros((1, 4, 16, 64, 128), np.float32)
    k_sample = np.zeros((1, 32, 8, 2, 128), np.float32)
    v_sample = np.zeros_like(k_sample)
    kidx_sample = np.zeros((1, 32, 8, 64), np.float32)
    conv_sample = np.zeros((1, 32, 3, 1536), np.float32)
    ssm_sample = np.zeros((1, 32, 16, 64, 128), np.float32)
    for c in range(8):
        b, half = c // 2, c % 2
        r = res[c]
        sl = slice(half * 1024, (half + 1) * 1024)
        yo = r["y_out"]
        y_prompt[b, sl] = yo[:NPT].reshape(1024, D)
        y_sample[4 * c:4 * c + 4] = yo[8, 0:32].reshape(4, 8, D)
        kv = r["kvs_out"]
        kvp = kv[:NPT].reshape(1024, 640)
        k_prompt[0, b, sl] = kvp[:, 0:256].reshape(1024, 2, 128)
        v_prompt[0, b, sl] = kvp[:, 256:512].reshape(1024, 2, 128)
        kidx_prompt[0, b, sl] = kvp[:, 512:576]
        kvs_ = kv[8, 0:32]
        k_sample[0, 4 * c:4 * c + 4] = kvs_[:, 0:256].reshape(4, 8, 2, 128)
        v_sample[0, 4 * c:4 * c + 4] = kvs_[:, 256:512].reshape(4, 8, 2, 128)
        kidx_sample[0, 4 * c:4 * c + 4] = kvs_[:, 512:576].reshape(4, 8, 64)
        so = r["ssm_out"].reshape(5, 128, 16, 64).transpose(0, 2, 3, 1)
        if half == 1:
            ssm_prompt[0, b] = so[0]
        ssm_sample[0, 4 * c:4 * c + 4] = so[1:5]
        co = r["conv_out"]
        if half == 1:
            conv_prompt[0, b] = co[0]
        conv_sample[0, 4 * c:4 * c + 4] = co[1:5]
    return (y_prompt, y_sample, k_prompt, v_prompt, kidx_prompt, conv_prompt, ssm_prompt,
            k_sample, v_sample, kidx_sample, conv_sample, ssm_sample)
```

```python
import numpy as np
import concourse.bass as bass
import concourse.mybir as mybir
from concourse.bass_utils import run_bass_kernel_spmd
from contextlib import ExitStack

F32 = mybir.dt.float32
BF16 = mybir.dt.bfloat16
I32 = mybir.dt.int32
AF = mybir.ActivationFunctionType
ALU = mybir.AluOpType
AX = mybir.AxisListType

D = 2048
DFF = 5632
NFF = DFF // 128
NPT = 8
NTT = 9
NTOK = 1024 + 35
TB = [(0, 512), (512, 512), (1024, 35)]
EPS = 1e-6
G = 4
NIN = 43


_DBG = {}


class Sched:
    ENGS = ("pe", "act", "dve", "pool", "sp")

    def __init__(self, nc):
        self.nc = nc
        self.ops = []
        self.last_w = {}
        self.readers = {}
        self.dma_cnt = {}
        self.last_eng = {}
        self.last_dma = {}
        self.last_bar = None
        self.batch_ends = {}

    def dma_batch_end(self, group):
        self.batch_ends.setdefault(group, []).append(self.dma_cnt.get(group, 0))

    def barrier(self, fn):
        deps = set(self.last_eng.values()) | set(self.last_dma.values())
        i = self.op("pool", fn, _extra=deps)
        self.last_bar = i
        return i

    def op(self, eng, fn, r=(), w=(), dma=None, _extra=()):
        i = len(self.ops)
        deps = set(_extra)
        if self.last_bar is not None:
            deps.add(self.last_bar)
        for k in list(r) + list(w):
            if k in self.last_w:
                deps.add(self.last_w[k])
        for k in w:
            lastc = {}
            for j in self.readers.get(k, ()):
                oj = self.ops[j]
                if oj["dma"] is None:
                    lastc[oj["eng"]] = max(lastc.get(oj["eng"], -1), j)
                else:
                    deps.add(j)
            deps.update(lastc.values())
        deps.discard(i)
        o = dict(eng=eng, fn=fn, deps=deps, dma=dma, sig=False, i=i)
        if dma is not None:
            self.dma_cnt[dma] = self.dma_cnt.get(dma, 0) + 1
            o["dcount"] = self.dma_cnt[dma]
            self.last_dma[dma] = i
        else:
            self.last_eng[eng] = i
        self.ops.append(o)
        for k in w:
            self.last_w[k] = i
            self.readers[k] = []
        for k in r:
            self.readers.setdefault(k, []).append(i)
        return i

    def emit(self, final_dma_groups=()):
        nc = self.nc
        ops = self.ops
        for o in ops:
            for d in o["deps"]:
                od = ops[d]
                if od["dma"] is None and od["eng"] == "pe" and o["eng"] == "pe" and o["dma"] is None:
                    continue
                od["sig"] = True
        cnt = {e: 0 for e in self.ENGS}
        for o in ops:
            if o["dma"] is None and o["sig"]:
                cnt[o["eng"]] += 1
                o["sval"] = cnt[o["eng"]]
        groups = sorted(self.dma_cnt.keys())
        with ExitStack() as es:
            esem = {e: es.enter_context(nc.semaphore("s_" + e)) for e in self.ENGS}
            dsem = {g: es.enter_context(nc.semaphore("d_" + str(g))) for g in groups}
            block = es.enter_context(nc.Block())
            per_eng = {e: [o for o in ops if o["eng"] == e] for e in self.ENGS}

            def run(engobj, ename):
                waited = {}
                for o in per_eng[ename]:
                    need = {}
                    for d in o["deps"]:
                        od = ops[d]
                        if od["dma"] is not None:
                            key = ("d", od["dma"])
                            dc = od["dcount"]
                            ends = [b for b in self.batch_ends.get(od["dma"], ()) if b >= dc]
                            val = 16 * (min(ends) if ends else dc)
                        else:
                            if od["eng"] == "pe" and ename == "pe" and o["dma"] is None:
                                continue
                            key = ("e", od["eng"])
                            val = od["sval"]
                        if need.get(key, 0) < val:
                            need[key] = val
                    for key, val in need.items():
                        if waited.get(key, 0) >= val:
                            continue
                        waited[key] = val
                        sem = dsem[key[1]] if key[0] == "d" else esem[key[1]]
                        engobj.wait_ge(sem, val)
                    ins = o["fn"](engobj)
                    if o["dma"] is not None:
                        ins.then_inc(dsem[o["dma"]], 16)
                    elif o["sig"]:
                        ins.then_inc(esem[ename], 1)
                if ename == "sp":
                    for g in final_dma_groups:
                        if g in dsem:
                            engobj.wait_ge(dsem[g], 16 * self.dma_cnt[g])

            block.tensor(lambda e: run(e, "pe"))
            block.scalar(lambda e: run(e, "act"))
            block.vector(lambda e: run(e, "dve"))
            block.gpsimd(lambda e: run(e, "pool"))
            block.sync(lambda e: run(e, "sp"))


class Arena:
    def __init__(self, nc, base=16512, top=229344):
        self.nc = nc
        self.base = base
        self.top = top
        self.n = 0

    def at(self, off, shape, dtype, name=None):
        self.n += 1
        nbytes = int(np.prod(shape[1:])) * (2 if dtype == BF16 else 4)
        assert self.base + off + nbytes <= self.top, (name, off, nbytes)
        return self.nc.alloc_sbuf_tensor_at(name or f"t{self.n}", list(shape), dtype, offset=self.base + off)


def build():
    nc = bass.Bass("TRN2", target_bir_lowering=False)
    S = Sched(nc)
    A = Arena(nc)

    def din(name, shape, dt=F32):
        return nc.dram_tensor(name, list(shape), dt, kind="ExternalInput").ap()

    def dout(name, shape, dt=F32):
        return nc.dram_tensor(name, list(shape), dt, kind="ExternalOutput").ap()

    xin = din("xin", [NTT, 128, D])
    xpre = din("xpre", [NPT, 128, D])
    flagc = din("flagc", [128, 2])
    gvec = din("gvec", [4, D])
    wf1 = [din("w1a", [NFF, 128, 16, 128]), din("w3a", [NFF, 128, 16, 128]), din("w2a", [NFF, 128, D])]
    wf2 = [din("w1b", [NFF, 128, 16, 128]), din("w3b", [NFF, 128, 16, 128]), din("w2b", [NFF, 128, D])]
    win = din("win", [NIN, 128, 16, 128])
    wout = din("wout", [4, 4, 128, 4, 512])
    sconvT = din("sconvT", [128, 12, 4, 3])
    sssmT = din("sssmT", [4, 128, 1024])
    cwT = din("cwT", [128, 12, 4])
    cbT = din("cbT", [128, 12])
    dcol = din("dcol", [128, 8])
    gssd = din("gssd", [128, 8])
    alog = din("alog", [1, 16])
    relb = din("relb", [32, 8])
    kidx_tab = din("kidx_tab", [5120 * 128, 64])
    kv_tab = din("kv_tab", [5120 * 128, 512])
    pt4 = din("pt4", [4, 128], I32)
    bm4 = din("bm4", [32, 4])
    dq8 = din("dq8", [32, 8])
    pen32 = din("pen32", [32, 4, 8])
    oh_d = din("oh_d", [32, 256])
    dtb = din("dtb", [1, 16])
    y_out = dout("y_out", [NTT, 128, D])
    kvs_out = dout("kvs_out", [NTT, 128, 640])
    conv_out = dout("conv_out", [5, 3, 1536])
    ssm_out = dout("ssm_out", [5, 128, 1024])
    xsp = nc.dram_tensor("xsp", [NTT, 128, D], F32).ap()
    pre_k = nc.dram_tensor("pre_k", [128, 2, 1024], BF16).ap()
    pre_v = nc.dram_tensor("pre_v", [128, 8, 258], BF16).ap()
    pre_ki = nc.dram_tensor("pre_ki", [128, 1024], BF16).ap()
    pre_h = nc.dram_tensor("pre_h", [128, 1024], F32).ap()
    pre_halo = nc.dram_tensor("pre_halo", [128, 36], F32).ap()
    bsc_t = nc.dram_tensor("bsc", [8, 128, 256], F32)
    bsc = bsc_t.ap()

    OX = 0
    OH = 73728
    OW = OH + 16 * NTOK * 2
    O_STAGE = OW
    O_WUP = O_STAGE + 3 * 8192
    O_W2G = O_WUP + 4 * 4096
    O_GT = O_W2G + 2 * G * D * 2
    GTB = G * NTOK * 2 + 8
    O_SIL = O_GT + 2 * GTB
    O_MISC = O_SIL + 2 * 2048
    xres = A.at(OX, [128, NTT, D], F32, "xres")
    hT = A.at(OH, [128, 16, NTOK], BF16, "hT")
    stage = [A.at(O_STAGE + i * 8192, [128, 16, 128], F32, f"stage{i}") for i in range(3)]
    wup = [A.at(O_WUP + i * 4096, [128, 16, 128], BF16, f"wup{i}") for i in range(4)]
    w2g = [A.at(O_W2G + i * G * D * 2, [128, G, D], BF16, f"w2g{i}") for i in range(2)]
    gT = [A.at(O_GT + i * GTB, [128, G, NTOK], BF16, f"gT{i}") for i in range(2)]
    sil = [A.at(O_SIL + i * 2048, [128, 512], F32, f"sil{i}") for i in range(2)]
    al = lambda v: (v + 31) // 32 * 32
    o = O_MISC
    ident_b = A.at(o, [128, 128], BF16, "ident_b"); o = al(o + 256)
    ident_f = A.at(o, [128, 128], F32, "ident_f"); o = al(o + 512)
    tri_f = A.at(o, [128, 128], F32, "tri_f"); o = al(o + 512)
    ones_f = A.at(o, [128, 128], F32, "ones_f"); o = al(o + 512)
    ones_b = A.at(o, [128, 128], BF16, "ones_b"); o = al(o + 256)
    stat = A.at(o, [128, 8], F32, "stat"); o = al(o + 32)
    flag_t = A.at(o, [128, 2], F32, "flag_t"); o = al(o + 8)
    cw_t = A.at(o, [128, 12, 4], F32, "cw_t"); o = al(o + 192)
    cb_t = A.at(o, [128, 12], F32, "cb_t"); o = al(o + 48)
    dcol_t = A.at(o, [128, 8], F32, "dcol_t"); o = al(o + 32)
    gssd_t = A.at(o, [128, 8], F32, "gssd_t"); o = al(o + 32)
    dtb_t = A.at(o, [128, 16], F32, "dtb_t"); o = al(o + 64)
    a_t = A.at(o, [128, 16], F32, "a_t"); o = al(o + 64)
    halo = A.at(o, [128, 12, 3], F32, "halo"); o = al(o + 144)
    sct = A.at(o, [128, 12, 4, 3], F32, "sct"); o = al(o + 576)
    tokst = [A.at(O_MISC - 2048 + i * 512, [128, 128], F32, f"tokst{i}") for i in range(2)]
    tails = [A.at(o + i * 2560, [3, 5, 128], F32, f"tails{i}") for i in range(1)]; o = al(o + 2560)
    gb = A.at(O_STAGE, [128, D], F32, "gb")
    xs = A.at(O_STAGE + 8192, [128, D], BF16, "xs")
    junk = A.at(O_STAGE + 8192 + 4096, [128, D], BF16, "junk")
    NTP = 1072
    NB = NTP * 2
    qT = A.at(OX, [128, 8, NTP], BF16, "qT")
    qiT = A.at(OX + 8 * NB, [128, 8, NTP], BF16, "qiT")
    szT = A.at(OX + 16 * NB, [128, 8, NTP], BF16, "szT")
    ssdT = szT
    attT = A.at(OX + 24 * NB, [128, 8, NTP], BF16, "attT")
    kT = A.at(OX + 32 * NB, [128, 2, NTP], BF16, "kT")
    o = O_W2G
    xcT = A.at(o, [128, 12, NTP], BF16, "xcT"); o = al(o + 12 * NB)
    vb1 = A.at(o, [128, NTT, 2, 129], BF16, "vb1"); o = al(o + NTT * 2 * 129 * 2 + 4)
    sm_tok = A.at(o, [128, NTT, 128], F32, "sm_tok"); o = al(o + NTT * 512)
    kiT2 = A.at(o, [128, NTOK + 1], BF16, "kiT2"); o = al(o + NB + 2)
    dtT = A.at(o, [16, NTOK], F32, "dtT"); o = al(o + NTOK * 4)
    rawx = A.at(o, [128, NTOK + 3], F32, "rawx"); o = al(o + (NTOK + 3) * 4)
    cacc = A.at(o, [128, 1024], F32, "cacc"); o = al(o + 4096)
    exts = A.at(o, [128, 4, 11], F32, "exts"); o = al(o + 176)
    assert o <= O_MISC - 2048, o - O_MISC
    o = OH
    kT_pre = A.at(o, [128, 2, 1024], BF16, "kT_pre"); o = al(o + 4096)
    vb1_pre = A.at(o, [128, 8, 2, 129], BF16, "vb1_pre"); o = al(o + 4128)
    kiT2_pre = A.at(o, [128, 1024], BF16, "kiT2_pre"); o = al(o + 2048)
    o = al(o + 16384)
    Hst = A.at(o, [128, 1024], F32, "Hst"); o = al(o + 4096)
    Hb = A.at(o, [128, 1024], BF16, "Hb"); o = al(o + 2048)
    assert o <= OW
    o = O_STAGE
    ysb = A.at(o, [128, 1024], F32, "ysb"); o = al(o + 4096)
    xd = A.at(o, [128, 1024], BF16, "xd"); o = al(o + 2048)
    xdd = A.at(o, [128, 1024], BF16, "xdd"); o = al(o + 2048)
    Btok = A.at(o, [128, 256], BF16, "Btok"); o = al(o + 512)
    Dm = A.at(o, [128, 8, 128], F32, "Dm"); o = al(o + 4096)
    CBm = A.at(o, [128, 2, 128], F32, "CBm"); o = al(o + 1024)
    t1 = A.at(o, [128, 128], F32, "t1"); o = al(o + 512)
    t2 = A.at(o, [128, 128], F32, "t2"); o = al(o + 512)
    Mh = A.at(o, [128, 128], BF16, "Mh"); o = al(o + 256)
    sm16 = [A.at(o + i * 64, [128, 16], F32, f"sm16_{i}") for i in range(8)]; o = al(o + 512)
    gbuf = A.at(o, [128, 8, 128], F32, "gbuf"); o = al(o + 4096)
    sq = A.at(o, [128, 8, 128], BF16, "sq"); o = al(o + 2048)
    rs = A.at(o, [128, 2, 128], F32, "rs"); o = al(o + 1024)
    assert o <= O_W2G

    psall = nc.alloc_psum_tensor("psall", [128, 4096], F32)
    ps = [psall[:, i * 512:(i + 1) * 512] for i in range(8)]

    import os
    KSTOP = int(os.environ.get("KSTOP", "99"))
    KSKIP = os.environ.get("KSKIP", "")
    nbar = [0]

    class _Stop(Exception):
        pass

    def bar():
        S.barrier(lambda e: e.memset(stat[:, 7:8], 0.0))
        nbar[0] += 1
        if nbar[0] >= KSTOP:
            raise _Stop()

    for t, nm in ((ident_b, "ident_b"), (ident_f, "ident_f")):
        S.op("pool", lambda e, t=t: e.memset(t[:], 1.0), w=[nm])
        S.op("pool", lambda e, t=t: e.affine_select(out=t[:], in_=t[:], pattern=[[-1, 128]], compare_op=ALU.is_equal,
                                                     fill=0.0, base=0, channel_multiplier=1), r=[nm], w=[nm])
    S.op("pool", lambda e: e.memset(tri_f[:], 1.0), w=["tri_f"])
    S.op("pool", lambda e: e.affine_select(out=tri_f[:], in_=tri_f[:], pattern=[[1, 128]], compare_op=ALU.is_ge,
                                           fill=0.0, base=0, channel_multiplier=-1), r=["tri_f"], w=["tri_f"])
    S.op("pool", lambda e: e.memset(ones_f[:], 1.0), w=["ones_f"])
    S.op("pool", lambda e: e.memset(ones_b[:], 1.0), w=["ones_b"])
    for i, (dst, src) in enumerate(((flag_t[:], flagc[:, :]), (cw_t[:], cwT[:, :, :]), (cb_t[:], cbT[:, :]), (dcol_t[:], dcol[:, :]),
                                    (gssd_t[:], gssd[:, :]), (sct[:], sconvT[:, :, :, :]),
                                    (dtb_t[:], dtb[0:1, :].partition_broadcast(128)), (a_t[:], alog[0:1, :].partition_broadcast(128)))):
        S.op("sp", lambda e, dst=dst, src=src: e.dma_start(out=dst, in_=src), w=[("cst", i)], dma="cst")
    S.dma_batch_end("cst")
    S.op("act", lambda e: e.activation(out=a_t[:], in_=a_t[:], func=AF.Exp), r=[("cst", 7)], w=[("cst", 7)])
    S.op("dve", lambda e: e.tensor_scalar(out=a_t[:], in0=a_t[:], scalar1=-1.0, scalar2=None, op0=ALU.mult), r=[("cst", 7)], w=[("cst", 7)])

    def load_x(src, ntiles):
        for t in range(ntiles):
            S.op("sp", lambda e, t=t: e.dma_start(out=xres[:, t, :], in_=src[t, :, :]), w=[("x", t)], dma="xld")
        S.dma_batch_end("xld")

    def norm_to_hT(gi, ntiles):
        S.op("sp", lambda e: e.dma_start(out=gb[:], in_=gvec[gi:gi + 1, :].partition_broadcast(128)),
             r=["stage0"], w=["gb", "stage0"], dma="gld")
        for t in range(ntiles):
            ncol = 128 if t < NPT else 35
            S.op("act", lambda e, t=t: e.activation(out=junk[:], in_=xres[:, t, :], func=AF.Square, accum_out=stat[:, 0:1]),
                 r=[("x", t)], w=["junk", "ss", "stage1"])
            S.op("dve", lambda e: e.tensor_scalar(out=stat[:, 1:2], in0=stat[:, 0:1], scalar1=1.0 / D, scalar2=EPS,
                                                  op0=ALU.mult, op1=ALU.add), r=["ss"], w=["ms"])
            S.op("act", lambda e: e.activation(out=stat[:, 2:3], in_=stat[:, 1:2], func=AF.Sqrt), r=["ms"], w=["sd"])
            S.op("dve", lambda e: e.reciprocal(out=stat[:, 3:4], in_=stat[:, 2:3]), r=["sd"], w=["rstd"])
            S.op("dve", lambda e, t=t: e.scalar_tensor_tensor(out=xs[:], in0=xres[:, t, :], scalar=stat[:, 3:4], in1=gb[:],
                                                              op0=ALU.mult, op1=ALU.mult),
                 r=[("x", t), "rstd", "gb", "stage0"], w=["xs", "stage1"])
            for half in range(2):
                pb = ps[2 * (t % 2) + half]
                pbv = pb.bitcast(BF16)
                pk = ("ps", 2 * (t % 2) + half)
                for j in range(8):
                    kc = half * 8 + j
                    S.op("pe", lambda e, pbv=pbv, j=j, kc=kc: e.transpose(out=pbv[:, j * 128:(j + 1) * 128],
                                                                          in_=xs[:, kc * 128:(kc + 1) * 128], identity=ident_b[:]),
                         r=["xs", "stage1", "ident_b"], w=[pk])
                tbk = ("hT", 0 if t < 4 else (1 if t < 8 else 2))
                c0 = t * 128
                if half == 0:
                    S.op("act", lambda e, pbv=pbv, half=half, c0=c0, ncol=ncol: e.activation(
                        out=hT[:, half * 8:(half + 1) * 8, c0:c0 + ncol],
                        in_=pbv.rearrange("p (j c) -> p j c", j=8)[:, :, 0:ncol], func=AF.Copy), r=[pk], w=[tbk])
                else:
                    S.op("dve", lambda e, pbv=pbv, half=half, c0=c0, ncol=ncol: e.tensor_copy(
                        out=hT[:, half * 8:(half + 1) * 8, c0:c0 + ncol],
                        in_=pbv.rearrange("p (j c) -> p j c", j=8)[:, :, 0:ncol]), r=[pk], w=[tbk])

    wctr = [0]
    suse = [0, 0, 0]

    def stage_slot():
        i = wctr[0]
        wctr[0] += 1
        k = i % 3
        suse[k] += 1
        return stage[k], f"stage{k}", f"stage{k}_{suse[k] // 100}", i

    def load_up_tile(src_ap, cast_eng):
        st, sk, sg, i = stage_slot()
        wb = wup[i % 4]
        wk = f"wup{i % 4}"
        S.op("sp", lambda e: e.dma_start(out=st[:], in_=src_ap), w=[sk], dma=sg)
        if cast_eng == "act":
            S.op("act", lambda e: e.activation(out=wb[:], in_=st[:], func=AF.Copy), r=[sk], w=[wk])
        else:
            S.op(cast_eng, lambda e: e.tensor_copy(out=wb[:], in_=st[:]), r=[sk], w=[wk])
        return wb, wk

    def up_mm(wb, wk, tb, pbank, pkey):
        c0, n = TB[tb]
        for kc in range(16):
            S.op("pe", lambda e, kc=kc: e.matmul(pbank[:, 0:n], lhsT=wb[:, kc, :], rhs=hT[:, kc, c0:c0 + n],
                                                 start=(kc == 0), stop=(kc == 15)),
                 r=[wk, ("hT", tb)], w=[pkey])

    def ffn(w, ntiles):
        w1, w3, w2 = w
        ntb = 3 if ntiles == NTT else 2
        pctr = 0
        for grp in range(NFF // G):
            gt = gT[grp % 2]
            w2t = w2g[grp % 2]
            for j in range(G):
                fc = grp * G + j
                wb1, wk1 = load_up_tile(w1[fc], "pool")
                wb3, wk3 = load_up_tile(w3[fc], "dve")
                st, sk, sg, i = stage_slot()
                S.op("sp", lambda e, st=st, fc=fc: e.dma_start(out=st[:].rearrange("p a b -> p (a b)"), in_=w2[fc]),
                     w=[sk], dma=sg)
                S.op("act", lambda e, st=st, w2t=w2t, j=j: e.activation(out=w2t[:, j, :], in_=st[:].rearrange("p a b -> p (a b)"),
                                                                        func=AF.Copy), r=[sk], w=[("w2g", grp % 2, j)])
                for tb in range(ntb):
                    c0, n = TB[tb]
                    pa, pbk = 2 * (pctr % 3), 2 * (pctr % 3) + 1
                    pctr += 1
                    up_mm(wb1, wk1, tb, ps[pa], ("ps", pa))
                    up_mm(wb3, wk3, tb, ps[pbk], ("ps", pbk))
                    sl = sil[pctr % 2]
                    slk = ("sil", pctr % 2)
                    S.op("act", lambda e, sl=sl, pa=pa, n=n: e.activation(out=sl[:, 0:n], in_=ps[pa][:, 0:n], func=AF.Silu),
                         r=[("ps", pa)], w=[slk])
                    S.op("dve", lambda e, sl=sl, pbk=pbk, n=n, c0=c0, gt=gt, j=j: e.tensor_tensor(
                        out=gt[:, j, c0:c0 + n], in0=sl[:, 0:n], in1=ps[pbk][:, 0:n], op=ALU.mult),
                         r=[slk, ("ps", pbk)], w=[("gT", grp % 2, j, tb)])
            for t in range(ntiles):
                m = 128 if t < NPT else 35
                tb = 0 if t < 4 else (1 if t < 8 else 2)
                for nb in range(4):
                    pd = 6 + (t * 4 + nb) % 2
                    for j in range(G):
                        S.op("pe", lambda e, t=t, j=j, nb=nb, pd=pd, m=m, gt=gt, w2t=w2t: e.matmul(
                            ps[pd][0:m, :], lhsT=gt[:, j, t * 128:t * 128 + m], rhs=w2t[:, j, nb * 512:(nb + 1) * 512],
                            start=(j == 0), stop=(j == G - 1)),
                             r=[("gT", grp % 2, j, tb), ("w2g", grp % 2, j)], w=[("ps", pd)])
                    S.op("dve", lambda e, t=t, nb=nb, pd=pd, m=m: e.scalar_tensor_tensor(
                        out=xres[0:m, t, nb * 512:(nb + 1) * 512], in0=ps[pd][0:m, :], scalar=0.5,
                        in1=xres[0:m, t, nb * 512:(nb + 1) * 512], op0=ALU.mult, op1=ALU.add),
                         r=[("ps", pd), ("x", t)], w=[("x", t)])

    QSCALE = 128.0 ** -0.5

    def inproj(main):
        ntiles = NTT if main else NPT
        ntb = 3 if main else 2
        chunks = list(range(NIN)) if main else [8, 9, 10, 11] + list(range(28, 40)) + [41, 42]
        if main and "q" in KSKIP:
            chunks = [c for c in chunks if not (c < 8 or 12 <= c < 28)]
        pctr = 0
        tctr = [0]
        if main:
            S.op("sp", lambda e: e.dma_start(out=halo[:], in_=pre_halo.rearrange("p (a b) -> p a b", b=3)),
                 w=["halo"], dma="pre")
            S.dma_batch_end("pre")
        else:
            S.op("pool", lambda e: e.memset(halo[:], 0.0), w=["halo"])
        S.op("pool", lambda e: e.memset(vb1[:, :, :, 128:129], 1.0), w=["vb1ones"])
        for c in chunks:
            wb, wk = load_up_tile(win[c], "pool" if c % 2 == 0 else "dve")
            is_q, is_k, is_v = c < 8, 8 <= c < 10, 10 <= c < 12
            is_qi, is_z, is_xbc = 12 <= c < 20, 20 <= c < 28, 28 <= c < 40
            is_sm, is_ki2, is_aux = c == 40, c == 41, c == 42
            if not is_v and not is_sm:
                for tb in range(ntb):
                    c0, n = TB[tb]
                    pa = pctr % 6
                    pctr += 1
                    up_mm(wb, wk, tb, ps[pa], ("ps", pa))
                    src = ps[pa][:, 0:n]
                    if is_q:
                        S.op("act", lambda e, src=src, c=c, c0=c0, n=n: e.activation(out=qT[:, c, c0:c0 + n], in_=src, func=AF.Copy, scale=QSCALE),
                             r=[("ps", pa)], w=[("qT", c, tb)])
                    elif is_k:
                        S.op("act", lambda e, src=src, c=c, c0=c0, n=n: e.activation(out=kT[:, c - 8, c0:c0 + n], in_=src, func=AF.Copy),
                             r=[("ps", pa)], w=[("kT", c - 8, tb)])
                    elif is_qi:
                        S.op("act", lambda e, src=src, c=c, c0=c0, n=n: e.activation(out=qiT[:, c - 12, c0:c0 + n], in_=src, func=AF.Copy),
                             r=[("ps", pa)], w=[("qiT", c - 12, tb)])
                    elif is_z:
                        S.op("act", lambda e, src=src, c=c, c0=c0, n=n: e.activation(out=szT[:, c - 20, c0:c0 + n], in_=src, func=AF.Silu),
                             r=[("ps", pa)], w=[("szT", c - 20, tb)])
                    elif is_ki2:
                        S.op("act", lambda e, src=src, c0=c0, n=n: e.activation(out=kiT2[:, c0:c0 + n], in_=src, func=AF.Copy),
                             r=[("ps", pa)], w=[("kiT2", tb)])
                    elif is_aux:
                        S.op("act", lambda e, pa=pa, c0=c0, n=n: e.activation(out=dtT[0:16, c0:c0 + n], in_=ps[pa][0:16, 0:n], func=AF.Copy),
                             r=[("ps", pa)], w=[("dtT", tb)])
                    elif is_xbc:
                        S.op("act", lambda e, src=src, c0=c0, n=n: e.activation(out=rawx[:, 3 + c0:3 + c0 + n], in_=src, func=AF.Copy),
                             r=[("ps", pa)], w=[("rawx", tb)])
            if is_xbc:
                xc = c - 28
                rk = [("rawx", tb) for tb in range(ntb)]
                S.op("dve", lambda e, xc=xc: e.tensor_copy(out=rawx[:, 0:3], in_=halo[:, xc, :]), r=["halo"], w=["rawxh"])
                if not main:
                    S.op("dve", lambda e, xc=xc: e.tensor_copy(out=halo[:, xc, :], in_=rawx[:, 3 + 1021:3 + 1024]),
                         r=rk + ["rawxh"], w=["halo"])
                S.op("dve", lambda e, xc=xc: e.tensor_scalar(out=cacc[:], in0=rawx[:, 0:1024], scalar1=cw_t[:, xc, 0:1], scalar2=None,
                                                             op0=ALU.mult), r=rk + ["rawxh", ("cst", 1)], w=["cacc"])
                for k in range(1, 4):
                    S.op("dve", lambda e, xc=xc, k=k: e.scalar_tensor_tensor(out=cacc[:], in0=rawx[:, k:k + 1024], scalar=cw_t[:, xc, k:k + 1],
                                                                            in1=cacc[:], op0=ALU.mult, op1=ALU.add),
                         r=rk + ["rawxh"], w=["cacc"])
                S.op("act", lambda e, xc=xc: e.activation(out=xcT[:, xc, 0:1024], in_=cacc[:], func=AF.Silu, bias=cb_t[:, xc:xc + 1]),
                     r=["cacc", ("cst", 2)], w=[("xcT", xc)])
                if main and "s" not in KSKIP:
                    S.op("dve", lambda e, xc=xc: e.tensor_copy(out=exts[:, :, 0:3], in_=sct[:, xc, :, :]), r=[("cst", 5)], w=["exts"])
                    S.op("dve", lambda e: e.tensor_copy(out=exts[:, :, 3:11], in_=rawx[:, 3 + 1024:3 + 1056].rearrange("p (b t) -> p b t", t=8)),
                         r=rk, w=["exts"])
                    S.op("dve", lambda e, xc=xc: e.tensor_scalar(out=cacc[:, 0:32].rearrange("p (b t) -> p b t", t=8), in0=exts[:, :, 0:8],
                                                                 scalar1=cw_t[:, xc, 0:1], scalar2=None, op0=ALU.mult),
                         r=["exts", ("xcT", xc)], w=["cacc"])
                    for k in range(1, 4):
                        S.op("dve", lambda e, xc=xc, k=k: e.scalar_tensor_tensor(
                            out=cacc[:, 0:32].rearrange("p (b t) -> p b t", t=8), in0=exts[:, :, k:k + 8], scalar=cw_t[:, xc, k:k + 1],
                            in1=cacc[:, 0:32].rearrange("p (b t) -> p b t", t=8), op0=ALU.mult, op1=ALU.add), r=["exts"], w=["cacc"])
                    S.op("act", lambda e, xc=xc: e.activation(out=xcT[:, xc, 1024:1056], in_=cacc[:, 0:32], func=AF.Silu, bias=cb_t[:, xc:xc + 1]),
                         r=["cacc"], w=[("xcT", xc)])
                if main and "t" not in KSKIP:
                    srcs = [1021] + [1024 + b * 8 + 5 for b in range(4)]
                    for si, s0 in enumerate(srcs):
                        pb, off = (7, si * 128) if si < 4 else (6, 0)
                        S.op("pe", lambda e, s0=s0, pb=pb, off=off: e.transpose(out=ps[pb][0:3, off:off + 128],
                                                                              in_=rawx[:, 3 + s0:3 + s0 + 3], identity=ident_f[:]),
                             r=rk + ["ident_f"], w=[("ps", pb)])
                    tl = tails[0]
                    tlk = ("tails", 0)
                    S.op("act", lambda e, tl=tl: e.activation(out=tl[0:3, 0:4, :], in_=ps[7][0:3, 0:512].rearrange("p (s c) -> p s c", s=4),
                                                              func=AF.Copy), r=[("ps", 7)], w=[tlk])
                    S.op("act", lambda e, tl=tl: e.activation(out=tl[0:3, 4, :], in_=ps[6][0:3, 0:128], func=AF.Copy), r=[("ps", 6)], w=[tlk])
                    S.op("sp", lambda e, tl=tl, xc=xc: e.dma_start(out=conv_out[:, :, xc * 128:(xc + 1) * 128].rearrange("s r c -> r s c"),
                                                                   in_=tl[0:3, :, :]), r=[tlk], w=[("conv_out", xc)], dma="tl0")
            if is_k or is_v or is_sm:
                if is_k and not main:
                    continue
                col = (c - 8) * 128 if (is_k or is_v) else 512
                for t in range(ntiles):
                    m = 128 if t < NPT else 35
                    tb = 0 if t < 4 else (1 if t < 8 else 2)
                    pd = 6 + t % 2
                    for kc in range(16):
                        S.op("pe", lambda e, t=t, kc=kc, pd=pd, m=m, wb=wb: e.matmul(
                            ps[pd][0:m, 0:128], lhsT=hT[:, kc, t * 128:t * 128 + m], rhs=wb[:, kc, :],
                            start=(kc == 0), stop=(kc == 15)), r=[wk, ("hT", tb)], w=[("ps", pd)])
                    if is_v:
                        S.op("dve", lambda e, t=t, pd=pd, c=c: e.tensor_copy(out=vb1[:, t, c - 10, 0:128], in_=ps[pd][:, 0:128]),
                             r=[("ps", pd)], w=[("vb1", t)])
                    if is_sm:
                        S.op("dve", lambda e, t=t, pd=pd: e.tensor_copy(out=sm_tok[:, t, :], in_=ps[pd][:, 0:128]),
                             r=[("ps", pd)], w=[("sm_tok", t)])
                    if main and "o" not in KSKIP:
                        ti = tctr[0] % 2
                        tctr[0] += 1
                        if "e" not in KSKIP:
                            S.op("dve", lambda e, ti=ti, pd=pd: e.tensor_copy(out=tokst[ti][:], in_=ps[pd][:, 0:128]),
                                 r=[("ps", pd)], w=[("tokst", ti)])
                        if "d" not in KSKIP:
                          S.op(os.environ.get("KTOKQ", "sp"), lambda e, t=t, ti=ti, col=col: e.dma_start(out=kvs_out[t, :, col:col + 128], in_=tokst[ti][:]),
                             r=[("tokst", ti)], w=[("kvs_out", t, col)], dma=f"tok{ti}")

    dtk, dta, acum, ea, dec, eal, scd, tmp16 = sm16

    def ssd_chunk(c0, L, full):
        cs0, cs1 = c0, c0 + L
        K = ["ssd"]

        def Q(eng, fn):
            S.op(eng, fn, w=K)

        Q("pe", lambda e: e.transpose(out=ps[6][0:L, 0:16], in_=dtT[0:16, cs0:cs1], identity=ident_f[0:16, 0:16]))
        Q("dve", lambda e: e.tensor_tensor(out=tmp16[0:L, :], in0=ps[6][0:L, 0:16], in1=dtb_t[0:L, :], op=ALU.add))
        Q("act", lambda e: e.activation(out=tmp16[0:L, :], in_=tmp16[0:L, :], func=AF.Exp))
        Q("act", lambda e: e.activation(out=dtk[0:L, :], in_=tmp16[0:L, :], func=AF.Ln, bias=1.0))
        Q("dve", lambda e: e.tensor_tensor(out=dta[0:L, :], in0=dtk[0:L, :], in1=a_t[0:L, :], op=ALU.mult))
        Q("pe", lambda e: e.matmul(ps[6][0:L, 16:32], lhsT=tri_f[0:L, 0:L], rhs=dta[0:L, :], start=True, stop=True))
        Q("pe", lambda e: e.matmul(ps[6][:, 32:48], lhsT=ones_f[0:L, :], rhs=dta[0:L, :], start=True, stop=True))
        Q("dve", lambda e: e.tensor_copy(out=acum[0:L, :], in_=ps[6][0:L, 16:32]))
        Q("act", lambda e: e.activation(out=ea[0:L, :], in_=acum[0:L, :], func=AF.Exp))
        Q("dve", lambda e: e.tensor_tensor(out=dec[0:L, :], in0=ps[6][0:L, 32:48], in1=acum[0:L, :], op=ALU.subtract))
        Q("act", lambda e: e.activation(out=dec[0:L, :], in_=dec[0:L, :], func=AF.Exp))
        Q("act", lambda e: e.activation(out=eal[:, :], in_=ps[6][:, 32:48], func=AF.Exp))
        Q("dve", lambda e: e.tensor_tensor(out=scd[0:L, :], in0=dtk[0:L, :], in1=dec[0:L, :], op=ALU.mult))
        pv = [ps[0].bitcast(BF16), ps[1].bitcast(BF16)]
        for j in range(10):
            dst = pv[0][0:L, j * 128:(j + 1) * 128] if j < 8 else pv[1][0:L, (j - 8) * 128:(j - 7) * 128]
            Q("pe", lambda e, j=j, dst=dst: e.transpose(out=dst, in_=xcT[:, j, cs0:cs1], identity=ident_b[:]))
        xv = pv[0][0:L, 0:1024].rearrange("p (h q) -> p h q", q=64)
        Q("dve", lambda e: e.tensor_tensor(out=xd[0:L, :].rearrange("p (h q) -> p h q", q=64), in0=xv,
                                           in1=dtk[0:L, :].rearrange("p (h o) -> p h o", o=1).to_broadcast([L, 16, 64]), op=ALU.mult))
        Q("dve", lambda e: e.tensor_tensor(out=xdd[0:L, :].rearrange("p (h q) -> p h q", q=64), in0=xv,
                                           in1=scd[0:L, :].rearrange("p (h o) -> p h o", o=1).to_broadcast([L, 16, 64]), op=ALU.mult))
        Q("act", lambda e: e.activation(out=Btok[0:L, :], in_=pv[1][0:L, 0:256], func=AF.Copy))
        if full:
            for g in range(2):
                Q("pe", lambda e, g=g: e.matmul(ps[6][0:L, 128 + g * 128:128 + g * 128 + L], lhsT=xcT[:, 8 + g, cs0:cs1],
                                               rhs=xcT[:, 10 + g, cs0:cs1], start=True, stop=True))
                Q("dve", lambda e, g=g: e.tensor_tensor(out=CBm[0:L, g, 0:L], in0=ps[6][0:L, 128 + g * 128:128 + g * 128 + L],
                                                        in1=tri_f[0:L, 0:L], op=ALU.mult))
            for half in range(2):
                Q("dve", lambda e, half=half: e.tensor_tensor(
                    out=Dm[0:L, :, 0:L], in0=ident_f[0:L, 0:L].rearrange("p (o i) -> p o i", o=1).to_broadcast([L, 8, L]),
                    in1=acum[0:L, half * 8:(half + 1) * 8].rearrange("p (h o) -> p h o", o=1).to_broadcast([L, 8, L]), op=ALU.mult))
                for q4 in range(2):
                    Q("pe", lambda e, q4=q4: e.matmul(ps[2 + q4][0:L, 0:4 * L].rearrange("p (h i) -> p h i", h=4), lhsT=ones_f[0:L, 0:L],
                                                     rhs=Dm[0:L, 4 * q4:4 * q4 + 4, 0:L], start=True, stop=True))
                for hh in range(8):
                    h = half * 8 + hh
                    bsrc = ps[2 + hh // 4][0:L, (hh % 4) * L:(hh % 4 + 1) * L]
                    Q("dve", lambda e, bsrc=bsrc, h=h: e.tensor_scalar(out=t1[0:L, 0:L], in0=bsrc, scalar1=acum[0:L, h:h + 1], scalar2=0.0,
                                                                       op0=ALU.subtract, op1=ALU.min))
                    Q("act", lambda e: e.activation(out=t2[0:L, 0:L], in_=t1[0:L, 0:L], func=AF.Exp))
                    Q("dve", lambda e, h=h: e.tensor_tensor(out=Mh[0:L, 0:L], in0=t2[0:L, 0:L], in1=CBm[0:L, h // 8, 0:L], op=ALU.mult))
                    Q("pe", lambda e, h=h: e.matmul(ps[h // 8][0:L, (h % 8) * 64:(h % 8 + 1) * 64], lhsT=Mh[0:L, 0:L],
                                                   rhs=xd[0:L, h * 64:(h + 1) * 64], start=True, stop=True))
            for g in range(2):
                Q("pe", lambda e, g=g: e.matmul(ps[4 + g][0:L, :], lhsT=xcT[:, 10 + g, cs0:cs1], rhs=Hb[:, g * 512:(g + 1) * 512],
                                               start=True, stop=True))
                Q("act", lambda e, g=g: e.activation(out=ysb[0:L, g * 512:(g + 1) * 512], in_=ps[g][0:L, :], func=AF.Copy))
            for h in range(16):
                Q("dve", lambda e, h=h: e.scalar_tensor_tensor(out=ysb[0:L, h * 64:(h + 1) * 64], in0=ps[4 + h // 8][0:L, (h % 8) * 64:(h % 8 + 1) * 64],
                                                               scalar=ea[0:L, h:h + 1], in1=ysb[0:L, h * 64:(h + 1) * 64], op0=ALU.mult, op1=ALU.add))
        for g in range(2):
            Q("pe", lambda e, g=g: e.matmul(ps[2 + g][:, :], lhsT=Btok[0:L, g * 128:(g + 1) * 128], rhs=xdd[0:L, g * 512:(g + 1) * 512],
                                           start=True, stop=True))
        Q("dve", lambda e: e.tensor_tensor(out=Hst[:].rearrange("p (h q) -> p h q", q=64), in0=Hst[:].rearrange("p (h q) -> p h q", q=64),
                                           in1=eal[:, :].rearrange("p (h o) -> p h o", o=1).to_broadcast([128, 16, 64]), op=ALU.mult))
        for g in range(2):
            Q("dve", lambda e, g=g: e.tensor_tensor(out=Hst[:, g * 512:(g + 1) * 512], in0=Hst[:, g * 512:(g + 1) * 512], in1=ps[2 + g][:, :],
                                                    op=ALU.add))
        Q("act", lambda e: e.activation(out=Hb[:], in_=Hst[:], func=AF.Copy))
        if full:
            for j in range(8):
                Q("pe", lambda e, j=j: e.transpose(out=ps[4 + j // 4][:, (j % 4) * L:(j % 4 + 1) * L], in_=ysb[0:L, j * 128:(j + 1) * 128],
                                                  identity=ident_f[0:L, 0:L]))
            for j in range(8):
                ysrc = ps[4 + j // 4][:, (j % 4) * L:(j % 4 + 1) * L]
                Q("dve", lambda e, j=j, ysrc=ysrc: e.scalar_tensor_tensor(out=gbuf[:, j, 0:L], in0=xcT[:, j, cs0:cs1], scalar=dcol_t[:, j:j + 1],
                                                                          in1=ysrc, op0=ALU.mult, op1=ALU.add))
                Q("dve", lambda e, j=j: e.tensor_tensor(out=gbuf[:, j, 0:L], in0=gbuf[:, j, 0:L], in1=szT[:, j, cs0:cs1], op=ALU.mult))
                Q("act", lambda e, j=j: e.activation(out=sq[:, j, 0:L], in_=gbuf[:, j, 0:L], func=AF.Square))
            for g in range(2):
                for jj in range(4):
                    Q("pe", lambda e, g=g, jj=jj: e.matmul(ps[7][:, g * 128:g * 128 + L], lhsT=ones_b[:, :], rhs=sq[:, 4 * g + jj, 0:L],
                                                          start=(jj == 0), stop=(jj == 3)))
                Q("dve", lambda e, g=g: e.tensor_scalar(out=rs[:, g, 0:L], in0=ps[7][:, g * 128:g * 128 + L], scalar1=1.0 / 512, scalar2=EPS,
                                                        op0=ALU.mult, op1=ALU.add))
                Q("act", lambda e, g=g: e.activation(out=rs[:, g, 0:L], in_=rs[:, g, 0:L], func=AF.Sqrt))
                Q("dve", lambda e, g=g: e.reciprocal(out=rs[:, g, 0:L], in_=rs[:, g, 0:L]))
            for j in range(8):
                Q("dve", lambda e, j=j: e.scalar_tensor_tensor(out=ssdT[:, j, cs0:cs1], in0=gbuf[:, j, 0:L], scalar=gssd_t[:, j:j + 1],
                                                               in1=rs[:, j // 4, 0:L], op0=ALU.mult, op1=ALU.mult))


    sc = A.at(OH + 10272, [128, 2048], F32, "sc")
    mk = A.at(OH + 10272 + 8192, [128, 2048], BF16, "mk")
    mkT = A.at(OH + 10272 + 12288, [128, 16, 128], BF16, "mkT")
    o = O_STAGE
    bT = A.at(o, [128, 2, 8, 128], F32, "bT"); o += 8192
    rtmp = A.at(o, [128, 2048], F32, "rtmp"); o += 8192
    pexp = A.at(o, [128, 512], F32, "pexp"); o += 2048
    pm = A.at(o, [128, 512], BF16, "pm"); o += 1024
    atok = A.at(o, [128, 1024], BF16, "atok"); o += 2048
    hw = A.at(o, [128, 32], F32, "hw"); o += 128
    p2 = A.at(o, [128, 32], F32, "p2"); o += 128
    wis = A.at(o, [128, 16], F32, "wis"); o += 64
    cfar = A.at(o, [128, 8], F32, "cfar"); o += 32
    rden = A.at(o, [128, 8], F32, "rden"); o += 32
    a1 = A.at(o, [128, 8], F32, "a1"); o += 32
    rb_t = A.at(o, [32, 8], F32, "rb_t"); o += 32
    oh_t = A.at(o, [32, 256], F32, "oh_t"); o += 1024
    rbb = A.at(o, [32, 8, 128], F32, "rbb"); o += 4096
    assert o <= O_W2G
    NIT = 30

    atn = [0]

    def AQ(eng, fn, dma=None, r=(), w=()):
        if dma is not None:
            atn[0] += 1
            dma = f"at{atn[0] // 100}"
        S.op(eng, fn, r=list(r), w=["att"] + list(w), dma=dma)

    gcn = [0]

    def gather(dst, table, idx_t, j, key):
        gcn[0] += 1
        S.op("pool", lambda e: e.indirect_dma_start(out=dst, out_offset=None, in_=table[:, :],
                                                    in_offset=bass.IndirectOffsetOnAxis(ap=idx_t[:, j:j + 1], axis=0)),
             r=["att_idx"], w=["gch", key], dma=f"ag{gcn[0] // 100}")

    def att_setup():
        AQ("sp", lambda e: e.dma_start(out=rb_t[:], in_=relb[:, :]), dma="at")
        AQ("sp", lambda e: e.dma_start(out=oh_t[:], in_=oh_d[:, :]), dma="at")
        AQ("sp", lambda e: e.dma_start(out=cfar[:], in_=relb[31:32, :].partition_broadcast(128)), dma="at")
        AQ("dve", lambda e: e.tensor_copy(out=rbb[:], in_=rb_t[:].rearrange("p (h o) -> p h o", o=1).to_broadcast([32, 8, 128])))
        for h in range(8):
            AQ("pe", lambda e, h=h: e.matmul(ps[0][:, 0:256], lhsT=rbb[:, h, :], rhs=oh_t[:, :], start=True, stop=True))
            AQ("dve", lambda e: e.tensor_copy(out=rtmp[:, 0:256], in_=ps[0][:, 0:256]))
            AQ("sp", lambda e, h=h: e.dma_start(out=bsc[h, :, :], in_=rtmp[:, 0:256]), dma="at")
        for h in range(8):
            for ty in range(2):
                src = bass.AP(tensor=bsc_t, offset=h * 128 * 256 + 128 * ty, ap=[[255, 128], [1, 128]])
                AQ("sp", lambda e, h=h, ty=ty, src=src: e.dma_start(out=bT[:, ty, h, :], in_=src), dma="at")
        for k in range(NIT):
            AQ("pool", lambda e, k=k: e.memset(p2[:, k:k + 1], 2.0 ** -(k + 1)))

    KATT = int(os.environ.get("KATT", "9"))
    KQT = int(os.environ.get("KQT", "8"))

    def att_prompt_tile(qt):
        if KATT < 2 or qt >= KQT:
            return
        q0 = qt * 128
        nb = 9 + qt
        Sx = nb * 128
        segs = [(kiT2_pre, 0, 512), (kiT2_pre, 512, 512)]
        own = 128 * (qt + 1)
        c = 0
        while c < own:
            n = min(512, own - c)
            segs.append((kiT2, c, n))
            c += n
        AQ("dve", lambda e: e.tensor_scalar(out=wis[:], in0=sm_tok[:, qt, 64:80], scalar1=1024.0 ** -0.5, scalar2=None, op0=ALU.mult))
        for hi in range(16):
            pb = 64 * (hi % 2)
            for si, (src, c0, n) in enumerate(segs):
                AQ("pe", lambda e, si=si, src=src, c0=c0, n=n, pb=pb, hi=hi: e.matmul(
                    ps[si][:, 0:n], lhsT=qiT[pb:pb + 64, hi // 2, q0:q0 + 128], rhs=src[pb:pb + 64, c0:c0 + n], start=True, stop=True))
            AQ("act", lambda e: e.activation(out=rtmp[:, 0:Sx], in_=psall[:, 0:Sx], func=AF.Relu))
            if hi == 0:
                AQ("dve", lambda e: e.tensor_scalar(out=sc[:, 0:Sx], in0=rtmp[:, 0:Sx], scalar1=wis[:, 0:1], scalar2=None, op0=ALU.mult))
            else:
                AQ("dve", lambda e, hi=hi: e.scalar_tensor_tensor(out=sc[:, 0:Sx], in0=rtmp[:, 0:Sx], scalar=wis[:, hi:hi + 1], in1=sc[:, 0:Sx],
                                                                  op0=ALU.mult, op1=ALU.add))
        if KATT < 3:
            return
        absm, lo, mid, cnt, tt, Wd = [a1[:, i:i + 1] for i in range(6)]
        AQ("dve", lambda e: e.tensor_reduce(out=absm, in_=sc[:, 0:Sx], axis=AX.X, op=ALU.max, apply_absolute_value=True))
        AQ("dve", lambda e: e.tensor_scalar(out=sc[:, 0:1024], in0=sc[:, 0:1024], scalar1=flag_t[:, 1:2], scalar2=None, op0=ALU.add))
        AQ("pool", lambda e: e.affine_select(out=sc[:, Sx - 128:Sx], in_=sc[:, Sx - 128:Sx], pattern=[[-1, 128]], compare_op=ALU.is_ge,
                                             fill=-1e30, base=0, channel_multiplier=1))
        AQ("dve", lambda e: e.tensor_scalar(out=lo, in0=absm, scalar1=-1.0, scalar2=-1.0, op0=ALU.mult, op1=ALU.add))
        AQ("dve", lambda e: e.tensor_scalar(out=Wd, in0=absm, scalar1=2.0, scalar2=2.0, op0=ALU.mult, op1=ALU.add))
        AQ("dve", lambda e: e.tensor_scalar(out=hw[:, 0:NIT], in0=p2[:, 0:NIT], scalar1=Wd, scalar2=None, op0=ALU.mult))
        for k in range(NIT):
            AQ("dve", lambda e, k=k: e.tensor_tensor(out=mid, in0=lo, in1=hw[:, k:k + 1], op=ALU.add))
            AQ("dve", lambda e: e.tensor_scalar(out=mk[:, 0:Sx], in0=sc[:, 0:Sx], scalar1=mid, scalar2=0.0, op0=ALU.is_ge, op1=ALU.add, accum_out=cnt))
            AQ("dve", lambda e, k=k: e.tensor_scalar(out=tt, in0=cnt, scalar1=256.0, scalar2=hw[:, k:k + 1], op0=ALU.is_ge, op1=ALU.mult))
            AQ("dve", lambda e: e.tensor_tensor(out=lo, in0=lo, in1=tt, op=ALU.add))
        AQ("dve", lambda e: e.tensor_scalar(out=mk[:, 0:Sx], in0=sc[:, 0:Sx], scalar1=lo, scalar2=None, op0=ALU.is_ge))
        if KATT < 4:
            return
        pv0, pv1 = ps[0].bitcast(BF16), ps[1].bitcast(BF16)
        for kb in range(nb):
            dst = pv0[:, kb * 128:(kb + 1) * 128] if kb < 8 else pv1[:, (kb - 8) * 128:(kb - 7) * 128]
            AQ("pe", lambda e, kb=kb, dst=dst: e.transpose(out=dst, in_=mk[:, kb * 128:(kb + 1) * 128], identity=ident_b[:]))
        AQ("dve", lambda e: e.tensor_copy(out=mkT[:, 0:8, :], in_=pv0.rearrange("p (k c) -> p k c", c=128)))
        AQ("dve", lambda e: e.tensor_copy(out=mkT[:, 8:nb, :], in_=pv1[:, 0:(nb - 8) * 128].rearrange("p (k c) -> p k c", c=128)))

        if KATT < 5:
            return

        def pso(h):
            return ps[5 + h // 3][:, (h % 3) * 160:(h % 3) * 160 + 129]

        for kb in range(nb):
            pre = kb < 8
            j = kb if pre else kb - 8
            ksrc = kT_pre if pre else kT
            vsrc = vb1_pre if pre else vb1
            diff = (8 + qt) - kb
            for kv in range(2):
                AQ("pe", lambda e, kv=kv, ksrc=ksrc, j=j: e.matmul(ps[2 + kv].rearrange("p (h c) -> p h c", c=128), lhsT=ksrc[:, kv, j * 128:(j + 1) * 128],
                                                                  rhs=qT[:, 4 * kv:4 * kv + 4, q0:q0 + 128], start=True, stop=True))
                if diff >= 2:
                    for hh in range(4):
                        AQ("act", lambda e, kv=kv, hh=hh: e.activation(out=pexp[:, hh * 128:(hh + 1) * 128], in_=ps[2 + kv][:, hh * 128:(hh + 1) * 128],
                                                                       func=AF.Exp, bias=cfar[:, 4 * kv + hh:4 * kv + hh + 1]))
                else:
                    AQ("dve", lambda e, kv=kv, diff=diff: e.tensor_tensor(out=pexp[:].rearrange("p (h c) -> p h c", c=128),
                                                                          in0=ps[2 + kv].rearrange("p (h c) -> p h c", c=128),
                                                                          in1=bT[:, diff, 4 * kv:4 * kv + 4, :], op=ALU.add))
                    AQ("act", lambda e: e.activation(out=pexp[:], in_=pexp[:], func=AF.Exp))
                AQ("dve", lambda e, kb=kb: e.tensor_tensor(out=pm[:].rearrange("p (h c) -> p h c", c=128), in0=pexp[:].rearrange("p (h c) -> p h c", c=128),
                                                           in1=mkT[:, kb:kb + 1, :].to_broadcast([128, 4, 128]), op=ALU.mult))
                for hh in range(4):
                    h = 4 * kv + hh
                    AQ("pe", lambda e, h=h, hh=hh, vsrc=vsrc, j=j, kv=kv, kb=kb: e.matmul(pso(h), lhsT=pm[:, hh * 128:(hh + 1) * 128], rhs=vsrc[:, j, kv, :],
                                                                                       start=(kb == 0), stop=(kb == nb - 1)))
        if KATT < 6:
            return
        for b3 in range(3):
            nh = 3 if b3 < 2 else 2
            AQ("dve", lambda e, b3=b3, nh=nh: e.reciprocal(out=rden[:, 3 * b3:3 * b3 + nh],
                                                          in_=ps[5 + b3][:, 0:480].rearrange("p (h c) -> p h c", c=160)[:, 0:nh, 128]))
        for h in range(8):
            AQ("dve", lambda e, h=h: e.tensor_scalar(out=atok[:, h * 128:(h + 1) * 128], in0=pso(h)[:, 0:128], scalar1=rden[:, h:h + 1], scalar2=None,
                                                     op0=ALU.mult))
        for h in range(8):
            AQ("pe", lambda e, h=h: e.transpose(out=pv0[:, h * 128:(h + 1) * 128], in_=atok[:, h * 128:(h + 1) * 128], identity=ident_b[:]))
        AQ("dve", lambda e: e.tensor_copy(out=attT[:, :, q0:q0 + 128], in_=pv0.rearrange("p (k c) -> p k c", c=128)))
        S.op("pool", lambda e: e.memset(stat[:, 6:7], 0.0), r=["att"], w=["mixT"])


    def att_sample():
        o = O_W2G
        def T(shape, dt, nm):
            nonlocal o
            nb_ = int(np.prod(shape[1:])) * (2 if dt == BF16 else 4)
            t_ = A.at(o, shape, dt, nm)
            o = al(o + nb_)
            return t_
        pti = T([128, 128], I32, "s_pti"); ptf = T([128, 128], F32, "s_ptf"); idx_i = T([128, 128], I32, "s_idx")
        iota_c = T([128, 8], F32, "s_iota")
        kig2 = [T([128, 4, 64], F32, "s_kig0"), A.at(O_STAGE + 18432, [128, 4, 64], F32, "s_kig1")]; kiTs = T([64, 512], BF16, "s_kiTs")
        qiS = T([64, 16, 8], BF16, "s_qiS")
        wis32 = T([32, 16], F32, "s_wis32"); wd32 = T([32, 16, 8], F32, "s_wd32"); wdb = T([32, 128], F32, "s_wdb")
        wrow = T([128, 128], F32, "s_wrow")
        bm_t = T([32, 4], F32, "s_bm"); dq_t = T([32, 8], F32, "s_dq"); pen_t = T([32, 4, 8], F32, "s_pen")
        scS = T([128, 132, 8], F32, "s_scS"); ind = T([128, 132, 8], BF16, "s_ind"); mS = T([128, 132, 8], BF16, "s_mS")
        KVg2 = [A.at(O_STAGE, [128, 4, 512], F32, "s_KVg0"), A.at(O_STAGE + 10240, [128, 4, 512], F32, "s_KVg1")]
        kTs = T([128, 4, 2, 128], BF16, "s_kTs"); Vb = T([128, 4, 2, 129], BF16, "s_Vb")
        pS = T([128, 256], F32, "s_pS"); pmS = T([128, 256], BF16, "s_pmS")
        cfS = T([128, 8, 8], F32, "s_cfS"); bSl = T([128, 8, 8], F32, "s_bSl"); bSn = T([32, 8, 8], F32, "s_bSn")
        rw = [T([128, 8], F32, f"s_rw{i}") for i in range(8)]
        dg = T([8, 8], F32, "s_dg"); m2 = T([8, 8], F32, "s_m2")
        atS = T([32, 2, 128], BF16, "s_atS"); rdS = T([32, 8], F32, "s_rdS")
        assert o <= O_W2G + 12 * NB, (o - O_W2G, 12 * NB)
        absr, lor, midr, cntr, ttr, Wr, pc, hwr = rw
        NPG = 128

        AQ("pool", lambda e: e.iota(iota_c[:, 0:1], pattern=[[0, 1]], base=0, channel_multiplier=1, allow_small_or_imprecise_dtypes=True))
        AQ("sp", lambda e: e.dma_start(out=bm_t[:], in_=bm4[:, :]), dma="at")
        AQ("sp", lambda e: e.dma_start(out=dq_t[:], in_=dq8[:, :]), dma="at")
        AQ("sp", lambda e: e.dma_start(out=pen_t[:], in_=pen32[:, :, :]), dma="at")
        AQ("dve", lambda e: e.tensor_scalar(out=wis32[:], in0=sm_tok[0:32, 8, 64:80], scalar1=1024.0 ** -0.5, scalar2=None, op0=ALU.mult))
        AQ("dve", lambda e: e.tensor_tensor(out=wd32[:], in0=wis32[:].rearrange("p (h o) -> p h o", o=1).to_broadcast([32, 16, 8]),
                                            in1=dq_t[:].rearrange("p (o q) -> p o q", o=1).to_broadcast([32, 16, 8]), op=ALU.mult))
        AQ("dve", lambda e: e.tensor_copy(out=cfS[:], in_=cfar[:].rearrange("p (h o) -> p h o", o=1).to_broadcast([128, 8, 8])))
        for h in range(8):
            src = bass.AP(tensor=bsc_t, offset=h * 128 * 256 + 128, ap=[[255, 128], [1, 8]])
            AQ("sp", lambda e, h=h, src=src: e.dma_start(out=bSl[:, h, :], in_=src), dma="at")
            for tb in range(4):
                src2 = bass.AP(tensor=bsc_t, offset=h * 128 * 256, ap=[[255, 8], [1, 8]])
                AQ("sp", lambda e, h=h, tb=tb, src2=src2: e.dma_start(out=bSn[8 * tb:8 * tb + 8, h, :], in_=src2), dma="at")
        AQ("pool", lambda e: e.memset(Vb[:, :, :, 128:129], 1.0))

        for b in range(4):
            cb = 1024 + 8 * b
            AQ("sp", lambda e, b=b: e.dma_start(out=pti[:], in_=pt4[b:b + 1, :].partition_broadcast(128)), dma="at")
            AQ("dve", lambda e: e.tensor_copy(out=ptf[:], in_=pti[:]))
            AQ("dve", lambda e: e.tensor_scalar(out=ptf[:], in0=ptf[:], scalar1=128.0, scalar2=iota_c[:, 0:1], op0=ALU.mult, op1=ALU.add))
            AQ("dve", lambda e: e.tensor_copy(out=idx_i[:], in_=ptf[:]), w=["att_idx"])
            qv = qiS[:].rearrange("d (hc two) q -> d hc two q", two=2)
            AQ("sp", lambda e, cb=cb, qv=qv: e.dma_start(out=qv[:, :, 0, :], in_=qiT[0:64, :, cb:cb + 8]), dma="at")
            AQ("sp", lambda e, cb=cb, qv=qv: e.dma_start(out=qv[:, :, 1, :], in_=qiT[64:128, :, cb:cb + 8]), dma="at")
            qflat = qiS[:].rearrange("d h q -> d (h q)")
            AQ("dve", lambda e, b=b: e.tensor_scalar(out=wdb[:], in0=wd32[:].rearrange("p h q -> p (h q)"), scalar1=bm_t[:, b:b + 1], scalar2=None, op0=ALU.mult))
            AQ("pe", lambda e: e.matmul(ps[0][:, 0:128], lhsT=ones_f[0:32, :], rhs=wdb[:], start=True, stop=True))
            AQ("dve", lambda e: e.tensor_copy(out=wrow[:], in_=ps[0][:, 0:128]))
            AQ("pool", lambda e: e.memset(scS[:, 128, :], -1e30))
            AQ("pe", lambda e, qflat=qflat: e.matmul(ps[0][0:32, 0:128], lhsT=kiT2[0:64, 1024:1056], rhs=qflat, start=True, stop=True))
            AQ("act", lambda e: e.activation(out=rtmp[0:32, 0:128], in_=ps[0][0:32, 0:128], func=AF.Relu))
            AQ("dve", lambda e: e.tensor_tensor(out=rtmp[0:32, 0:128], in0=rtmp[0:32, 0:128], in1=wrow[0:32, :], op=ALU.mult))
            AQ("dve", lambda e: e.tensor_reduce(out=scS[0:32, 128, :], in_=rtmp[0:32, 0:128].rearrange("p (h q) -> p q h", q=8), axis=AX.X, op=ALU.add))
            AQ("dve", lambda e, b=b: e.tensor_tensor(out=scS[0:32, 128, :], in0=scS[0:32, 128, :], in1=pen_t[:, b, :], op=ALU.add))
            for st in range(NPG // 4):
                j0 = 4 * st
                kig = kig2[st % 2]
                for pg in range(4):
                    gather(kig[:, pg, :], kidx_tab, idx_i, j0 + pg, ("kig", st % 2, pg))
                for pg in range(4):
                    AQ("pe", lambda e, pg=pg, kig=kig: e.transpose(out=ps[0][0:64, pg * 128:(pg + 1) * 128], in_=kig[:, pg, :], identity=ident_f[:]),
                       r=[("kig", st % 2, pg)])
                AQ("dve", lambda e: e.tensor_copy(out=kiTs[:], in_=ps[0][0:64, :]))
                for pg in range(4):
                    AQ("pe", lambda e, pg=pg, qflat=qflat: e.matmul(ps[1][:, pg * 128:(pg + 1) * 128], lhsT=kiTs[:, pg * 128:(pg + 1) * 128], rhs=qflat,
                                                                   start=True, stop=True))
                AQ("act", lambda e: e.activation(out=rtmp[:, 0:512], in_=ps[1][:, :], func=AF.Relu))
                AQ("dve", lambda e: e.tensor_tensor(out=rtmp[:, 0:512].rearrange("p (g c) -> p g c", g=4), in0=rtmp[:, 0:512].rearrange("p (g c) -> p g c", g=4),
                                                    in1=wrow[:].rearrange("p (o c) -> p o c", o=1).to_broadcast([128, 4, 128]), op=ALU.mult))
                AQ("dve", lambda e, j0=j0: e.tensor_reduce(out=scS[:, j0:j0 + 4, :], in_=rtmp[:, 0:512].rearrange("p (g h q) -> p g q h", g=4, q=8),
                                                           axis=AX.X, op=ALU.add))
            AQ("dve", lambda e: e.tensor_reduce(out=pc[:], in_=scS[:, 0:128, :].rearrange("p k q -> p q k"), axis=AX.X, op=ALU.max, apply_absolute_value=True))
            AQ("pe", lambda e: e.transpose(out=ps[0][0:8, 0:128], in_=pc[:], identity=ident_f[:]))
            AQ("dve", lambda e: e.tensor_reduce(out=m2[:, 0:1], in_=ps[0][0:8, 0:128], axis=AX.X, op=ALU.max))
            AQ("dve", lambda e: e.tensor_scalar(out=dg[:], in0=ident_f[0:8, 0:8], scalar1=m2[:, 0:1], scalar2=None, op0=ALU.mult))
            AQ("pe", lambda e: e.matmul(ps[0][:, 0:8], lhsT=ones_f[0:8, :], rhs=dg[:], start=True, stop=True))
            AQ("dve", lambda e: e.tensor_copy(out=absr[:], in_=ps[0][:, 0:8]))
            AQ("dve", lambda e: e.tensor_scalar(out=lor[:], in0=absr[:], scalar1=-1.0, scalar2=-1.0, op0=ALU.mult, op1=ALU.add))
            AQ("dve", lambda e: e.tensor_scalar(out=Wr[:], in0=absr[:], scalar1=2.0, scalar2=2.0, op0=ALU.mult, op1=ALU.add))
            for k in range(NIT):
                AQ("dve", lambda e, k=k: e.tensor_scalar(out=hwr[:], in0=Wr[:], scalar1=2.0 ** -(k + 1), scalar2=None, op0=ALU.mult))
                AQ("dve", lambda e: e.tensor_tensor(out=midr[:], in0=lor[:], in1=hwr[:], op=ALU.add))
                AQ("dve", lambda e: e.tensor_tensor(out=ind[:, 0:129, :], in0=scS[:, 0:129, :],
                                                    in1=midr[:].rearrange("p (o q) -> p o q", o=1).to_broadcast([128, 129, 8]), op=ALU.is_ge))
                AQ("dve", lambda e: e.tensor_reduce(out=pc[:], in_=ind[:, 0:129, :].rearrange("p k q -> p q k"), axis=AX.X, op=ALU.add))
                AQ("pe", lambda e: e.matmul(ps[0][:, 0:8], lhsT=ones_f[:, :], rhs=pc[:], start=True, stop=True))
                AQ("dve", lambda e: e.tensor_scalar(out=ttr[:], in0=ps[0][:, 0:8], scalar1=256.0, scalar2=None, op0=ALU.is_ge))
                AQ("dve", lambda e: e.tensor_tensor(out=ttr[:], in0=ttr[:], in1=hwr[:], op=ALU.mult))
                AQ("dve", lambda e: e.tensor_tensor(out=lor[:], in0=lor[:], in1=ttr[:], op=ALU.add))
            AQ("dve", lambda e: e.tensor_tensor(out=mS[:, 0:129, :], in0=scS[:, 0:129, :],
                                                in1=lor[:].rearrange("p (o q) -> p o q", o=1).to_broadcast([128, 129, 8]), op=ALU.is_ge))
            def pso(kv):
                return ps[4][0:32, kv * 160:kv * 160 + 129]
            for st in range(NPG // 4):
                j0 = 4 * st
                KVg = KVg2[st % 2]
                for pg in range(4):
                    gather(KVg[:, pg, :], kv_tab, idx_i, j0 + pg, ("kvg", st % 2, pg))
                for pg in range(4):
                    for kv in range(2):
                        r_ = pg * 2 + kv
                        AQ("pe", lambda e, pg=pg, kv=kv, r_=r_, KVg=KVg: e.transpose(out=ps[r_ // 4][:, (r_ % 4) * 128:(r_ % 4 + 1) * 128],
                                                                                   in_=KVg[:, pg, kv * 128:(kv + 1) * 128], identity=ident_f[:]),
                           r=[("kvg", st % 2, pg)])
                AQ("dve", lambda e: e.tensor_copy(out=kTs[:, 0:2, :, :].rearrange("p g k s -> p (g k s)"), in_=ps[0][:, :]))
                AQ("act", lambda e: e.activation(out=kTs[:, 2:4, :, :].rearrange("p g k s -> p (g k s)"), in_=ps[1][:, :], func=AF.Copy))
                AQ("dve", lambda e, KVg=KVg: e.tensor_copy(out=Vb[:, :, :, 0:128], in_=KVg[:, :, 256:512].rearrange("p g (k d) -> p g k d", k=2)),
                   r=[("kvg", st % 2, pg) for pg in range(4)])
                for pg in range(4):
                    for kv in range(2):
                        r_ = pg * 2 + kv
                        AQ("pe", lambda e, pg=pg, kv=kv, r_=r_, cb=cb: e.matmul(ps[2][:, r_ * 32:(r_ + 1) * 32].rearrange("p (h q) -> p h q", q=8),
                                                                              lhsT=kTs[:, pg, kv, :], rhs=qT[:, 4 * kv:4 * kv + 4, cb:cb + 8], start=True, stop=True))
                AQ("dve", lambda e: e.tensor_tensor(out=pS[:].rearrange("p (g c) -> p g c", g=4), in0=ps[2][:, 0:256].rearrange("p (g c) -> p g c", g=4),
                                                    in1=cfS[:].rearrange("p h q -> p (h q)").rearrange("p (o c) -> p o c", o=1).to_broadcast([128, 4, 64]), op=ALU.add))
                if st == NPG // 4 - 1:
                    AQ("dve", lambda e: e.tensor_tensor(out=pS[:, 192:256], in0=ps[2][:, 192:256], in1=bSl[:].rearrange("p h q -> p (h q)"), op=ALU.add))
                AQ("act", lambda e: e.activation(out=pS[:], in_=pS[:], func=AF.Exp))
                AQ("dve", lambda e, j0=j0: e.tensor_tensor(out=pmS[:].rearrange("p (g h q) -> p g h q", g=4, q=8), in0=pS[:].rearrange("p (g h q) -> p g h q", g=4, q=8),
                                                           in1=mS[:, j0:j0 + 4, :].rearrange("p g (o q) -> p g o q", o=1).to_broadcast([128, 4, 8, 8]), op=ALU.mult))
                for pg in range(4):
                    for kv in range(2):
                        r_ = pg * 2 + kv
                        AQ("pe", lambda e, pg=pg, kv=kv, r_=r_, st=st: e.matmul(pso(kv), lhsT=pmS[:, r_ * 32:(r_ + 1) * 32], rhs=Vb[:, pg, kv, :],
                                                                              start=(st == 0 and pg == 0), stop=False))
            for kv in range(2):
                AQ("pe", lambda e, kv=kv, cb=cb: e.matmul(ps[2][0:32, kv * 32:(kv + 1) * 32].rearrange("p (h q) -> p h q", q=8), lhsT=kT[:, kv, 1024:1056],
                                                         rhs=qT[:, 4 * kv:4 * kv + 4, cb:cb + 8], start=True, stop=True))
            AQ("dve", lambda e: e.tensor_tensor(out=pS[0:32, 0:64], in0=ps[2][0:32, 0:64], in1=bSn[:].rearrange("p h q -> p (h q)"), op=ALU.add))
            AQ("act", lambda e: e.activation(out=pS[0:32, 0:64], in_=pS[0:32, 0:64], func=AF.Exp))
            AQ("dve", lambda e: e.tensor_tensor(out=pmS[0:32, 0:64].rearrange("p (h q) -> p h q", q=8), in0=pS[0:32, 0:64].rearrange("p (h q) -> p h q", q=8),
                                                in1=mS[0:32, 128:129, :].to_broadcast([32, 8, 8]), op=ALU.mult))
            for kv in range(2):
                AQ("pe", lambda e, kv=kv: e.matmul(pso(kv), lhsT=pmS[0:32, kv * 32:(kv + 1) * 32], rhs=vb1[0:32, 8, kv, :], start=False, stop=True))
            for kv in range(2):
                AQ("dve", lambda e, kv=kv: e.reciprocal(out=rdS[:, kv:kv + 1], in_=pso(kv)[:, 128:129]))
                AQ("dve", lambda e, kv=kv: e.tensor_scalar(out=atS[:, kv, :], in0=pso(kv)[:, 0:128], scalar1=rdS[:, kv:kv + 1], scalar2=None, op0=ALU.mult))
            pvb = ps[0].bitcast(BF16)
            for kv in range(2):
                AQ("pe", lambda e, kv=kv: e.transpose(out=pvb[:, kv * 32:(kv + 1) * 32], in_=atS[:, kv, :], identity=ident_b[0:32, 0:32]))
            AQ("dve", lambda e, cb=cb: e.tensor_copy(out=attT[:, :, cb:cb + 8], in_=pvb[:, 0:64].rearrange("p (h q) -> p h q", q=8)))
        S.op("pool", lambda e: e.memset(stat[:, 6:7], 0.0), r=["att"], w=["mixT"])

    xst = [A.at(OH + 10272 + i * 2048, [128, 512], F32, f"xst{i}") for i in range(4)]
    xctr = [0]

    def outproj():
        for nb in range(4):
            wt = w2g[nb % 2]
            wv = wt[:].rearrange("p g d -> p (g d)").rearrange("p (k c) -> p k c", c=512)
            for kg in range(4):
                st, sk, sg, i = stage_slot()
                S.op("sp", lambda e, st=st, nb=nb, kg=kg: e.dma_start(out=st[:].rearrange("p a b -> p (a b)"),
                                                                      in_=wout[nb, kg].rearrange("p k c -> p (k c)")), w=[sk], dma=sg)
                S.op("act", lambda e, st=st, wv=wv, kg=kg: e.activation(out=wv[:, kg * 4:(kg + 1) * 4, :],
                                                                        in_=st[:].rearrange("p a b -> p (a b)").rearrange("p (k c) -> p k c", c=512),
                                                                        func=AF.Copy), r=[sk], w=[("wo", nb % 2, kg)])
            for t in range(NTT):
                m = 128 if t < NPT else 35
                pd = 6 + t % 2
                xi = xctr[0] % 2
                xctr[0] += 1
                S.op("sp", lambda e, t=t, nb=nb, xi=xi: e.dma_start(out=xst[xi][:], in_=xsp[t, :, nb * 512:(nb + 1) * 512]),
                     r=[("xsp", t, nb)], w=[("xst", xi)], dma=f"xsi{xi}")
                for kc in range(16):
                    src = attT if kc < 8 else ssdT
                    S.op("pe", lambda e, t=t, kc=kc, pd=pd, m=m, src=src, wv=wv: e.matmul(
                        ps[pd][0:m, :], lhsT=src[:, kc % 8, t * 128:t * 128 + m], rhs=wv[:, kc, :], start=(kc == 0), stop=(kc == 15)),
                         r=[("wo", nb % 2, kc // 4), "mixT"], w=[("ps", pd)])
                S.op("dve", lambda e, xi=xi, pd=pd, m=m: e.tensor_tensor(out=xst[xi][0:m, :], in0=ps[pd][0:m, :], in1=xst[xi][0:m, :], op=ALU.add),
                     r=[("ps", pd)], w=[("xst", xi)])
                S.op("sp", lambda e, t=t, nb=nb, xi=xi: e.dma_start(out=xsp[t, :, nb * 512:(nb + 1) * 512], in_=xst[xi][:]),
                     r=[("xst", xi)], w=[("xsp", t, nb)], dma=f"xso{xi}")

    def final_norm():
        S.op("sp", lambda e: e.dma_start(out=gb[:], in_=gvec[3:4, :].partition_broadcast(128)),
             r=["stage0"], w=["gb", "stage0"], dma="gld")
        for t in range(NTT):
            S.op("act", lambda e, t=t: e.activation(out=junk[:], in_=xres[:, t, :], func=AF.Square, accum_out=stat[:, 0:1]),
                 r=[("x", t)], w=["junk", "ss", "stage1"])
            S.op("dve", lambda e: e.tensor_scalar(out=stat[:, 1:2], in0=stat[:, 0:1], scalar1=1.0 / D, scalar2=EPS,
                                                  op0=ALU.mult, op1=ALU.add), r=["ss"], w=["ms"])
            S.op("act", lambda e: e.activation(out=stat[:, 2:3], in_=stat[:, 1:2], func=AF.Sqrt), r=["ms"], w=["sd"])
            S.op("dve", lambda e: e.reciprocal(out=stat[:, 3:4], in_=stat[:, 2:3]), r=["sd"], w=["rstd"])
            S.op("dve", lambda e, t=t: e.scalar_tensor_tensor(out=xres[:, t, :], in0=xres[:, t, :], scalar=stat[:, 3:4], in1=gb[:],
                                                              op0=ALU.mult, op1=ALU.mult),
                 r=[("x", t), "rstd", "gb", "stage0"], w=[("x", t)])
            S.op("sp", lambda e, t=t: e.dma_start(out=y_out[t, :, :], in_=xres[:, t, :]), r=[("x", t)], w=[("y_out", t)], dma="out")

    try:
        load_x(xpre, NPT)
        norm_to_hT(0, NPT)
        ffn(wf1, NPT)
        norm_to_hT(1, NPT)
        bar()
        inproj(False)
        bar()
        S.op("pool", lambda e: e.memset(Hst[:], 0.0), w=["ssd"])
        for t in range(NPT):
            ssd_chunk(t * 128, 128, False)
        S.op("dve", lambda e: e.tensor_scalar(out=Hst[:], in0=Hst[:], scalar1=flag_t[:, 0:1], scalar2=None, op0=ALU.mult), r=[("cst", 0)], w=["ssd"])
        S.op("sp", lambda e: e.dma_start(out=pre_k[:, :, :], in_=kT[:, :, 0:1024]), w=["pre0"], dma="pre")
        S.op("sp", lambda e: e.dma_start(out=pre_v[:, :, :], in_=vb1[:, 0:8, :, :].rearrange("p t k d -> p t (k d)")),
             r=["vb1ones"], w=["pre1"], dma="pre")
        S.op("sp", lambda e: e.dma_start(out=pre_ki[:, :], in_=kiT2[:, 0:1024]), w=["pre2"], dma="pre")
        S.op("sp", lambda e: e.dma_start(out=pre_h[:, :], in_=Hst[:]), r=["ssd"], w=["pre3"], dma="pre")
        S.op("sp", lambda e: e.dma_start(out=pre_halo[:, :], in_=halo[:].rearrange("p a b -> p (a b)")), r=["halo"], w=["pre4"], dma="pre")
        S.dma_batch_end("pre")
        bar()
        load_x(xin, NTT)
        norm_to_hT(0, NTT)
        ffn(wf1, NTT)
        norm_to_hT(1, NTT)
        for t in range(NTT):
            S.op("sp", lambda e, t=t: e.dma_start(out=xsp[t, :, :], in_=xres[:, t, :]), r=[("x", t)], w=[("xsp", t, nb) for nb in range(4)], dma="xsp")
        bar()
        inproj(True)
        bar()
        S.op("sp", lambda e: e.dma_start(out=kT_pre[:], in_=pre_k[:, :, :]), w=["kT_pre"], dma="pre")
        S.op("sp", lambda e: e.dma_start(out=vb1_pre[:].rearrange("p t k d -> p t (k d)"), in_=pre_v[:, :, :]),
             w=["vb1_pre"], dma="pre")
        S.op("sp", lambda e: e.dma_start(out=kiT2_pre[:], in_=pre_ki[:, :]), w=["kiT2_pre"], dma="pre")
        S.op("sp", lambda e: e.dma_start(out=Hst[:], in_=pre_h[:, :]), w=["ssd"], dma="pre")
        S.dma_batch_end("pre")
        S.op("act", lambda e: e.activation(out=Hb[:], in_=Hst[:], func=AF.Copy), w=["ssd"])
        for t in range(NPT):
            ssd_chunk(t * 128, 128, True)
        S.op("sp", lambda e: e.dma_start(out=ssm_out[0, :, :], in_=Hst[:]), r=["ssd"], w=[("ssm_out", 0)], dma="hs")
        for b in range(4):
            S.op("sp", lambda e, b=b: e.dma_start(out=Hst[:], in_=sssmT[b]), r=[("ssm_out", b)], w=["ssd"], dma="hs")
            S.op("act", lambda e: e.activation(out=Hb[:], in_=Hst[:], func=AF.Copy), w=["ssd"])
            ssd_chunk(1024 + 8 * b, 8, True)
            S.op("sp", lambda e, b=b: e.dma_start(out=ssm_out[1 + b, :, :], in_=Hst[:]), r=["ssd"], w=[("ssm_out", 1 + b)], dma="hs")
        bar()
        S.op("pool", lambda e: e.memset(attT[:], 0.0), w=["mixT", "att"])
        att_setup()
        for qt in range(NPT):
            att_prompt_tile(qt)
        if "S" not in KSKIP:
            att_sample()
        S.op("pool", lambda e: e.memset(ssdT[:, :, 1056:NTOK], 0.0), r=["ssd"], w=["mixT", "ssd"])
        bar()
        outproj()
        bar()
        load_x(xsp, NTT)
        if "h" not in KSKIP:
            norm_to_hT(2, NTT)
        if "f" not in KSKIP:
            ffn(wf2, NTT)
        if "n" not in KSKIP:
            final_norm()
    except _Stop:
        pass
    fin = ["out"] + [f"tok{i}" for i in range(2)] + ["tl0"] + ["hs"]
    S.emit(final_dma_groups=fin)
    return nc


def _up_layout(W):
    K, N = W.shape
    assert K == D and N % 128 == 0
    return np.ascontiguousarray(W.reshape(16, 128, N // 128, 128).transpose(2, 1, 0, 3))


def _t5_onehot():
    n = np.arange(256)
    nf = np.maximum(n, 1).astype(np.float32)
    large = 16 + (np.log(nf / np.float32(16)) / np.float32(np.log(128 / 16)) * np.float32(16)).astype(np.int32)
    bucket = np.where(n < 16, n, np.minimum(large, 31))
    oh = np.zeros((32, 256), np.float32)
    oh[bucket, n] = 1.0
    return oh


_IN_OFF = dict(q=(0, 1024), k=(1024, 256), v=(1280, 256), qi=(1536, 1024), ki=(2560, 64), wi=(2624, 16),
               z=(2640, 1024), xbc=(3664, 1536), dt=(5200, 16))


def kernel(x_prompt, x_sample, cache_k, cache_v, cache_kidx, state_conv, state_ssm, page_table, rel_bias,
           g_ffn1, w1_ffn1, w3_ffn1, w2_ffn1, g_mix, w_in, conv_w, conv_b, a_log, dt_bias, d_skip, g_ssd,
           w_out, g_ffn2, w1_ffn2, w3_ffn2, w2_ffn2, g_final):
    f = lambda a: np.asarray(a, dtype=np.float32)
    xp, xs_ = f(x_prompt), f(x_sample)
    win_full = f(w_in)[0]
    cols = []
    for nm in ("q", "k", "v", "qi", "z", "xbc", "ki", "wi", "dt"):
        o, n = _IN_OFF[nm]
        cols.append(win_full[:, o:o + n])
    cols.append(np.zeros((D, 32), np.float32))
    ko, kn = _IN_OFF["ki"]
    cols += [win_full[:, ko:ko + kn], win_full[:, ko:ko + kn]]
    do, dn = _IN_OFF["dt"]
    cols += [win_full[:, do:do + dn], np.zeros((D, 112), np.float32)]
    win_r = _up_layout(np.concatenate(cols, axis=1))
    shared = dict(
        gvec=np.stack([f(g_ffn1)[0], f(g_mix)[0], f(g_ffn2)[0], f(g_final)]),
        w1a=_up_layout(f(w1_ffn1)[0]), w3a=_up_layout(f(w3_ffn1)[0]), w2a=np.ascontiguousarray(f(w2_ffn1)[0].reshape(NFF, 128, D)),
        w1b=_up_layout(f(w1_ffn2)[0]), w3b=_up_layout(f(w3_ffn2)[0]), w2b=np.ascontiguousarray(f(w2_ffn2)[0].reshape(NFF, 128, D)),
        win=win_r,
        wout=np.ascontiguousarray(f(w_out)[0].reshape(4, 4, 128, 4, 512).transpose(3, 0, 2, 1, 4)),
        cwT=np.ascontiguousarray(f(conv_w)[0].reshape(4, 12, 128).transpose(2, 1, 0)),
        cbT=np.ascontiguousarray(f(conv_b)[0].reshape(12, 128).T),
        dcol=np.ascontiguousarray(np.repeat(f(d_skip)[0], 64).reshape(8, 128).T),
        gssd=np.ascontiguousarray(f(g_ssd)[0].reshape(8, 128).T),
        alog=f(a_log), dtb=f(dt_bias), relb=f(rel_bias), oh_d=_t5_onehot(),
    )
    ck = np.ascontiguousarray(f(cache_k)[0].reshape(5120 * 128, 256))
    cv = np.ascontiguousarray(f(cache_v)[0].reshape(5120 * 128, 256))
    cki = np.ascontiguousarray(f(cache_kidx)[0].reshape(5120 * 128, 64))
    ptab = np.asarray(page_table, dtype=np.int32)
    tt_ = np.arange(32)
    bm4 = (tt_[:, None] // 8 == np.arange(4)[None]).astype(np.float32)
    dq8 = (tt_[:, None] % 8 == np.arange(8)[None]).astype(np.float32)
    pen32 = np.where((tt_[:, None, None] // 8 == np.arange(4)[None, :, None]) & (tt_[:, None, None] % 8 <= np.arange(8)[None, None, :]), 0.0, -1e30).astype(np.float32)
    shared.update(kidx_tab=cki, kv_tab=np.concatenate([ck, cv], axis=1), bm4=bm4, dq8=dq8, pen32=pen32)
    sconv_all = f(state_conv)[0]
    sssm_all = f(state_ssm)[0]
    in_maps = []
    for c in range(8):
        b, half = c // 2, c % 2
        xin = np.zeros((NTT, 128, D), np.float32)
        xin[:NPT] = xp[b, half * 1024:(half + 1) * 1024].reshape(NPT, 128, D)
        xin[8, 0:32] = xs_[4 * c:4 * c + 4].reshape(32, D)
        if half == 1:
            xin[8, 32:35] = xp[b, 1021:1024]
        m = dict(shared)
        m["xin"] = xin
        m["pt4"] = np.ascontiguousarray(ptab[4 * c:4 * c + 4])
        m["xpre"] = np.ascontiguousarray(xp[b, 0:1024].reshape(NPT, 128, D)) if half == 1 else np.zeros((NPT, 128, D), np.float32)
        fl = np.zeros((128, 2), np.float32)
        fl[:, 0] = float(half)
        fl[:, 1] = (float(half) - 1.0) * 1e30
        m["flagc"] = fl
        m["sconvT"] = np.ascontiguousarray(sconv_all[4 * c:4 * c + 4].reshape(4, 3, 12, 128).transpose(3, 2, 0, 1))
        m["sssmT"] = np.ascontiguousarray(sssm_all[4 * c:4 * c + 4].reshape(4, 1024, 128).transpose(0, 2, 1))
        in_maps.append(m)
    nc = build()
    res = run_bass_kernel_spmd(nc, in_maps, core_ids=list(range(8))).results
    _DBG["res"] = res

    y_prompt = np.zeros((4, 2048, D), np.float32)
    y_sample = np.zeros((32, 8, D), np.float32)
    k_prompt = np.zeros((1, 4, 2048, 2, 128), np.float32)
    v_prompt = np.zeros_like(k_prompt)
    kidx_prompt = np.zeros((1, 4, 2048, 64), np.float32)
    conv_prompt = np.zeros((1, 4, 3, 1536), np.float32)
    ssm_prompt = np.zeros((1, 4, 16, 64, 128), np.float32)
    k_sample = np.zeros((1, 32, 8, 2, 128), np.float32)
    v_sample = np.zeros_like(k_sample)
    kidx_sample = np.zeros((1, 32, 8, 64), np.float32)
    conv_sample = np.zeros((1, 32, 3, 1536), np.float32)
    ssm_sample = np.zeros((1, 32, 16, 64, 128), np.float32)
    for c in range(8):
        b, half = c // 2, c % 2
        r = res[c]
        sl = slice(half * 1024, (half + 1) * 1024)
        yo = r["y_out"]
        y_prompt[b, sl] = yo[:NPT].reshape(1024, D)
        y_sample[4 * c:4 * c + 4] = yo[8, 0:32].reshape(4, 8, D)
        kv = r["kvs_out"]
        kvp = kv[:NPT].reshape(1024, 640)
        k_prompt[0, b, sl] = kvp[:, 0:256].reshape(1024, 2, 128)
        v_prompt[0, b, sl] = kvp[:, 256:512].reshape(1024, 2, 128)
        kidx_prompt[0, b, sl] = kvp[:, 512:576]
        kvs_ = kv[8, 0:32]
        k_sample[0, 4 * c:4 * c + 4] = kvs_[:, 0:256].reshape(4, 8, 2, 128)
        v_sample[0, 4 * c:4 * c + 4] = kvs_[:, 256:512].reshape(4, 8, 2, 128)
        kidx_sample[0, 4 * c:4 * c + 4] = kvs_[:, 512:576].reshape(4, 8, 64)
        so = r["ssm_out"].reshape(5, 128, 16, 64).transpose(0, 2, 3, 1)
        if half == 1:
            ssm_prompt[0, b] = so[0]
        ssm_sample[0, 4 * c:4 * c + 4] = so[1:5]
        co = r["conv_out"]
        if half == 1:
            conv_prompt[0, b] = co[0]
        conv_sample[0, 4 * c:4 * c + 4] = co[1:5]
    return (y_prompt, y_sample, k_prompt, v_prompt, kidx_prompt, conv_prompt, ssm_prompt,
            k_sample, v_sample, kidx_sample, conv_sample, ssm_sample)
```

```python
import numpy as np
import concourse.bass as bass
import concourse.mybir as mybir
from concourse.bass_utils import run_bass_kernel_spmd
from contextlib import ExitStack

F32 = mybir.dt.float32
BF16 = mybir.dt.bfloat16
I32 = mybir.dt.int32
AF = mybir.ActivationFunctionType
ALU = mybir.AluOpType
AX = mybir.AxisListType

D = 2048
DFF = 5632
NFF = DFF // 128
NPT = 8
NTT = 9
NTOK = 1024 + 35
TB = [(0, 512), (512, 512), (1024, 35)]
EPS = 1e-6
G = 4
NIN = 43


_DBG = {}


class Sched:
    ENGS = ("pe", "act", "dve", "pool", "sp")

    def __init__(self, nc):
        self.nc = nc
        self.ops = []
        self.last_w = {}
        self.readers = {}
        self.dma_cnt = {}
        self.last_eng = {}
        self.last_dma = {}
        self.last_bar = None
        self.batch_ends = {}

    def dma_batch_end(self, group):
        self.batch_ends.setdefault(group, []).append(self.dma_cnt.get(group, 0))

    def barrier(self, fn):
        deps = set(self.last_eng.values()) | set(self.last_dma.values())
        i = self.op("pool", fn, _extra=deps)
        self.last_bar = i
        return i

    def op(self, eng, fn, r=(), w=(), dma=None, _extra=()):
        i = len(self.ops)
        deps = set(_extra)
        if self.last_bar is not None:
            deps.add(self.last_bar)
        for k in list(r) + list(w):
            if k in self.last_w:
                deps.add(self.last_w[k])
        for k in w:
            lastc = {}
            for j in self.readers.get(k, ()):
                oj = self.ops[j]
                if oj["dma"] is None:
                    lastc[oj["eng"]] = max(lastc.get(oj["eng"], -1), j)
                else:
                    deps.add(j)
            deps.update(lastc.values())
        deps.discard(i)
        o = dict(eng=eng, fn=fn, deps=deps, dma=dma, sig=False, i=i)
        if dma is not None:
            self.dma_cnt[dma] = self.dma_cnt.get(dma, 0) + 1
            o["dcount"] = self.dma_cnt[dma]
            self.last_dma[dma] = i
        else:
            self.last_eng[eng] = i
        self.ops.append(o)
        for k in w:
            self.last_w[k] = i
            self.readers[k] = []
        for k in r:
            self.readers.setdefault(k, []).append(i)
        return i

    def emit(self, final_dma_groups=()):
        nc = self.nc
        ops = self.ops
        for o in ops:
            for d in o["deps"]:
                od = ops[d]
                if od["dma"] is None and od["eng"] == "pe" and o["eng"] == "pe" and o["dma"] is None:
                    continue
                od["sig"] = True
        cnt = {e: 0 for e in self.ENGS}
        for o in ops:
            if o["dma"] is None and o["sig"]:
                cnt[o["eng"]] += 1
                o["sval"] = cnt[o["eng"]]
        groups = sorted(self.dma_cnt.keys())
        with ExitStack() as es:
            esem = {e: es.enter_context(nc.semaphore("s_" + e)) for e in self.ENGS}
            dsem = {g: es.enter_context(nc.semaphore("d_" + str(g))) for g in groups}
            block = es.enter_context(nc.Block())
            per_eng = {e: [o for o in ops if o["eng"] == e] for e in self.ENGS}

            def run(engobj, ename):
                waited = {}
                for o in per_eng[ename]:
                    need = {}
                    for d in o["deps"]:
                        od = ops[d]
                        if od["dma"] is not None:
                            key = ("d", od["dma"])
                            dc = od["dcount"]
                            ends = [b for b in self.batch_ends.get(od["dma"], ()) if b >= dc]
                            val = 16 * (min(ends) if ends else dc)
                        else:
                            if od["eng"] == "pe" and ename == "pe" and o["dma"] is None:
                                continue
                            key = ("e", od["eng"])
                            val = od["sval"]
                        if need.get(key, 0) < val:
                            need[key] = val
                    for key, val in need.items():
                        if waited.get(key, 0) >= val:
                            continue
                        waited[key] = val
                        sem = dsem[key[1]] if key[0] == "d" else esem[key[1]]
                        engobj.wait_ge(sem, val)
                    ins = o["fn"](engobj)
                    if o["dma"] is not None:
                        ins.then_inc(dsem[o["dma"]], 16)
                    elif o["sig"]:
                        ins.then_inc(esem[ename], 1)
                if ename == "sp":
                    for g in final_dma_groups:
                        if g in dsem:
                            engobj.wait_ge(dsem[g], 16 * self.dma_cnt[g])

            block.tensor(lambda e: run(e, "pe"))
            block.scalar(lambda e: run(e, "act"))
            block.vector(lambda e: run(e, "dve"))
            block.gpsimd(lambda e: run(e, "pool"))
            block.sync(lambda e: run(e, "sp"))


class Arena:
    def __init__(self, nc, base=16512, top=229344):
        self.nc = nc
        self.base = base
        self.top = top
        self.n = 0

    def at(self, off, shape, dtype, name=None):
        self.n += 1
        nbytes = int(np.prod(shape[1:])) * (2 if dtype == BF16 else 4)
        assert self.base + off + nbytes <= self.top, (name, off, nbytes)
        return self.nc.alloc_sbuf_tensor_at(name or f"t{self.n}", list(shape), dtype, offset=self.base + off)


def build():
    nc = bass.Bass("TRN2", target_bir_lowering=False)
    S = Sched(nc)
    A = Arena(nc)

    def din(name, shape, dt=F32):
        return nc.dram_tensor(name, list(shape), dt, kind="ExternalInput").ap()

    def dout(name, shape, dt=F32):
        return nc.dram_tensor(name, list(shape), dt, kind="ExternalOutput").ap()

    xin = din("xin", [NTT, 128, D])
    xpre = din("xpre", [NPT, 128, D])
    flagc = din("flagc", [128, 2])
    gvec = din("gvec", [4, D])
    wf1 = [din("w1a", [NFF, 128, 16, 128]), din("w3a", [NFF, 128, 16, 128]), din("w2a", [NFF, 128, D])]
    wf2 = [din("w1b", [NFF, 128, 16, 128]), din("w3b", [NFF, 128, 16, 128]), din("w2b", [NFF, 128, D])]
    win = din("win", [NIN, 128, 16, 128])
    wout = din("wout", [4, 4, 128, 4, 512])
    sconvT = din("sconvT", [128, 12, 4, 3])
    sssmT = din("sssmT", [4, 128, 1024])
    cwT = din("cwT", [128, 12, 4])
    cbT = din("cbT", [128, 12])
    dcol = din("dcol", [128, 8])
    gssd = din("gssd", [128, 8])
    alog = din("alog", [1, 16])
    relb = din("relb", [32, 8])
    kidx_tab = din("kidx_tab", [5120 * 128, 64])
    kv_tab = din("kv_tab", [5120 * 128, 512])
    pt4 = din("pt4", [4, 128], I32)
    bm4 = din("bm4", [32, 4])
    dq8 = din("dq8", [32, 8])
    pen32 = din("pen32", [32, 4, 8])
    oh_d = din("oh_d", [32, 256])
    dtb = din("dtb", [1, 16])
    y_out = dout("y_out", [NTT, 128, D])
    kvs_out = dout("kvs_out", [NTT, 128, 640])
    conv_out = dout("conv_out", [5, 3, 1536])
    ssm_out = dout("ssm_out", [5, 128, 1024])
    xsp = nc.dram_tensor("xsp", [NTT, 128, D], F32).ap()
    pre_k = nc.dram_tensor("pre_k", [128, 2, 1024], BF16).ap()
    pre_v = nc.dram_tensor("pre_v", [128, 8, 258], BF16).ap()
    pre_ki = nc.dram_tensor("pre_ki", [128, 1024], BF16).ap()
    pre_h = nc.dram_tensor("pre_h", [128, 1024], F32).ap()
    pre_halo = nc.dram_tensor("pre_halo", [128, 36], F32).ap()
    bsc_t = nc.dram_tensor("bsc", [8, 128, 256], F32)
    bsc = bsc_t.ap()

    OX = 0
    OH = 73728
    OW = OH + 16 * NTOK * 2
    O_STAGE = OW
    O_WUP = O_STAGE + 3 * 8192
    O_W2G = O_WUP + 4 * 4096
    O_GT = O_W2G + 2 * G * D * 2
    GTB = G * NTOK * 2 + 8
    O_SIL = O_GT + 2 * GTB
    O_MISC = O_SIL + 2 * 2048
    xres = A.at(OX, [128, NTT, D], F32, "xres")
    hT = A.at(OH, [128, 16, NTOK], BF16, "hT")
    stage = [A.at(O_STAGE + i * 8192, [128, 16, 128], F32, f"stage{i}") for i in range(3)]
    wup = [A.at(O_WUP + i * 4096, [128, 16, 128], BF16, f"wup{i}") for i in range(4)]
    w2g = [A.at(O_W2G + i * G * D * 2, [128, G, D], BF16, f"w2g{i}") for i in range(2)]
    gT = [A.at(O_GT + i * GTB, [128, G, NTOK], BF16, f"gT{i}") for i in range(2)]
    sil = [A.at(O_SIL + i * 2048, [128, 512], F32, f"sil{i}") for i in range(2)]
    al = lambda v: (v + 31) // 32 * 32
    o = O_MISC
    ident_b = A.at(o, [128, 128], BF16, "ident_b"); o = al(o + 256)
    ident_f = A.at(o, [128, 128], F32, "ident_f"); o = al(o + 512)
    tri_f = A.at(o, [128, 128], F32, "tri_f"); o = al(o + 512)
    ones_f = A.at(o, [128, 128], F32, "ones_f"); o = al(o + 512)
    ones_b = A.at(o, [128, 128], BF16, "ones_b"); o = al(o + 256)
    stat = A.at(o, [128, 8], F32, "stat"); o = al(o + 32)
    flag_t = A.at(o, [128, 2], F32, "flag_t"); o = al(o + 8)
    cw_t = A.at(o, [128, 12, 4], F32, "cw_t"); o = al(o + 192)
    cb_t = A.at(o, [128, 12], F32, "cb_t"); o = al(o + 48)
    dcol_t = A.at(o, [128, 8], F32, "dcol_t"); o = al(o + 32)
    gssd_t = A.at(o, [128, 8], F32, "gssd_t"); o = al(o + 32)
    dtb_t = A.at(o, [128, 16], F32, "dtb_t"); o = al(o + 64)
    a_t = A.at(o, [128, 16], F32, "a_t"); o = al(o + 64)
    halo = A.at(o, [128, 12, 3], F32, "halo"); o = al(o + 144)
    sct = A.at(o, [128, 12, 4, 3], F32, "sct"); o = al(o + 576)
    tokst = [A.at(O_MISC - 2048 + i * 512, [128, 128], F32, f"tokst{i}") for i in range(2)]
    tails = [A.at(o + i * 2560, [3, 5, 128], F32, f"tails{i}") for i in range(1)]; o = al(o + 2560)
    gb = A.at(O_STAGE, [128, D], F32, "gb")
    xs = A.at(O_STAGE + 8192, [128, D], BF16, "xs")
    junk = A.at(O_STAGE + 8192 + 4096, [128, D], BF16, "junk")
    NTP = 1072
    NB = NTP * 2
    qT = A.at(OX, [128, 8, NTP], BF16, "qT")
    qiT = A.at(OX + 8 * NB, [128, 8, NTP], BF16, "qiT")
    szT = A.at(OX + 16 * NB, [128, 8, NTP], BF16, "szT")
    ssdT = szT
    attT = A.at(OX + 24 * NB, [128, 8, NTP], BF16, "attT")
    kT = A.at(OX + 32 * NB, [128, 2, NTP], BF16, "kT")
    o = O_W2G
    xcT = A.at(o, [128, 12, NTP], BF16, "xcT"); o = al(o + 12 * NB)
    vb1 = A.at(o, [128, NTT, 2, 129], BF16, "vb1"); o = al(o + NTT * 2 * 129 * 2 + 4)
    sm_tok = A.at(o, [128, NTT, 128], F32, "sm_tok"); o = al(o + NTT * 512)
    kiT2 = A.at(o, [128, NTOK + 1], BF16, "kiT2"); o = al(o + NB + 2)
    dtT = A.at(o, [16, NTOK], F32, "dtT"); o = al(o + NTOK * 4)
    rawx = A.at(o, [128, NTOK + 3], F32, "rawx"); o = al(o + (NTOK + 3) * 4)
    cacc = A.at(o, [128, 1024], F32, "cacc"); o = al(o + 4096)
    exts = A.at(o, [128, 4, 11], F32, "exts"); o = al(o + 176)
    assert o <= O_MISC - 2048, o - O_MISC
    o = OH
    kT_pre = A.at(o, [128, 2, 1024], BF16, "kT_pre"); o = al(o + 4096)
    vb1_pre = A.at(o, [128, 8, 2, 129], BF16, "vb1_pre"); o = al(o + 4128)
    kiT2_pre = A.at(o, [128, 1024], BF16, "kiT2_pre"); o = al(o + 2048)
    o = al(o + 16384)
    Hst = A.at(o, [128, 1024], F32, "Hst"); o = al(o + 4096)
    Hb = A.at(o, [128, 1024], BF16, "Hb"); o = al(o + 2048)
    assert o <= OW
    o = O_STAGE
    ysb = A.at(o, [128, 1024], F32, "ysb"); o = al(o + 4096)
    xd = A.at(o, [128, 1024], BF16, "xd"); o = al(o + 2048)
    xdd = A.at(o, [128, 1024], BF16, "xdd"); o = al(o + 2048)
    Btok = A.at(o, [128, 256], BF16, "Btok"); o = al(o + 512)
    Dm = A.at(o, [128, 8, 128], F32, "Dm"); o = al(o + 4096)
    CBm = A.at(o, [128, 2, 128], F32, "CBm"); o = al(o + 1024)
    t1 = A.at(o, [128, 128], F32, "t1"); o = al(o + 512)
    t2 = A.at(o, [128, 128], F32, "t2"); o = al(o + 512)
    Mh = A.at(o, [128, 128], BF16, "Mh"); o = al(o + 256)
    sm16 = [A.at(o + i * 64, [128, 16], F32, f"sm16_{i}") for i in range(8)]; o = al(o + 512)
    gbuf = A.at(o, [128, 8, 128], F32, "gbuf"); o = al(o + 4096)
    sq = A.at(o, [128, 8, 128], BF16, "sq"); o = al(o + 2048)
    rs = A.at(o, [128, 2, 128], F32, "rs"); o = al(o + 1024)
    assert o <= O_W2G

    psall = nc.alloc_psum_tensor("psall", [128, 4096], F32)
    ps = [psall[:, i * 512:(i + 1) * 512] for i in range(8)]

    import os
    KSTOP = int(os.environ.get("KSTOP", "99"))
    KSKIP = os.environ.get("KSKIP", "")
    nbar = [0]

    class _Stop(Exception):
        pass

    def bar():
        S.barrier(lambda e: e.memset(stat[:, 7:8], 0.0))
        nbar[0] += 1
        if nbar[0] >= KSTOP:
            raise _Stop()

    for t, nm in ((ident_b, "ident_b"), (ident_f, "ident_f")):
        S.op("pool", lambda e, t=t: e.memset(t[:], 1.0), w=[nm])
        S.op("pool", lambda e, t=t: e.affine_select(out=t[:], in_=t[:], pattern=[[-1, 128]], compare_op=ALU.is_equal,
                                                     fill=0.0, base=0, channel_multiplier=1), r=[nm], w=[nm])
    S.op("pool", lambda e: e.memset(tri_f[:], 1.0), w=["tri_f"])
    S.op("pool", lambda e: e.affine_select(out=tri_f[:], in_=tri_f[:], pattern=[[1, 128]], compare_op=ALU.is_ge,
                                           fill=0.0, base=0, channel_multiplier=-1), r=["tri_f"], w=["tri_f"])
    S.op("pool", lambda e: e.memset(ones_f[:], 1.0), w=["ones_f"])
    S.op("pool", lambda e: e.memset(ones_b[:], 1.0), w=["ones_b"])
    for i, (dst, src) in enumerate(((flag_t[:], flagc[:, :]), (cw_t[:], cwT[:, :, :]), (cb_t[:], cbT[:, :]), (dcol_t[:], dcol[:, :]),
                                    (gssd_t[:], gssd[:, :]), (sct[:], sconvT[:, :, :, :]),
                                    (dtb_t[:], dtb[0:1, :].partition_broadcast(128)), (a_t[:], alog[0:1, :].partition_broadcast(128)))):
        S.op("sp", lambda e, dst=dst, src=src: e.dma_start(out=dst, in_=src), w=[("cst", i)], dma="cst")
    S.dma_batch_end("cst")
    S.op("act", lambda e: e.activation(out=a_t[:], in_=a_t[:], func=AF.Exp), r=[("cst", 7)], w=[("cst", 7)])
    S.op("dve", lambda e: e.tensor_scalar(out=a_t[:], in0=a_t[:], scalar1=-1.0, scalar2=None, op0=ALU.mult), r=[("cst", 7)], w=[("cst", 7)])

    def load_x(src, ntiles):
        for t in range(ntiles):
            S.op("sp", lambda e, t=t: e.dma_start(out=xres[:, t, :], in_=src[t, :, :]), w=[("x", t)], dma="xld")
        S.dma_batch_end("xld")

    def norm_to_hT(gi, ntiles):
        S.op("sp", lambda e: e.dma_start(out=gb[:], in_=gvec[gi:gi + 1, :].partition_broadcast(128)),
             r=["stage0"], w=["gb", "stage0"], dma="gld")
        for t in range(ntiles):
            ncol = 128 if t < NPT else 35
            S.op("act", lambda e, t=t: e.activation(out=junk[:], in_=xres[:, t, :], func=AF.Square, accum_out=stat[:, 0:1]),
                 r=[("x", t)], w=["junk", "ss", "stage1"])
            S.op("dve", lambda e: e.tensor_scalar(out=stat[:, 1:2], in0=stat[:, 0:1], scalar1=1.0 / D, scalar2=EPS,
                                                  op0=ALU.mult, op1=ALU.add), r=["ss"], w=["ms"])
            S.op("act", lambda e: e.activation(out=stat[:, 2:3], in_=stat[:, 1:2], func=AF.Sqrt), r=["ms"], w=["sd"])
            S.op("dve", lambda e: e.reciprocal(out=stat[:, 3:4], in_=stat[:, 2:3]), r=["sd"], w=["rstd"])
            S.op("dve", lambda e, t=t: e.scalar_tensor_tensor(out=xs[:], in0=xres[:, t, :], scalar=stat[:, 3:4], in1=gb[:],
                                                              op0=ALU.mult, op1=ALU.mult),
                 r=[("x", t), "rstd", "gb", "stage0"], w=["xs", "stage1"])
            for half in range(2):
                pb = ps[2 * (t % 2) + half]
                pbv = pb.bitcast(BF16)
                pk = ("ps", 2 * (t % 2) + half)
                for j in range(8):
                    kc = half * 8 + j
                    S.op("pe", lambda e, pbv=pbv, j=j, kc=kc: e.transpose(out=pbv[:, j * 128:(j + 1) * 128],
                                                                          in_=xs[:, kc * 128:(kc + 1) * 128], identity=ident_b[:]),
                         r=["xs", "stage1", "ident_b"], w=[pk])
                tbk = ("hT", 0 if t < 4 else (1 if t < 8 else 2))
                c0 = t * 128
                if half == 0:
                    S.op("act", lambda e, pbv=pbv, half=half, c0=c0, ncol=ncol: e.activation(
                        out=hT[:, half * 8:(half + 1) * 8, c0:c0 + ncol],
                        in_=pbv.rearrange("p (j c) -> p j c", j=8)[:, :, 0:ncol], func=AF.Copy), r=[pk], w=[tbk])
                else:
                    S.op("dve", lambda e, pbv=pbv, half=half, c0=c0, ncol=ncol: e.tensor_copy(
                        out=hT[:, half * 8:(half + 1) * 8, c0:c0 + ncol],
                        in_=pbv.rearrange("p (j c) -> p j c", j=8)[:, :, 0:ncol]), r=[pk], w=[tbk])

    wctr = [0]
    suse = [0, 0, 0]

    def stage_slot():
        i = wctr[0]
        wctr[0] += 1
        k = i % 3
        suse[k] += 1
        return stage[k], f"stage{k}", f"stage{k}_{suse[k] // 100}", i

    def load_up_tile(src_ap, cast_eng):
        st, sk, sg, i = stage_slot()
        wb = wup[i % 4]
        wk = f"wup{i % 4}"
        S.op("sp", lambda e: e.dma_start(out=st[:], in_=src_ap), w=[sk], dma=sg)
        if cast_eng == "act":
            S.op("act", lambda e: e.activation(out=wb[:], in_=st[:], func=AF.Copy), r=[sk], w=[wk])
        else:
            S.op(cast_eng, lambda e: e.tensor_copy(out=wb[:], in_=st[:]), r=[sk], w=[wk])
        return wb, wk

    def up_mm(wb, wk, tb, pbank, pkey):
        c0, n = TB[tb]
        for kc in range(16):
            S.op("pe", lambda e, kc=kc: e.matmul(pbank[:, 0:n], lhsT=wb[:, kc, :], rhs=hT[:, kc, c0:c0 + n],
                                                 start=(kc == 0), stop=(kc == 15)),
                 r=[wk, ("hT", tb)], w=[pkey])

    def ffn(w, ntiles):
        w1, w3, w2 = w
        ntb = 3 if ntiles == NTT else 2
        pctr = 0
        for grp in range(NFF // G):
            gt = gT[grp % 2]
            w2t = w2g[grp % 2]
            for j in range(G):
                fc = grp * G + j
                wb1, wk1 = load_up_tile(w1[fc], "pool")
                wb3, wk3 = load_up_tile(w3[fc], "dve")
                st, sk, sg, i = stage_slot()
                S.op("sp", lambda e, st=st, fc=fc: e.dma_start(out=st[:].rearrange("p a b -> p (a b)"), in_=w2[fc]),
                     w=[sk], dma=sg)
                S.op("act", lambda e, st=st, w2t=w2t, j=j: e.activation(out=w2t[:, j, :], in_=st[:].rearrange("p a b -> p (a b)"),
                                                                        func=AF.Copy), r=[sk], w=[("w2g", grp % 2, j)])
                for tb in range(ntb):
                    c0, n = TB[tb]
                    pa, pbk = 2 * (pctr % 3), 2 * (pctr % 3) + 1
                    pctr += 1
                    up_mm(wb1, wk1, tb, ps[pa], ("ps", pa))
                    up_mm(wb3, wk3, tb, ps[pbk], ("ps", pbk))
                    sl = sil[pctr % 2]
                    slk = ("sil", pctr % 2)
                    S.op("act", lambda e, sl=sl, pa=pa, n=n: e.activation(out=sl[:, 0:n], in_=ps[pa][:, 0:n], func=AF.Silu),
                         r=[("ps", pa)], w=[slk])
                    S.op("dve", lambda e, sl=sl, pbk=pbk, n=n, c0=c0, gt=gt, j=j: e.tensor_tensor(
                        out=gt[:, j, c0:c0 + n], in0=sl[:, 0:n], in1=ps[pbk][:, 0:n], op=ALU.mult),
                         r=[slk, ("ps", pbk)], w=[("gT", grp % 2, j, tb)])
            for t in range(ntiles):
                m = 128 if t < NPT else 35
                tb = 0 if t < 4 else (1 if t < 8 else 2)
                for nb in range(4):
                    pd = 6 + (t * 4 + nb) % 2
                    for j in range(G):
                        S.op("pe", lambda e, t=t, j=j, nb=nb, pd=pd, m=m, gt=gt, w2t=w2t: e.matmul(
                            ps[pd][0:m, :], lhsT=gt[:, j, t * 128:t * 128 + m], rhs=w2t[:, j, nb * 512:(nb + 1) * 512],
                            start=(j == 0), stop=(j == G - 1)),
                             r=[("gT", grp % 2, j, tb), ("w2g", grp % 2, j)], w=[("ps", pd)])
                    S.op("dve", lambda e, t=t, nb=nb, pd=pd, m=m: e.scalar_tensor_tensor(
                        out=xres[0:m, t, nb * 512:(nb + 1) * 512], in0=ps[pd][0:m, :], scalar=0.5,
                        in1=xres[0:m, t, nb * 512:(nb + 1) * 512], op0=ALU.mult, op1=ALU.add),
                         r=[("ps", pd), ("x", t)], w=[("x", t)])

    QSCALE = 128.0 ** -0.5

    def inproj(main):
        ntiles = NTT if main else NPT
        ntb = 3 if main else 2
        chunks = list(range(NIN)) if main else [8, 9, 10, 11] + list(range(28, 40)) + [41, 42]
        if main and "q" in KSKIP:
            chunks = [c for c in chunks if not (c < 8 or 12 <= c < 28)]
        pctr = 0
        tctr = [0]
        if main:
            S.op("sp", lambda e: e.dma_start(out=halo[:], in_=pre_halo.rearrange("p (a b) -> p a b", b=3)),
                 w=["halo"], dma="pre")
            S.dma_batch_end("pre")
        else:
            S.op("pool", lambda e: e.memset(halo[:], 0.0), w=["halo"])
        S.op("pool", lambda e: e.memset(vb1[:, :, :, 128:129], 1.0), w=["vb1ones"])
        for c in chunks:
            wb, wk = load_up_tile(win[c], "pool" if c % 2 == 0 else "dve")
            is_q, is_k, is_v = c < 8, 8 <= c < 10, 10 <= c < 12
            is_qi, is_z, is_xbc = 12 <= c < 20, 20 <= c < 28, 28 <= c < 40
            is_sm, is_ki2, is_aux = c == 40, c == 41, c == 42
            if not is_v and not is_sm:
                for tb in range(ntb):
                    c0, n = TB[tb]
                    pa = pctr % 6
                    pctr += 1
                    up_mm(wb, wk, tb, ps[pa], ("ps", pa))
                    src = ps[pa][:, 0:n]
                    if is_q:
                        S.op("act", lambda e, src=src, c=c, c0=c0, n=n: e.activation(out=qT[:, c, c0:c0 + n], in_=src, func=AF.Copy, scale=QSCALE),
                             r=[("ps", pa)], w=[("qT", c, tb)])
                    elif is_k:
                        S.op("act", lambda e, src=src, c=c, c0=c0, n=n: e.activation(out=kT[:, c - 8, c0:c0 + n], in_=src, func=AF.Copy),
                             r=[("ps", pa)], w=[("kT", c - 8, tb)])
                    elif is_qi:
                        S.op("act", lambda e, src=src, c=c, c0=c0, n=n: e.activation(out=qiT[:, c - 12, c0:c0 + n], in_=src, func=AF.Copy),
                             r=[("ps", pa)], w=[("qiT", c - 12, tb)])
                    elif is_z:
                        S.op("act", lambda e, src=src, c=c, c0=c0, n=n: e.activation(out=szT[:, c - 20, c0:c0 + n], in_=src, func=AF.Silu),
                             r=[("ps", pa)], w=[("szT", c - 20, tb)])
                    elif is_ki2:
                        S.op("act", lambda e, src=src, c0=c0, n=n: e.activation(out=kiT2[:, c0:c0 + n], in_=src, func=AF.Copy),
                             r=[("ps", pa)], w=[("kiT2", tb)])
                    elif is_aux:
                        S.op("act", lambda e, pa=pa, c0=c0, n=n: e.activation(out=dtT[0:16, c0:c0 + n], in_=ps[pa][0:16, 0:n], func=AF.Copy),
                             r=[("ps", pa)], w=[("dtT", tb)])
                    elif is_xbc:
                        S.op("act", lambda e, src=src, c0=c0, n=n: e.activation(out=rawx[:, 3 + c0:3 + c0 + n], in_=src, func=AF.Copy),
                             r=[("ps", pa)], w=[("rawx", tb)])
            if is_xbc:
                xc = c - 28
                rk = [("rawx", tb) for tb in range(ntb)]
                S.op("dve", lambda e, xc=xc: e.tensor_copy(out=rawx[:, 0:3], in_=halo[:, xc, :]), r=["halo"], w=["rawxh"])
                if not main:
                    S.op("dve", lambda e, xc=xc: e.tensor_copy(out=halo[:, xc, :], in_=rawx[:, 3 + 1021:3 + 1024]),
                         r=rk + ["rawxh"], w=["halo"])
                S.op("dve", lambda e, xc=xc: e.tensor_scalar(out=cacc[:], in0=rawx[:, 0:1024], scalar1=cw_t[:, xc, 0:1], scalar2=None,
                                                             op0=ALU.mult), r=rk + ["rawxh", ("cst", 1)], w=["cacc"])
                for k in range(1, 4):
                    S.op("dve", lambda e, xc=xc, k=k: e.scalar_tensor_tensor(out=cacc[:], in0=rawx[:, k:k + 1024], scalar=cw_t[:, xc, k:k + 1],
                                                                            in1=cacc[:], op0=ALU.mult, op1=ALU.add),
                         r=rk + ["rawxh"], w=["cacc"])
                S.op("act", lambda e, xc=xc: e.activation(out=xcT[:, xc, 0:1024], in_=cacc[:], func=AF.Silu, bias=cb_t[:, xc:xc + 1]),
                     r=["cacc", ("cst", 2)], w=[("xcT", xc)])
                if main and "s" not in KSKIP:
                    S.op("dve", lambda e, xc=xc: e.tensor_copy(out=exts[:, :, 0:3], in_=sct[:, xc, :, :]), r=[("cst", 5)], w=["exts"])
                    S.op("dve", lambda e: e.tensor_copy(out=exts[:, :, 3:11], in_=rawx[:, 3 + 1024:3 + 1056].rearrange("p (b t) -> p b t", t=8)),
                         r=rk, w=["exts"])
                    S.op("dve", lambda e, xc=xc: e.tensor_scalar(out=cacc[:, 0:32].rearrange("p (b t) -> p b t", t=8), in0=exts[:, :, 0:8],
                                                                 scalar1=cw_t[:, xc, 0:1], scalar2=None, op0=ALU.mult),
                         r=["exts", ("xcT", xc)], w=["cacc"])
                    for k in range(1, 4):
                        S.op("dve", lambda e, xc=xc, k=k: e.scalar_tensor_tensor(
                            out=cacc[:, 0:32].rearrange("p (b t) -> p b t", t=8), in0=exts[:, :, k:k + 8], scalar=cw_t[:, xc, k:k + 1],
                            in1=cacc[:, 0:32].rearrange("p (b t) -> p b t", t=8), op0=ALU.mult, op1=ALU.add), r=["exts"], w=["cacc"])
                    S.op("act", lambda e, xc=xc: e.activation(out=xcT[:, xc, 1024:1056], in_=cacc[:, 0:32], func=AF.Silu, bias=cb_t[:, xc:xc + 1]),
                         r=["cacc"], w=[("xcT", xc)])
                if main and "t" not in KSKIP:
                    srcs = [1021] + [1024 + b * 8 + 5 for b in range(4)]
                    for si, s0 in enumerate(srcs):
                        pb, off = (7, si * 128) if si < 4 else (6, 0)
                        S.op("pe", lambda e, s0=s0, pb=pb, off=off: e.transpose(out=ps[pb][0:3, off:off + 128],
                                                                              in_=rawx[:, 3 + s0:3 + s0 + 3], identity=ident_f[:]),
                             r=rk + ["ident_f"], w=[("ps", pb)])
                    tl = tails[0]
                    tlk = ("tails", 0)
                    S.op("act", lambda e, tl=tl: e.activation(out=tl[0:3, 0:4, :], in_=ps[7][0:3, 0:512].rearrange("p (s c) -> p s c", s=4),
                                                              func=AF.Copy), r=[("ps", 7)], w=[tlk])
                    S.op("act", lambda e, tl=tl: e.activation(out=tl[0:3, 4, :], in_=ps[6][0:3, 0:128], func=AF.Copy), r=[("ps", 6)], w=[tlk])
                    S.op("sp", lambda e, tl=tl, xc=xc: e.dma_start(out=conv_out[:, :, xc * 128:(xc + 1) * 128].rearrange("s r c -> r s c"),
                                                                   in_=tl[0:3, :, :]), r=[tlk], w=[("conv_out", xc)], dma="tl0")
            if is_k or is_v or is_sm:
                if is_k and not main:
                    continue
                col = (c - 8) * 128 if (is_k or is_v) else 512
                for t in range(ntiles):
                    m = 128 if t < NPT else 35
                    tb = 0 if t < 4 else (1 if t < 8 else 2)
                    pd = 6 + t % 2
                    for kc in range(16):
                        S.op("pe", lambda e, t=t, kc=kc, pd=pd, m=m, wb=wb: e.matmul(
                            ps[pd][0:m, 0:128], lhsT=hT[:, kc, t * 128:t * 128 + m], rhs=wb[:, kc, :],
                            start=(kc == 0), stop=(kc == 15)), r=[wk, ("hT", tb)], w=[("ps", pd)])
                    if is_v:
                        S.op("dve", lambda e, t=t, pd=pd, c=c: e.tensor_copy(out=vb1[:, t, c - 10, 0:128], in_=ps[pd][:, 0:128]),
                             r=[("ps", pd)], w=[("vb1", t)])
                    if is_sm:
                        S.op("dve", lambda e, t=t, pd=pd: e.tensor_copy(out=sm_tok[:, t, :], in_=ps[pd][:, 0:128]),
                             r=[("ps", pd)], w=[("sm_tok", t)])
                    if main and "o" not in KSKIP:
                        ti = tctr[0] % 2
                        tctr[0] += 1
                        if "e" not in KSKIP:
                            S.op("dve", lambda e, ti=ti, pd=pd: e.tensor_copy(out=tokst[ti][:], in_=ps[pd][:, 0:128]),
                                 r=[("ps", pd)], w=[("tokst", ti)])
                        if "d" not in KSKIP:
                          S.op(os.environ.get("KTOKQ", "sp"), lambda e, t=t, ti=ti, col=col: e.dma_start(out=kvs_out[t, :, col:col + 128], in_=tokst[ti][:]),
                             r=[("tokst", ti)], w=[("kvs_out", t, col)], dma=f"tok{ti}")

    dtk, dta, acum, ea, dec, eal, scd, tmp16 = sm16

    def ssd_chunk(c0, L, full):
        cs0, cs1 = c0, c0 + L
        K = ["ssd"]

        def Q(eng, fn):
            S.op(eng, fn, w=K)

        Q("pe", lambda e: e.transpose(out=ps[6][0:L, 0:16], in_=dtT[0:16, cs0:cs1], identity=ident_f[0:16, 0:16]))
        Q("dve", lambda e: e.tensor_tensor(out=tmp16[0:L, :], in0=ps[6][0:L, 0:16], in1=dtb_t[0:L, :], op=ALU.add))
        Q("act", lambda e: e.activation(out=tmp16[0:L, :], in_=tmp16[0:L, :], func=AF.Exp))
        Q("act", lambda e: e.activation(out=dtk[0:L, :], in_=tmp16[0:L, :], func=AF.Ln, bias=1.0))
        Q("dve", lambda e: e.tensor_tensor(out=dta[0:L, :], in0=dtk[0:L, :], in1=a_t[0:L, :], op=ALU.mult))
        Q("pe", lambda e: e.matmul(ps[6][0:L, 16:32], lhsT=tri_f[0:L, 0:L], rhs=dta[0:L, :], start=True, stop=True))
        Q("pe", lambda e: e.matmul(ps[6][:, 32:48], lhsT=ones_f[0:L, :], rhs=dta[0:L, :], start=True, stop=True))
        Q("dve", lambda e: e.tensor_copy(out=acum[0:L, :], in_=ps[6][0:L, 16:32]))
        Q("act", lambda e: e.activation(out=ea[0:L, :], in_=acum[0:L, :], func=AF.Exp))
        Q("dve", lambda e: e.tensor_tensor(out=dec[0:L, :], in0=ps[6][0:L, 32:48], in1=acum[0:L, :], op=ALU.subtract))
        Q("act", lambda e: e.activation(out=dec[0:L, :], in_=dec[0:L, :], func=AF.Exp))
        Q("act", lambda e: e.activation(out=eal[:, :], in_=ps[6][:, 32:48], func=AF.Exp))
        Q("dve", lambda e: e.tensor_tensor(out=scd[0:L, :], in0=dtk[0:L, :], in1=dec[0:L, :], op=ALU.mult))
        pv = [ps[0].bitcast(BF16), ps[1].bitcast(BF16)]
        for j in range(10):
            dst = pv[0][0:L, j * 128:(j + 1) * 128] if j < 8 else pv[1][0:L, (j - 8) * 128:(j - 7) * 128]
            Q("pe", lambda e, j=j, dst=dst: e.transpose(out=dst, in_=xcT[:, j, cs0:cs1], identity=ident_b[:]))
        xv = pv[0][0:L, 0:1024].rearrange("p (h q) -> p h q", q=64)
        Q("dve", lambda e: e.tensor_tensor(out=xd[0:L, :].rearrange("p (h q) -> p h q", q=64), in0=xv,
                                           in1=dtk[0:L, :].rearrange("p (h o) -> p h o", o=1).to_broadcast([L, 16, 64]), op=ALU.mult))
        Q("dve", lambda e: e.tensor_tensor(out=xdd[0:L, :].rearrange("p (h q) -> p h q", q=64), in0=xv,
                                           in1=scd[0:L, :].rearrange("p (h o) -> p h o", o=1).to_broadcast([L, 16, 64]), op=ALU.mult))
        Q("act", lambda e: e.activation(out=Btok[0:L, :], in_=pv[1][0:L, 0:256], func=AF.Copy))
        if full:
            for g in range(2):
                Q("pe", lambda e, g=g: e.matmul(ps[6][0:L, 128 + g * 128:128 + g * 128 + L], lhsT=xcT[:, 8 + g, cs0:cs1],
                                               rhs=xcT[:, 10 + g, cs0:cs1], start=True, stop=True))
                Q("dve", lambda e, g=g: e.tensor_tensor(out=CBm[0:L, g, 0:L], in0=ps[6][0:L, 128 + g * 128:128 + g * 128 + L],
                                                        in1=tri_f[0:L, 0:L], op=ALU.mult))
            for half in range(2):
                Q("dve", lambda e, half=half: e.tensor_tensor(
                    out=Dm[0:L, :, 0:L], in0=ident_f[0:L, 0:L].rearrange("p (o i) -> p o i", o=1).to_broadcast([L, 8, L]),
                    in1=acum[0:L, half * 8:(half + 1) * 8].rearrange("p (h o) -> p h o", o=1).to_broadcast([L, 8, L]), op=ALU.mult))
                for q4 in range(2):
                    Q("pe", lambda e, q4=q4: e.matmul(ps[2 + q4][0:L, 0:4 * L].rearrange("p (h i) -> p h i", h=4), lhsT=ones_f[0:L, 0:L],
                                                     rhs=Dm[0:L, 4 * q4:4 * q4 + 4, 0:L], start=True, stop=True))
                for hh in range(8):
                    h = half * 8 + hh
                    bsrc = ps[2 + hh // 4][0:L, (hh % 4) * L:(hh % 4 + 1) * L]
                    Q("dve", lambda e, bsrc=bsrc, h=h: e.tensor_scalar(out=t1[0:L, 0:L], in0=bsrc, scalar1=acum[0:L, h:h + 1], scalar2=0.0,
                                                                       op0=ALU.subtract, op1=ALU.min))
                    Q("act", lambda e: e.activation(out=t2[0:L, 0:L], in_=t1[0:L, 0:L], func=AF.Exp))
                    Q("dve", lambda e, h=h: e.tensor_tensor(out=Mh[0:L, 0:L], in0=t2[0:L, 0:L], in1=CBm[0:L, h // 8, 0:L], op=ALU.mult))
                    Q("pe", lambda e, h=h: e.matmul(ps[h // 8][0:L, (h % 8) * 64:(h % 8 + 1) * 64], lhsT=Mh[0:L, 0:L],
                                                   rhs=xd[0:L, h * 64:(h + 1) * 64], start=True, stop=True))
            for g in range(2):
                Q("pe", lambda e, g=g: e.matmul(ps[4 + g][0:L, :], lhsT=xcT[:, 10 + g, cs0:cs1], rhs=Hb[:, g * 512:(g + 1) * 512],
                                               start=True, stop=True))
                Q("act", lambda e, g=g: e.activation(out=ysb[0:L, g * 512:(g + 1) * 512], in_=ps[g][0:L, :], func=AF.Copy))
            for h in range(16):
                Q("dve", lambda e, h=h: e.scalar_tensor_tensor(out=ysb[0:L, h * 64:(h + 1) * 64], in0=ps[4 + h // 8][0:L, (h % 8) * 64:(h % 8 + 1) * 64],
                                                               scalar=ea[0:L, h:h + 1], in1=ysb[0:L, h * 64:(h + 1) * 64], op0=ALU.mult, op1=ALU.add))
        for g in range(2):
            Q("pe", lambda e, g=g: e.matmul(ps[2 + g][:, :], lhsT=Btok[0:L, g * 128:(g + 1) * 128], rhs=xdd[0:L, g * 512:(g + 1) * 512],
                                           start=True, stop=True))
        Q("dve", lambda e: e.tensor_tensor(out=Hst[:].rearrange("p (h q) -> p h q", q=64), in0=Hst[:].rearrange("p (h q) -> p h q", q=64),
                                           in1=eal[:, :].rearrange("p (h o) -> p h o", o=1).to_broadcast([128, 16, 64]), op=ALU.mult))
        for g in range(2):
            Q("dve", lambda e, g=g: e.tensor_tensor(out=Hst[:, g * 512:(g + 1) * 512], in0=Hst[:, g * 512:(g + 1) * 512], in1=ps[2 + g][:, :],
                                                    op=ALU.add))
        Q("act", lambda e: e.activation(out=Hb[:], in_=Hst[:], func=AF.Copy))
        if full:
            for j in range(8):
                Q("pe", lambda e, j=j: e.transpose(out=ps[4 + j // 4][:, (j % 4) * L:(j % 4 + 1) * L], in_=ysb[0:L, j * 128:(j + 1) * 128],
                                                  identity=ident_f[0:L, 0:L]))
            for j in range(8):
                ysrc = ps[4 + j // 4][:, (j % 4) * L:(j % 4 + 1) * L]
                Q("dve", lambda e, j=j, ysrc=ysrc: e.scalar_tensor_tensor(out=gbuf[:, j, 0:L], in0=xcT[:, j, cs0:cs1], scalar=dcol_t[:, j:j + 1],
                                                                          in1=ysrc, op0=ALU.mult, op1=ALU.add))
                Q("dve", lambda e, j=j: e.tensor_tensor(out=gbuf[:, j, 0:L], in0=gbuf[:, j, 0:L], in1=szT[:, j, cs0:cs1], op=ALU.mult))
                Q("act", lambda e, j=j: e.activation(out=sq[:, j, 0:L], in_=gbuf[:, j, 0:L], func=AF.Square))
            for g in range(2):
                for jj in range(4):
                    Q("pe", lambda e, g=g, jj=jj: e.matmul(ps[7][:, g * 128:g * 128 + L], lhsT=ones_b[:, :], rhs=sq[:, 4 * g + jj, 0:L],
                                                          start=(jj == 0), stop=(jj == 3)))
                Q("dve", lambda e, g=g: e.tensor_scalar(out=rs[:, g, 0:L], in0=ps[7][:, g * 128:g * 128 + L], scalar1=1.0 / 512, scalar2=EPS,
                                                        op0=ALU.mult, op1=ALU.add))
                Q("act", lambda e, g=g: e.activation(out=rs[:, g, 0:L], in_=rs[:, g, 0:L], func=AF.Sqrt))
                Q("dve", lambda e, g=g: e.reciprocal(out=rs[:, g, 0:L], in_=rs[:, g, 0:L]))
            for j in range(8):
                Q("dve", lambda e, j=j: e.scalar_tensor_tensor(out=ssdT[:, j, cs0:cs1], in0=gbuf[:, j, 0:L], scalar=gssd_t[:, j:j + 1],
                                                               in1=rs[:, j // 4, 0:L], op0=ALU.mult, op1=ALU.mult))


    sc = A.at(OH + 10272, [128, 2048], F32, "sc")
    mk = A.at(OH + 10272 + 8192, [128, 2048], BF16, "mk")
    mkT = A.at(OH + 10272 + 12288, [128, 16, 128], BF16, "mkT")
    o = O_STAGE
    bT = A.at(o, [128, 2, 8, 128], F32, "bT"); o += 8192
    rtmp = A.at(o, [128, 2048], F32, "rtmp"); o += 8192
    pexp = A.at(o, [128, 512], F32, "pexp"); o += 2048
    pm = A.at(o, [128, 512], BF16, "pm"); o += 1024
    atok = A.at(o, [128, 1024], BF16, "atok"); o += 2048
    hw = A.at(o, [128, 32], F32, "hw"); o += 128
    p2 = A.at(o, [128, 32], F32, "p2"); o += 128
    wis = A.at(o, [128, 16], F32, "wis"); o += 64
    cfar = A.at(o, [128, 8], F32, "cfar"); o += 32
    rden = A.at(o, [128, 8], F32, "rden"); o += 32
    a1 = A.at(o, [128, 8], F32, "a1"); o += 32
    rb_t = A.at(o, [32, 8], F32, "rb_t"); o += 32
    oh_t = A.at(o, [32, 256], F32, "oh_t"); o += 1024
    rbb = A.at(o, [32, 8, 128], F32, "rbb"); o += 4096
    cfT = A.at(o, [128, 8, 128], F32, "cfT"); o += 4096
    rtmp2 = A.at(o, [128, 2048], F32, "rtmp2"); o += 8192
    assert o <= O_W2G
    NIT = 24

    atn = [0]

    def AQ(eng, fn, dma=None, r=(), w=()):
        if dma is not None:
            atn[0] += 1
            dma = f"at{atn[0] // 100}"
        S.op(eng, fn, r=list(r), w=["att"] + list(w), dma=dma)

    gcn = [0]

    def gather(dst, table, idx_t, j, key):
        gcn[0] += 1
        S.op("pool", lambda e: e.indirect_dma_start(out=dst, out_offset=None, in_=table[:, :],
                                                    in_offset=bass.IndirectOffsetOnAxis(ap=idx_t[:, j:j + 1], axis=0)),
             r=["att_idx"], w=["gch", key], dma=f"ag{gcn[0] // 100}")

    def att_setup():
        AQ("sp", lambda e: e.dma_start(out=rb_t[:], in_=relb[:, :]), dma="at")
        AQ("sp", lambda e: e.dma_start(out=oh_t[:], in_=oh_d[:, :]), dma="at")
        AQ("sp", lambda e: e.dma_start(out=cfar[:], in_=relb[31:32, :].partition_broadcast(128)), dma="at")
        AQ("dve", lambda e: e.tensor_copy(out=rbb[:], in_=rb_t[:].rearrange("p (h o) -> p h o", o=1).to_broadcast([32, 8, 128])))
        for h in range(8):
            AQ("pe", lambda e, h=h: e.matmul(ps[0][:, 0:256], lhsT=rbb[:, h, :], rhs=oh_t[:, :], start=True, stop=True))
            AQ("dve", lambda e: e.tensor_copy(out=rtmp[:, 0:256], in_=ps[0][:, 0:256]))
            AQ("sp", lambda e, h=h: e.dma_start(out=bsc[h, :, :], in_=rtmp[:, 0:256]), dma="at")
        for h in range(8):
            for ty in range(2):
                src = bass.AP(tensor=bsc_t, offset=h * 128 * 256 + 128 * ty, ap=[[255, 128], [1, 128]])
                AQ("sp", lambda e, h=h, ty=ty, src=src: e.dma_start(out=bT[:, ty, h, :], in_=src), dma="at")
        for k in range(NIT):
            AQ("pool", lambda e, k=k: e.memset(p2[:, k:k + 1], 2.0 ** -(k + 1)))
        AQ("dve", lambda e: e.tensor_copy(out=cfT[:], in_=cfar[:].rearrange("p (h o) -> p h o", o=1).to_broadcast([128, 8, 128])))

    KATT = int(os.environ.get("KATT", "9"))
    KQT = int(os.environ.get("KQT", "8"))

    def att_prompt_tile(qt):
        if KATT < 2 or qt >= KQT:
            return
        q0 = qt * 128
        nb = 9 + qt
        Sx = nb * 128
        segs = [(kiT2_pre, 0, 512), (kiT2_pre, 512, 512)]
        own = 128 * (qt + 1)
        c = 0
        while c < own:
            n = min(512, own - c)
            segs.append((kiT2, c, n))
            c += n
        AQ("dve", lambda e: e.tensor_scalar(out=wis[:], in0=sm_tok[:, qt, 64:80], scalar1=1024.0 ** -0.5, scalar2=None, op0=ALU.mult))
        for hi in range(16):
            pb = 64 * (hi % 2)
            par = hi % 2
            rt = rtmp if par == 0 else rtmp2
            for si, (src, c0, n) in enumerate(segs):
                S.op("pe", lambda e, si=si, src=src, c0=c0, n=n, pb=pb, hi=hi, par=par: e.matmul(
                    ps[4 * par + si][:, 0:n], lhsT=qiT[pb:pb + 64, hi // 2, q0:q0 + 128], rhs=src[pb:pb + 64, c0:c0 + n], start=True, stop=True),
                     r=["att"], w=[("ixp", par)])
            S.op("act", lambda e, par=par, rt=rt: e.activation(out=rt[:, 0:Sx], in_=psall[:, 2048 * par:2048 * par + Sx], func=AF.Relu),
                 r=[("ixp", par)], w=[("ixr", par)])
            if hi == 0:
                S.op("dve", lambda e, rt=rt: e.tensor_scalar(out=sc[:, 0:Sx], in0=rt[:, 0:Sx], scalar1=wis[:, 0:1], scalar2=None, op0=ALU.mult),
                     r=[("ixr", par), "att"], w=["sc"])
            else:
                S.op("dve", lambda e, hi=hi, rt=rt: e.scalar_tensor_tensor(out=sc[:, 0:Sx], in0=rt[:, 0:Sx], scalar=wis[:, hi:hi + 1], in1=sc[:, 0:Sx],
                                                                            op0=ALU.mult, op1=ALU.add), r=[("ixr", par)], w=["sc"])
        AQ("pool", lambda e: e.memset(stat[:, 5:6], 0.0), r=["sc", ("ixp", 0), ("ixp", 1), ("ixr", 0), ("ixr", 1)])
        if KATT < 3:
            return
        absm, lo, mid, cnt, tt, Wd = [a1[:, i:i + 1] for i in range(6)]
        AQ("dve", lambda e: e.tensor_reduce(out=absm, in_=sc[:, 0:Sx], axis=AX.X, op=ALU.max, apply_absolute_value=True))
        AQ("dve", lambda e: e.tensor_scalar(out=sc[:, 0:1024], in0=sc[:, 0:1024], scalar1=flag_t[:, 1:2], scalar2=None, op0=ALU.add))
        AQ("pool", lambda e: e.affine_select(out=sc[:, Sx - 128:Sx], in_=sc[:, Sx - 128:Sx], pattern=[[-1, 128]], compare_op=ALU.is_ge,
                                             fill=-1e30, base=0, channel_multiplier=1))
        AQ("dve", lambda e: e.tensor_scalar(out=lo, in0=absm, scalar1=-1.0, scalar2=-1.0, op0=ALU.mult, op1=ALU.add))
        AQ("dve", lambda e: e.tensor_scalar(out=Wd, in0=absm, scalar1=2.0, scalar2=2.0, op0=ALU.mult, op1=ALU.add))
        AQ("dve", lambda e: e.tensor_scalar(out=hw[:, 0:NIT], in0=p2[:, 0:NIT], scalar1=Wd, scalar2=None, op0=ALU.mult))
        for k in range(NIT):
            AQ("dve", lambda e, k=k: e.tensor_tensor(out=mid, in0=lo, in1=hw[:, k:k + 1], op=ALU.add))
            AQ("dve", lambda e: e.tensor_scalar(out=mk[:, 0:Sx], in0=sc[:, 0:Sx], scalar1=mid, scalar2=0.0, op0=ALU.is_ge, op1=ALU.add, accum_out=cnt))
            AQ("dve", lambda e, k=k: e.tensor_scalar(out=tt, in0=cnt, scalar1=256.0, scalar2=hw[:, k:k + 1], op0=ALU.is_ge, op1=ALU.mult))
            AQ("dve", lambda e: e.tensor_tensor(out=lo, in0=lo, in1=tt, op=ALU.add))
        AQ("dve", lambda e: e.tensor_scalar(out=mk[:, 0:Sx], in0=sc[:, 0:Sx], scalar1=lo, scalar2=None, op0=ALU.is_ge))
        if KATT < 4:
            return
        pv0, pv1 = ps[0].bitcast(BF16), ps[1].bitcast(BF16)
        for kb in range(nb):
            dst = pv0[:, kb * 128:(kb + 1) * 128] if kb < 8 else pv1[:, (kb - 8) * 128:(kb - 7) * 128]
            AQ("pe", lambda e, kb=kb, dst=dst: e.transpose(out=dst, in_=mk[:, kb * 128:(kb + 1) * 128], identity=ident_b[:]))
        AQ("dve", lambda e: e.tensor_copy(out=mkT[:, 0:8, :], in_=pv0.rearrange("p (k c) -> p k c", c=128)))
        AQ("dve", lambda e: e.tensor_copy(out=mkT[:, 8:nb, :], in_=pv1[:, 0:(nb - 8) * 128].rearrange("p (k c) -> p k c", c=128)))

        if KATT < 5:
            return

        def pso(h):
            return ps[5 + h // 3][:, (h % 3) * 160:(h % 3) * 160 + 129]

        for kb in range(nb):
            pre = kb < 8
            j = kb if pre else kb - 8
            ksrc = kT_pre if pre else kT
            vsrc = vb1_pre if pre else vb1
            diff = (8 + qt) - kb
            for kv in range(2):
                AQ("pe", lambda e, kv=kv, ksrc=ksrc, j=j: e.matmul(ps[2 + kv].rearrange("p (h c) -> p h c", c=128), lhsT=ksrc[:, kv, j * 128:(j + 1) * 128],
                                                                  rhs=qT[:, 4 * kv:4 * kv + 4, q0:q0 + 128], start=True, stop=True))
                btile = cfT[:, 4 * kv:4 * kv + 4, :] if diff >= 2 else bT[:, diff, 4 * kv:4 * kv + 4, :]
                AQ("dve", lambda e, kv=kv, btile=btile: e.tensor_tensor(out=pexp[:].rearrange("p (h c) -> p h c", c=128),
                                                                        in0=ps[2 + kv].rearrange("p (h c) -> p h c", c=128), in1=btile, op=ALU.add))
                AQ("act", lambda e: e.activation(out=pexp[:], in_=pexp[:], func=AF.Exp))
                AQ("dve", lambda e, kb=kb: e.tensor_tensor(out=pm[:].rearrange("p (h c) -> p h c", c=128), in0=pexp[:].rearrange("p (h c) -> p h c", c=128),
                                                           in1=mkT[:, kb:kb + 1, :].to_broadcast([128, 4, 128]), op=ALU.mult))
                for hh in range(4):
                    h = 4 * kv + hh
                    AQ("pe", lambda e, h=h, hh=hh, vsrc=vsrc, j=j, kv=kv, kb=kb: e.matmul(pso(h), lhsT=pm[:, hh * 128:(hh + 1) * 128], rhs=vsrc[:, j, kv, :],
                                                                                       start=(kb == 0), stop=(kb == nb - 1)))
        if KATT < 6:
            return
        for b3 in range(3):
            nh = 3 if b3 < 2 else 2
            AQ("dve", lambda e, b3=b3, nh=nh: e.reciprocal(out=rden[:, 3 * b3:3 * b3 + nh],
                                                          in_=ps[5 + b3][:, 0:480].rearrange("p (h c) -> p h c", c=160)[:, 0:nh, 128]))
        for h in range(8):
            AQ("dve", lambda e, h=h: e.tensor_scalar(out=atok[:, h * 128:(h + 1) * 128], in0=pso(h)[:, 0:128], scalar1=rden[:, h:h + 1], scalar2=None,
                                                     op0=ALU.mult))
        for h in range(8):
            AQ("pe", lambda e, h=h: e.transpose(out=pv0[:, h * 128:(h + 1) * 128], in_=atok[:, h * 128:(h + 1) * 128], identity=ident_b[:]))
        AQ("dve", lambda e: e.tensor_copy(out=attT[:, :, q0:q0 + 128], in_=pv0.rearrange("p (k c) -> p k c", c=128)))
        S.op("pool", lambda e: e.memset(stat[:, 6:7], 0.0), r=["att"], w=["mixT"])


    def att_sample():
        o = O_W2G
        def T(shape, dt, nm):
            nonlocal o
            nb_ = int(np.prod(shape[1:])) * (2 if dt == BF16 else 4)
            t_ = A.at(o, shape, dt, nm)
            o = al(o + nb_)
            return t_
        pti = T([128, 128], I32, "s_pti"); ptf = T([128, 128], F32, "s_ptf"); idx_i = T([128, 128], I32, "s_idx")
        iota_c = T([128, 8], F32, "s_iota")
        kig2 = [T([128, 4, 64], F32, "s_kig0"), A.at(O_STAGE + 18432, [128, 4, 64], F32, "s_kig1")]; kiTs = T([64, 512], BF16, "s_kiTs")
        qiS = T([64, 16, 8], BF16, "s_qiS")
        wis32 = T([32, 16], F32, "s_wis32"); wd32 = T([32, 16, 8], F32, "s_wd32"); wdb = T([32, 128], F32, "s_wdb")
        wrow = T([128, 128], F32, "s_wrow")
        bm_t = T([32, 4], F32, "s_bm"); dq_t = T([32, 8], F32, "s_dq"); pen_t = T([32, 4, 8], F32, "s_pen")
        scS = T([128, 132, 8], F32, "s_scS"); ind = T([128, 132, 8], BF16, "s_ind"); mS = T([128, 132, 8], BF16, "s_mS")
        KVg2 = [A.at(O_STAGE, [128, 4, 512], F32, "s_KVg0"), A.at(O_STAGE + 10240, [128, 4, 512], F32, "s_KVg1")]
        kTs = T([128, 4, 2, 128], BF16, "s_kTs"); Vb = T([128, 4, 2, 129], BF16, "s_Vb")
        pS = T([128, 256], F32, "s_pS"); pmS = T([128, 256], BF16, "s_pmS")
        cfS = T([128, 8, 8], F32, "s_cfS"); bSl = T([128, 8, 8], F32, "s_bSl"); bSn = T([32, 8, 8], F32, "s_bSn")
        rw = [T([128, 8], F32, f"s_rw{i}") for i in range(8)]
        dg = T([8, 8], F32, "s_dg"); m2 = T([8, 8], F32, "s_m2")
        atS = T([32, 2, 128], BF16, "s_atS"); rdS = T([32, 8], F32, "s_rdS")
        assert o <= O_W2G + 12 * NB, (o - O_W2G, 12 * NB)
        absr, lor, midr, cntr, ttr, Wr, pc, hwr = rw
        NPG = 128

        AQ("pool", lambda e: e.iota(iota_c[:, 0:1], pattern=[[0, 1]], base=0, channel_multiplier=1, allow_small_or_imprecise_dtypes=True))
        AQ("sp", lambda e: e.dma_start(out=bm_t[:], in_=bm4[:, :]), dma="at")
        AQ("sp", lambda e: e.dma_start(out=dq_t[:], in_=dq8[:, :]), dma="at")
        AQ("sp", lambda e: e.dma_start(out=pen_t[:], in_=pen32[:, :, :]), dma="at")
        AQ("dve", lambda e: e.tensor_scalar(out=wis32[:], in0=sm_tok[0:32, 8, 64:80], scalar1=1024.0 ** -0.5, scalar2=None, op0=ALU.mult))
        AQ("dve", lambda e: e.tensor_tensor(out=wd32[:], in0=wis32[:].rearrange("p (h o) -> p h o", o=1).to_broadcast([32, 16, 8]),
                                            in1=dq_t[:].rearrange("p (o q) -> p o q", o=1).to_broadcast([32, 16, 8]), op=ALU.mult))
        AQ("dve", lambda e: e.tensor_copy(out=cfS[:], in_=cfar[:].rearrange("p (h o) -> p h o", o=1).to_broadcast([128, 8, 8])))
        for h in range(8):
            src = bass.AP(tensor=bsc_t, offset=h * 128 * 256 + 128, ap=[[255, 128], [1, 8]])
            AQ("sp", lambda e, h=h, src=src: e.dma_start(out=bSl[:, h, :], in_=src), dma="at")
            for tb in range(4):
                src2 = bass.AP(tensor=bsc_t, offset=h * 128 * 256, ap=[[255, 8], [1, 8]])
                AQ("sp", lambda e, h=h, tb=tb, src2=src2: e.dma_start(out=bSn[8 * tb:8 * tb + 8, h, :], in_=src2), dma="at")
        AQ("pool", lambda e: e.memset(Vb[:, :, :, 128:129], 1.0))

        for b in range(4):
            cb = 1024 + 8 * b
            AQ("sp", lambda e, b=b: e.dma_start(out=pti[:], in_=pt4[b:b + 1, :].partition_broadcast(128)), dma="at")
            AQ("dve", lambda e: e.tensor_copy(out=ptf[:], in_=pti[:]))
            AQ("dve", lambda e: e.tensor_scalar(out=ptf[:], in0=ptf[:], scalar1=128.0, scalar2=iota_c[:, 0:1], op0=ALU.mult, op1=ALU.add))
            AQ("dve", lambda e: e.tensor_copy(out=idx_i[:], in_=ptf[:]), w=["att_idx"])
            qv = qiS[:].rearrange("d (hc two) q -> d hc two q", two=2)
            AQ("sp", lambda e, cb=cb, qv=qv: e.dma_start(out=qv[:, :, 0, :], in_=qiT[0:64, :, cb:cb + 8]), dma="at")
            AQ("sp", lambda e, cb=cb, qv=qv: e.dma_start(out=qv[:, :, 1, :], in_=qiT[64:128, :, cb:cb + 8]), dma="at")
            qflat = qiS[:].rearrange("d h q -> d (h q)")
            AQ("dve", lambda e, b=b: e.tensor_scalar(out=wdb[:], in0=wd32[:].rearrange("p h q -> p (h q)"), scalar1=bm_t[:, b:b + 1], scalar2=None, op0=ALU.mult))
            AQ("pe", lambda e: e.matmul(ps[0][:, 0:128], lhsT=ones_f[0:32, :], rhs=wdb[:], start=True, stop=True))
            AQ("dve", lambda e: e.tensor_copy(out=wrow[:], in_=ps[0][:, 0:128]))
            AQ("pool", lambda e: e.memset(scS[:, 128, :], -1e30))
            AQ("pe", lambda e, qflat=qflat: e.matmul(ps[0][0:32, 0:128], lhsT=kiT2[0:64, 1024:1056], rhs=qflat, start=True, stop=True))
            AQ("act", lambda e: e.activation(out=rtmp[0:32, 0:128], in_=ps[0][0:32, 0:128], func=AF.Relu))
            AQ("dve", lambda e: e.tensor_tensor(out=rtmp[0:32, 0:128], in0=rtmp[0:32, 0:128], in1=wrow[0:32, :], op=ALU.mult))
            AQ("dve", lambda e: e.tensor_reduce(out=scS[0:32, 128, :], in_=rtmp[0:32, 0:128].rearrange("p (h q) -> p q h", q=8), axis=AX.X, op=ALU.add))
            AQ("dve", lambda e, b=b: e.tensor_tensor(out=scS[0:32, 128, :], in0=scS[0:32, 128, :], in1=pen_t[:, b, :], op=ALU.add))
            for st in range(NPG // 4):
                j0 = 4 * st
                kig = kig2[st % 2]
                for pg in range(4):
                    gather(kig[:, pg, :], kidx_tab, idx_i, j0 + pg, ("kig", st % 2, pg))
                for pg in range(4):
                    AQ("pe", lambda e, pg=pg, kig=kig: e.transpose(out=ps[0][0:64, pg * 128:(pg + 1) * 128], in_=kig[:, pg, :], identity=ident_f[:]),
                       r=[("kig", st % 2, pg)])
                AQ("dve", lambda e: e.tensor_copy(out=kiTs[:], in_=ps[0][0:64, :]))
                for pg in range(4):
                    AQ("pe", lambda e, pg=pg, qflat=qflat: e.matmul(ps[1][:, pg * 128:(pg + 1) * 128], lhsT=kiTs[:, pg * 128:(pg + 1) * 128], rhs=qflat,
                                                                   start=True, stop=True))
                AQ("act", lambda e: e.activation(out=rtmp[:, 0:512], in_=ps[1][:, :], func=AF.Relu))
                AQ("dve", lambda e: e.tensor_tensor(out=rtmp[:, 0:512].rearrange("p (g c) -> p g c", g=4), in0=rtmp[:, 0:512].rearrange("p (g c) -> p g c", g=4),
                                                    in1=wrow[:].rearrange("p (o c) -> p o c", o=1).to_broadcast([128, 4, 128]), op=ALU.mult))
                AQ("dve", lambda e, j0=j0: e.tensor_reduce(out=scS[:, j0:j0 + 4, :], in_=rtmp[:, 0:512].rearrange("p (g h q) -> p g q h", g=4, q=8),
                                                           axis=AX.X, op=ALU.add))
            AQ("dve", lambda e: e.tensor_reduce(out=pc[:], in_=scS[:, 0:128, :].rearrange("p k q -> p q k"), axis=AX.X, op=ALU.max, apply_absolute_value=True))
            AQ("pe", lambda e: e.transpose(out=ps[0][0:8, 0:128], in_=pc[:], identity=ident_f[:]))
            AQ("dve", lambda e: e.tensor_reduce(out=m2[:, 0:1], in_=ps[0][0:8, 0:128], axis=AX.X, op=ALU.max))
            AQ("dve", lambda e: e.tensor_scalar(out=dg[:], in0=ident_f[0:8, 0:8], scalar1=m2[:, 0:1], scalar2=None, op0=ALU.mult))
            AQ("pe", lambda e: e.matmul(ps[0][:, 0:8], lhsT=ones_f[0:8, :], rhs=dg[:], start=True, stop=True))
            AQ("dve", lambda e: e.tensor_copy(out=absr[:], in_=ps[0][:, 0:8]))
            AQ("dve", lambda e: e.tensor_scalar(out=lor[:], in0=absr[:], scalar1=-1.0, scalar2=-1.0, op0=ALU.mult, op1=ALU.add))
            AQ("dve", lambda e: e.tensor_scalar(out=Wr[:], in0=absr[:], scalar1=2.0, scalar2=2.0, op0=ALU.mult, op1=ALU.add))
            for k in range(NIT):
                AQ("dve", lambda e, k=k: e.tensor_scalar(out=hwr[:], in0=Wr[:], scalar1=2.0 ** -(k + 1), scalar2=None, op0=ALU.mult))
                AQ("dve", lambda e: e.tensor_tensor(out=midr[:], in0=lor[:], in1=hwr[:], op=ALU.add))
                AQ("dve", lambda e: e.tensor_tensor(out=ind[:, 0:129, :], in0=scS[:, 0:129, :],
                                                    in1=midr[:].rearrange("p (o q) -> p o q", o=1).to_broadcast([128, 129, 8]), op=ALU.is_ge))
                AQ("dve", lambda e: e.tensor_reduce(out=pc[:], in_=ind[:, 0:129, :].rearrange("p k q -> p q k"), axis=AX.X, op=ALU.add))
                AQ("pe", lambda e: e.matmul(ps[0][:, 0:8], lhsT=ones_f[:, :], rhs=pc[:], start=True, stop=True))
                AQ("dve", lambda e: e.tensor_scalar(out=ttr[:], in0=ps[0][:, 0:8], scalar1=256.0, scalar2=None, op0=ALU.is_ge))
                AQ("dve", lambda e: e.tensor_tensor(out=ttr[:], in0=ttr[:], in1=hwr[:], op=ALU.mult))
                AQ("dve", lambda e: e.tensor_tensor(out=lor[:], in0=lor[:], in1=ttr[:], op=ALU.add))
            AQ("dve", lambda e: e.tensor_tensor(out=mS[:, 0:129, :], in0=scS[:, 0:129, :],
                                                in1=lor[:].rearrange("p (o q) -> p o q", o=1).to_broadcast([128, 129, 8]), op=ALU.is_ge))
            def pso(kv):
                return ps[4][0:32, kv * 160:kv * 160 + 129]
            for st in range(NPG // 4):
                j0 = 4 * st
                KVg = KVg2[st % 2]
                for pg in range(4):
                    gather(KVg[:, pg, :], kv_tab, idx_i, j0 + pg, ("kvg", st % 2, pg))
                for pg in range(4):
                    for kv in range(2):
                        r_ = pg * 2 + kv
                        AQ("pe", lambda e, pg=pg, kv=kv, r_=r_, KVg=KVg: e.transpose(out=ps[r_ // 4][:, (r_ % 4) * 128:(r_ % 4 + 1) * 128],
                                                                                   in_=KVg[:, pg, kv * 128:(kv + 1) * 128], identity=ident_f[:]),
                           r=[("kvg", st % 2, pg)])
                AQ("dve", lambda e: e.tensor_copy(out=kTs[:, 0:2, :, :].rearrange("p g k s -> p (g k s)"), in_=ps[0][:, :]))
                AQ("act", lambda e: e.activation(out=kTs[:, 2:4, :, :].rearrange("p g k s -> p (g k s)"), in_=ps[1][:, :], func=AF.Copy))
                AQ("dve", lambda e, KVg=KVg: e.tensor_copy(out=Vb[:, :, :, 0:128], in_=KVg[:, :, 256:512].rearrange("p g (k d) -> p g k d", k=2)),
                   r=[("kvg", st % 2, pg) for pg in range(4)])
                for pg in range(4):
                    for kv in range(2):
                        r_ = pg * 2 + kv
                        AQ("pe", lambda e, pg=pg, kv=kv, r_=r_, cb=cb: e.matmul(ps[2][:, r_ * 32:(r_ + 1) * 32].rearrange("p (h q) -> p h q", q=8),
                                                                              lhsT=kTs[:, pg, kv, :], rhs=qT[:, 4 * kv:4 * kv + 4, cb:cb + 8], start=True, stop=True))
                AQ("dve", lambda e: e.tensor_tensor(out=pS[:].rearrange("p (g c) -> p g c", g=4), in0=ps[2][:, 0:256].rearrange("p (g c) -> p g c", g=4),
                                                    in1=cfS[:].rearrange("p h q -> p (h q)").rearrange("p (o c) -> p o c", o=1).to_broadcast([128, 4, 64]), op=ALU.add))
                if st == NPG // 4 - 1:
                    AQ("dve", lambda e: e.tensor_tensor(out=pS[:, 192:256], in0=ps[2][:, 192:256], in1=bSl[:].rearrange("p h q -> p (h q)"), op=ALU.add))
                AQ("act", lambda e: e.activation(out=pS[:], in_=pS[:], func=AF.Exp))
                AQ("dve", lambda e, j0=j0: e.tensor_tensor(out=pmS[:].rearrange("p (g h q) -> p g h q", g=4, q=8), in0=pS[:].rearrange("p (g h q) -> p g h q", g=4, q=8),
                                                           in1=mS[:, j0:j0 + 4, :].rearrange("p g (o q) -> p g o q", o=1).to_broadcast([128, 4, 8, 8]), op=ALU.mult))
                for pg in range(4):
                    for kv in range(2):
                        r_ = pg * 2 + kv
                        AQ("pe", lambda e, pg=pg, kv=kv, r_=r_, st=st: e.matmul(pso(kv), lhsT=pmS[:, r_ * 32:(r_ + 1) * 32], rhs=Vb[:, pg, kv, :],
                                                                              start=(st == 0 and pg == 0), stop=False))
            for kv in range(2):
                AQ("pe", lambda e, kv=kv, cb=cb: e.matmul(ps[2][0:32, kv * 32:(kv + 1) * 32].rearrange("p (h q) -> p h q", q=8), lhsT=kT[:, kv, 1024:1056],
                                                         rhs=qT[:, 4 * kv:4 * kv + 4, cb:cb + 8], start=True, stop=True))
            AQ("dve", lambda e: e.tensor_tensor(out=pS[0:32, 0:64], in0=ps[2][0:32, 0:64], in1=bSn[:].rearrange("p h q -> p (h q)"), op=ALU.add))
            AQ("act", lambda e: e.activation(out=pS[0:32, 0:64], in_=pS[0:32, 0:64], func=AF.Exp))
            AQ("dve", lambda e: e.tensor_tensor(out=pmS[0:32, 0:64].rearrange("p (h q) -> p h q", q=8), in0=pS[0:32, 0:64].rearrange("p (h q) -> p h q", q=8),
                                                in1=mS[0:32, 128:129, :].to_broadcast([32, 8, 8]), op=ALU.mult))
            for kv in range(2):
                AQ("pe", lambda e, kv=kv: e.matmul(pso(kv), lhsT=pmS[0:32, kv * 32:(kv + 1) * 32], rhs=vb1[0:32, 8, kv, :], start=False, stop=True))
            for kv in range(2):
                AQ("dve", lambda e, kv=kv: e.reciprocal(out=rdS[:, kv:kv + 1], in_=pso(kv)[:, 128:129]))
                AQ("dve", lambda e, kv=kv: e.tensor_scalar(out=atS[:, kv, :], in0=pso(kv)[:, 0:128], scalar1=rdS[:, kv:kv + 1], scalar2=None, op0=ALU.mult))
            pvb = ps[0].bitcast(BF16)
            for kv in range(2):
                AQ("pe", lambda e, kv=kv: e.transpose(out=pvb[:, kv * 32:(kv + 1) * 32], in_=atS[:, kv, :], identity=ident_b[0:32, 0:32]))
            AQ("dve", lambda e, cb=cb: e.tensor_copy(out=attT[:, :, cb:cb + 8], in_=pvb[:, 0:64].rearrange("p (h q) -> p h q", q=8)))
        S.op("pool", lambda e: e.memset(stat[:, 6:7], 0.0), r=["att"], w=["mixT"])

    xst = [A.at(OH + 10272 + i * 2048, [128, 512], F32, f"xst{i}") for i in range(4)]
    xctr = [0]

    def outproj():
        for nb in range(4):
            wt = w2g[nb % 2]
            wv = wt[:].rearrange("p g d -> p (g d)").rearrange("p (k c) -> p k c", c=512)
            for kg in range(4):
                st, sk, sg, i = stage_slot()
                S.op("sp", lambda e, st=st, nb=nb, kg=kg: e.dma_start(out=st[:].rearrange("p a b -> p (a b)"),
                                                                      in_=wout[nb, kg].rearrange("p k c -> p (k c)")), w=[sk], dma=sg)
                S.op("act", lambda e, st=st, wv=wv, kg=kg: e.activation(out=wv[:, kg * 4:(kg + 1) * 4, :],
                                                                        in_=st[:].rearrange("p a b -> p (a b)").rearrange("p (k c) -> p k c", c=512),
                                                                        func=AF.Copy), r=[sk], w=[("wo", nb % 2, kg)])
            for t in range(NTT):
                m = 128 if t < NPT else 35
                pd = 6 + t % 2
                xi = xctr[0] % 2
                xctr[0] += 1
                S.op("sp", lambda e, t=t, nb=nb, xi=xi: e.dma_start(out=xst[xi][:], in_=xsp[t, :, nb * 512:(nb + 1) * 512]),
                     r=[("xsp", t, nb)], w=[("xst", xi)], dma=f"xsi{xi}")
                for kc in range(16):
                    src = attT if kc < 8 else ssdT
                    S.op("pe", lambda e, t=t, kc=kc, pd=pd, m=m, src=src, wv=wv: e.matmul(
                        ps[pd][0:m, :], lhsT=src[:, kc % 8, t * 128:t * 128 + m], rhs=wv[:, kc, :], start=(kc == 0), stop=(kc == 15)),
                         r=[("wo", nb % 2, kc // 4), "mixT"], w=[("ps", pd)])
                S.op("dve", lambda e, xi=xi, pd=pd, m=m: e.tensor_tensor(out=xst[xi][0:m, :], in0=ps[pd][0:m, :], in1=xst[xi][0:m, :], op=ALU.add),
                     r=[("ps", pd)], w=[("xst", xi)])
                S.op("sp", lambda e, t=t, nb=nb, xi=xi: e.dma_start(out=xsp[t, :, nb * 512:(nb + 1) * 512], in_=xst[xi][:]),
                     r=[("xst", xi)], w=[("xsp", t, nb)], dma=f"xso{xi}")

    def final_norm():
        S.op("sp", lambda e: e.dma_start(out=gb[:], in_=gvec[3:4, :].partition_broadcast(128)),
             r=["stage0"], w=["gb", "stage0"], dma="gld")
        for t in range(NTT):
            S.op("act", lambda e, t=t: e.activation(out=junk[:], in_=xres[:, t, :], func=AF.Square, accum_out=stat[:, 0:1]),
                 r=[("x", t)], w=["junk", "ss", "stage1"])
            S.op("dve", lambda e: e.tensor_scalar(out=stat[:, 1:2], in0=stat[:, 0:1], scalar1=1.0 / D, scalar2=EPS,
                                                  op0=ALU.mult, op1=ALU.add), r=["ss"], w=["ms"])
            S.op("act", lambda e: e.activation(out=stat[:, 2:3], in_=stat[:, 1:2], func=AF.Sqrt), r=["ms"], w=["sd"])
            S.op("dve", lambda e: e.reciprocal(out=stat[:, 3:4], in_=stat[:, 2:3]), r=["sd"], w=["rstd"])
            S.op("dve", lambda e, t=t: e.scalar_tensor_tensor(out=xres[:, t, :], in0=xres[:, t, :], scalar=stat[:, 3:4], in1=gb[:],
                                                              op0=ALU.mult, op1=ALU.mult),
                 r=[("x", t), "rstd", "gb", "stage0"], w=[("x", t)])
            S.op("sp", lambda e, t=t: e.dma_start(out=y_out[t, :, :], in_=xres[:, t, :]), r=[("x", t)], w=[("y_out", t)], dma="out")

    try:
        load_x(xpre, NPT)
        norm_to_hT(0, NPT)
        ffn(wf1, NPT)
        norm_to_hT(1, NPT)
        bar()
        inproj(False)
        bar()
        S.op("pool", lambda e: e.memset(Hst[:], 0.0), w=["ssd"])
        for t in range(NPT):
            ssd_chunk(t * 128, 128, False)
        S.op("dve", lambda e: e.tensor_scalar(out=Hst[:], in0=Hst[:], scalar1=flag_t[:, 0:1], scalar2=None, op0=ALU.mult), r=[("cst", 0)], w=["ssd"])
        S.op("sp", lambda e: e.dma_start(out=pre_k[:, :, :], in_=kT[:, :, 0:1024]), w=["pre0"], dma="pre")
        S.op("sp", lambda e: e.dma_start(out=pre_v[:, :, :], in_=vb1[:, 0:8, :, :].rearrange("p t k d -> p t (k d)")),
             r=["vb1ones"], w=["pre1"], dma="pre")
        S.op("sp", lambda e: e.dma_start(out=pre_ki[:, :], in_=kiT2[:, 0:1024]), w=["pre2"], dma="pre")
        S.op("sp", lambda e: e.dma_start(out=pre_h[:, :], in_=Hst[:]), r=["ssd"], w=["pre3"], dma="pre")
        S.op("sp", lambda e: e.dma_start(out=pre_halo[:, :], in_=halo[:].rearrange("p a b -> p (a b)")), r=["halo"], w=["pre4"], dma="pre")
        S.dma_batch_end("pre")
        bar()
        load_x(xin, NTT)
        norm_to_hT(0, NTT)
        ffn(wf1, NTT)
        norm_to_hT(1, NTT)
        for t in range(NTT):
            S.op("sp", lambda e, t=t: e.dma_start(out=xsp[t, :, :], in_=xres[:, t, :]), r=[("x", t)], w=[("xsp", t, nb) for nb in range(4)], dma="xsp")
        bar()
        inproj(True)
        bar()
        S.op("sp", lambda e: e.dma_start(out=kT_pre[:], in_=pre_k[:, :, :]), w=["kT_pre"], dma="pre")
        S.op("sp", lambda e: e.dma_start(out=vb1_pre[:].rearrange("p t k d -> p t (k d)"), in_=pre_v[:, :, :]),
             w=["vb1_pre"], dma="pre")
        S.op("sp", lambda e: e.dma_start(out=kiT2_pre[:], in_=pre_ki[:, :]), w=["kiT2_pre"], dma="pre")
        S.op("sp", lambda e: e.dma_start(out=Hst[:], in_=pre_h[:, :]), w=["ssd"], dma="pre")
        S.dma_batch_end("pre")
        S.op("act", lambda e: e.activation(out=Hb[:], in_=Hst[:], func=AF.Copy), w=["ssd"])
        for t in range(NPT):
            ssd_chunk(t * 128, 128, True)
        S.op("sp", lambda e: e.dma_start(out=ssm_out[0, :, :], in_=Hst[:]), r=["ssd"], w=[("ssm_out", 0)], dma="hs")
        for b in range(4):
            S.op("sp", lambda e, b=b: e.dma_start(out=Hst[:], in_=sssmT[b]), r=[("ssm_out", b)], w=["ssd"], dma="hs")
            S.op("act", lambda e: e.activation(out=Hb[:], in_=Hst[:], func=AF.Copy), w=["ssd"])
            ssd_chunk(1024 + 8 * b, 8, True)
            S.op("sp", lambda e, b=b: e.dma_start(out=ssm_out[1 + b, :, :], in_=Hst[:]), r=["ssd"], w=[("ssm_out", 1 + b)], dma="hs")
        bar()
        S.op("pool", lambda e: e.memset(attT[:], 0.0), w=["mixT", "att"])
        att_setup()
        for qt in range(NPT):
            att_prompt_tile(qt)
        if "S" not in KSKIP:
            att_sample()
        S.op("pool", lambda e: e.memset(ssdT[:, :, 1056:NTOK], 0.0), r=["ssd"], w=["mixT", "ssd"])
        bar()
        outproj()
        bar()
        load_x(xsp, NTT)
        if "h" not in KSKIP:
            norm_to_hT(2, NTT)
        if "f" not in KSKIP:
            ffn(wf2, NTT)
        if "n" not in KSKIP:
            final_norm()
    except _Stop:
        pass
    fin = ["out"] + [f"tok{i}" for i in range(2)] + ["tl0"] + ["hs"]
    S.emit(final_dma_groups=fin)
    return nc


def _up_layout(W):
    K, N = W.shape
    assert K == D and N % 128 == 0
    return np.ascontiguousarray(W.reshape(16, 128, N // 128, 128).transpose(2, 1, 0, 3))


def _t5_onehot():
    n = np.arange(256)
    nf = np.maximum(n, 1).astype(np.float32)
    large = 16 + (np.log(nf / np.float32(16)) / np.float32(np.log(128 / 16)) * np.float32(16)).astype(np.int32)
    bucket = np.where(n < 16, n, np.minimum(large, 31))
    oh = np.zeros((32, 256), np.float32)
    oh[bucket, n] = 1.0
    return oh


_IN_OFF = dict(q=(0, 1024), k=(1024, 256), v=(1280, 256), qi=(1536, 1024), ki=(2560, 64), wi=(2624, 16),
               z=(2640, 1024), xbc=(3664, 1536), dt=(5200, 16))


def kernel(x_prompt, x_sample, cache_k, cache_v, cache_kidx, state_conv, state_ssm, page_table, rel_bias,
           g_ffn1, w1_ffn1, w3_ffn1, w2_ffn1, g_mix, w_in, conv_w, conv_b, a_log, dt_bias, d_skip, g_ssd,
           w_out, g_ffn2, w1_ffn2, w3_ffn2, w2_ffn2, g_final):
    f = lambda a: np.asarray(a, dtype=np.float32)
    xp, xs_ = f(x_prompt), f(x_sample)
    win_full = f(w_in)[0]
    cols = []
    for nm in ("q", "k", "v", "qi", "z", "xbc", "ki", "wi", "dt"):
        o, n = _IN_OFF[nm]
        cols.append(win_full[:, o:o + n])
    cols.append(np.zeros((D, 32), np.float32))
    ko, kn = _IN_OFF["ki"]
    cols += [win_full[:, ko:ko + kn], win_full[:, ko:ko + kn]]
    do, dn = _IN_OFF["dt"]
    cols += [win_full[:, do:do + dn], np.zeros((D, 112), np.float32)]
    win_r = _up_layout(np.concatenate(cols, axis=1))
    shared = dict(
        gvec=np.stack([f(g_ffn1)[0], f(g_mix)[0], f(g_ffn2)[0], f(g_final)]),
        w1a=_up_layout(f(w1_ffn1)[0]), w3a=_up_layout(f(w3_ffn1)[0]), w2a=np.ascontiguousarray(f(w2_ffn1)[0].reshape(NFF, 128, D)),
        w1b=_up_layout(f(w1_ffn2)[0]), w3b=_up_layout(f(w3_ffn2)[0]), w2b=np.ascontiguousarray(f(w2_ffn2)[0].reshape(NFF, 128, D)),
        win=win_r,
        wout=np.ascontiguousarray(f(w_out)[0].reshape(4, 4, 128, 4, 512).transpose(3, 0, 2, 1, 4)),
        cwT=np.ascontiguousarray(f(conv_w)[0].reshape(4, 12, 128).transpose(2, 1, 0)),
        cbT=np.ascontiguousarray(f(conv_b)[0].reshape(12, 128).T),
        dcol=np.ascontiguousarray(np.repeat(f(d_skip)[0], 64).reshape(8, 128).T),
        gssd=np.ascontiguousarray(f(g_ssd)[0].reshape(8, 128).T),
        alog=f(a_log), dtb=f(dt_bias), relb=f(rel_bias), oh_d=_t5_onehot(),
    )
    ck = np.ascontiguousarray(f(cache_k)[0].reshape(5120 * 128, 256))
    cv = np.ascontiguousarray(f(cache_v)[0].reshape(5120 * 128, 256))
    cki = np.ascontiguousarray(f(cache_kidx)[0].reshape(5120 * 128, 64))
    ptab = np.asarray(page_table, dtype=np.int32)
    tt_ = np.arange(32)
    bm4 = (tt_[:, None] // 8 == np.arange(4)[None]).astype(np.float32)
    dq8 = (tt_[:, None] % 8 == np.arange(8)[None]).astype(np.float32)
    pen32 = np.where((tt_[:, None, None] // 8 == np.arange(4)[None, :, None]) & (tt_[:, None, None] % 8 <= np.arange(8)[None, None, :]), 0.0, -1e30).astype(np.float32)
    shared.update(kidx_tab=cki, kv_tab=np.concatenate([ck, cv], axis=1), bm4=bm4, dq8=dq8, pen32=pen32)
    sconv_all = f(state_conv)[0]
    sssm_all = f(state_ssm)[0]
    in_maps = []
    for c in range(8):
        b, half = c // 2, c % 2
        xin = np.zeros((NTT, 128, D), np.float32)
        xin[:NPT] = xp[b, half * 1024:(half + 1) * 1024].reshape(NPT, 128, D)
        xin[8, 0:32] = xs_[4 * c:4 * c + 4].reshape(32, D)
        if half == 1:
            xin[8, 32:35] = xp[b, 1021:1024]
        m = dict(shared)
        m["xin"] = xin
        m["pt4"] = np.ascontiguousarray(ptab[4 * c:4 * c + 4])
        m["xpre"] = np.ascontiguousarray(xp[b, 0:1024].reshape(NPT, 128, D)) if half == 1 else np.zeros((NPT, 128, D), np.float32)
        fl = np.zeros((128, 2), np.float32)
        fl[:, 0] = float(half)
        fl[:, 1] = (float(half) - 1.0) * 1e30
        m["flagc"] = fl
        m["sconvT"] = np.ascontiguousarray(sconv_all[4 * c:4 * c + 4].reshape(4, 3, 12, 128).transpose(3, 2, 0, 1))
        m["sssmT"] = np.ascontiguousarray(sssm_all[4 * c:4 * c + 4].reshape(4, 1024, 128).transpose(0, 2, 1))
        in_maps.append(m)
    nc = build()
    res = run_bass_kernel_spmd(nc, in_maps, core_ids=list(range(8))).results
    _DBG["res"] = res

    y_prompt = np.zeros((4, 2048, D), np.float32)
    y_sample = np.zeros((32, 8, D), np.float32)
    k_prompt = np.zeros((1, 4, 2048, 2, 128), np.float32)
    v_prompt = np.zeros_like(k_prompt)
    kidx_prompt = np.zeros((1, 4, 2048, 64), np.float32)
    conv_prompt = np.zeros((1, 4, 3, 1536), np.float32)
    ssm_prompt = np.zeros((1, 4, 16, 64, 128), np.float32)
    k_sample = np.zeros((1, 32, 8, 2, 128), np.float32)
    v_sample = np.zeros_like(k_sample)
    kidx_sample = np.zeros((1, 32, 8, 64), np.float32)
    conv_sample = np.zeros((1, 32, 3, 1536), np.float32)
    ssm_sample = np.zeros((1, 32, 16, 64, 128), np.float32)
    for c in range(8):
        b, half = c // 2, c % 2
        r = res[c]
        sl = slice(half * 1024, (half + 1) * 1024)
        yo = r["y_out"]
        y_prompt[b, sl] = yo[:NPT].reshape(1024, D)
        y_sample[4 * c:4 * c + 4] = yo[8, 0:32].reshape(4, 8, D)
        kv = r["kvs_out"]
        kvp = kv[:NPT].reshape(1024, 640)
        k_prompt[0, b, sl] = kvp[:, 0:256].reshape(1024, 2, 128)
        v_prompt[0, b, sl] = kvp[:, 256:512].reshape(1024, 2, 128)
        kidx_prompt[0, b, sl] = kvp[:, 512:576]
        kvs_ = kv[8, 0:32]
        k_sample[0, 4 * c:4 * c + 4] = kvs_[:, 0:256].reshape(4, 8, 2, 128)
        v_sample[0, 4 * c:4 * c + 4] = kvs_[:, 256:512].reshape(4, 8, 2, 128)
        kidx_sample[0, 4 * c:4 * c + 4] = kvs_[:, 512:576].reshape(4, 8, 64)
        so = r["ssm_out"].reshape(5, 128, 16, 64).transpose(0, 2, 3, 1)
        if half == 1:
            ssm_prompt[0, b] = so[0]
        ssm_sample[0, 4 * c:4 * c + 4] = so[1:5]
        co = r["conv_out"]
        if half == 1:
            conv_prompt[0, b] = co[0]
        conv_sample[0, 4 * c:4 * c + 4] = co[1:5]
    return (y_prompt, y_sample, k_prompt, v_prompt, kidx_prompt, conv_prompt, ssm_prompt,
            k_sample, v_sample, kidx_sample, conv_sample, ssm_sample)
```

```python
import numpy as np
import concourse.bass as bass
import concourse.mybir as mybir
from concourse.bass_utils import run_bass_kernel_spmd
from contextlib import ExitStack

F32 = mybir.dt.float32
BF16 = mybir.dt.bfloat16
I32 = mybir.dt.int32
AF = mybir.ActivationFunctionType
ALU = mybir.AluOpType
AX = mybir.AxisListType

D = 2048
DFF = 5632
NFF = DFF // 128
NPT = 8
NTT = 9
NTOK = 1024 + 35
TB = [(0, 512), (512, 512), (1024, 35)]
EPS = 1e-6
G = 4
NIN = 43


_DBG = {}


class Sched:
    ENGS = ("pe", "act", "dve", "pool", "sp")

    def __init__(self, nc):
        self.nc = nc
        self.ops = []
        self.last_w = {}
        self.readers = {}
        self.dma_cnt = {}
        self.last_eng = {}
        self.last_dma = {}
        self.last_bar = None
        self.batch_ends = {}

    def dma_batch_end(self, group):
        self.batch_ends.setdefault(group, []).append(self.dma_cnt.get(group, 0))

    def barrier(self, fn):
        deps = set(self.last_eng.values()) | set(self.last_dma.values())
        i = self.op("pool", fn, _extra=deps)
        self.last_bar = i
        return i

    def op(self, eng, fn, r=(), w=(), dma=None, _extra=()):
        i = len(self.ops)
        deps = set(_extra)
        if self.last_bar is not None:
            deps.add(self.last_bar)
        for k in list(r) + list(w):
            if k in self.last_w:
                deps.add(self.last_w[k])
        for k in w:
            lastc = {}
            for j in self.readers.get(k, ()):
                oj = self.ops[j]
                if oj["dma"] is None:
                    lastc[oj["eng"]] = max(lastc.get(oj["eng"], -1), j)
                else:
                    deps.add(j)
            deps.update(lastc.values())
        deps.discard(i)
        o = dict(eng=eng, fn=fn, deps=deps, dma=dma, sig=False, i=i)
        if dma is not None:
            self.dma_cnt[dma] = self.dma_cnt.get(dma, 0) + 1
            o["dcount"] = self.dma_cnt[dma]
            self.last_dma[dma] = i
        else:
            self.last_eng[eng] = i
        self.ops.append(o)
        for k in w:
            self.last_w[k] = i
            self.readers[k] = []
        for k in r:
            self.readers.setdefault(k, []).append(i)
        return i

    def emit(self, final_dma_groups=()):
        nc = self.nc
        ops = self.ops
        for o in ops:
            for d in o["deps"]:
                od = ops[d]
                if od["dma"] is None and od["eng"] == "pe" and o["eng"] == "pe" and o["dma"] is None:
                    continue
                od["sig"] = True
        cnt = {e: 0 for e in self.ENGS}
        for o in ops:
            if o["dma"] is None and o["sig"]:
                cnt[o["eng"]] += 1
                o["sval"] = cnt[o["eng"]]
        groups = sorted(self.dma_cnt.keys())
        with ExitStack() as es:
            esem = {e: es.enter_context(nc.semaphore("s_" + e)) for e in self.ENGS}
            dsem = {g: es.enter_context(nc.semaphore("d_" + str(g))) for g in groups}
            block = es.enter_context(nc.Block())
            per_eng = {e: [o for o in ops if o["eng"] == e] for e in self.ENGS}

            def run(engobj, ename):
                waited = {}
                for o in per_eng[ename]:
                    need = {}
                    for d in o["deps"]:
                        od = ops[d]
                        if od["dma"] is not None:
                            key = ("d", od["dma"])
                            dc = od["dcount"]
                            ends = [b for b in self.batch_ends.get(od["dma"], ()) if b >= dc]
                            val = 16 * (min(ends) if ends else dc)
                        else:
                            if od["eng"] == "pe" and ename == "pe" and o["dma"] is None:
                                continue
                            key = ("e", od["eng"])
                            val = od["sval"]
                        if need.get(key, 0) < val:
                            need[key] = val
                    for key, val in need.items():
                        if waited.get(key, 0) >= val:
                            continue
                        waited[key] = val
                        sem = dsem[key[1]] if key[0] == "d" else esem[key[1]]
                        engobj.wait_ge(sem, val)
                    ins = o["fn"](engobj)
                    if o["dma"] is not None:
                        ins.then_inc(dsem[o["dma"]], 16)
                    elif o["sig"]:
                        ins.then_inc(esem[ename], 1)
                if ename == "sp":
                    for g in final_dma_groups:
                        if g in dsem:
                            engobj.wait_ge(dsem[g], 16 * self.dma_cnt[g])

            block.tensor(lambda e: run(e, "pe"))
            block.scalar(lambda e: run(e, "act"))
            block.vector(lambda e: run(e, "dve"))
            block.gpsimd(lambda e: run(e, "pool"))
            block.sync(lambda e: run(e, "sp"))


class Arena:
    def __init__(self, nc, base=16512, top=229344):
        self.nc = nc
        self.base = base
        self.top = top
        self.n = 0

    def at(self, off, shape, dtype, name=None):
        self.n += 1
        nbytes = int(np.prod(shape[1:])) * (2 if dtype == BF16 else 4)
        assert self.base + off + nbytes <= self.top, (name, off, nbytes)
        return self.nc.alloc_sbuf_tensor_at(name or f"t{self.n}", list(shape), dtype, offset=self.base + off)


def build():
    nc = bass.Bass("TRN2", target_bir_lowering=False)
    S = Sched(nc)
    A = Arena(nc)

    def din(name, shape, dt=F32):
        return nc.dram_tensor(name, list(shape), dt, kind="ExternalInput").ap()

    def dout(name, shape, dt=F32):
        return nc.dram_tensor(name, list(shape), dt, kind="ExternalOutput").ap()

    xin = din("xin", [NTT, 128, D])
    xpre = din("xpre", [NPT, 128, D])
    flagc = din("flagc", [128, 2])
    gvec = din("gvec", [4, D])
    wf1 = [din("w1a", [NFF, 128, 16, 128]), din("w3a", [NFF, 128, 16, 128]), din("w2a", [NFF, 128, D])]
    wf2 = [din("w1b", [NFF, 128, 16, 128]), din("w3b", [NFF, 128, 16, 128]), din("w2b", [NFF, 128, D])]
    win = din("win", [NIN, 128, 16, 128])
    wout = din("wout", [4, 4, 128, 4, 512])
    sconvT = din("sconvT", [128, 12, 4, 3])
    sssmT = din("sssmT", [4, 128, 1024])
    cwT = din("cwT", [128, 12, 4])
    cbT = din("cbT", [128, 12])
    dcol = din("dcol", [128, 8])
    gssd = din("gssd", [128, 8])
    alog = din("alog", [1, 16])
    relb = din("relb", [32, 8])
    kidx_tab = din("kidx_tab", [5120 * 128, 64])
    kv_tab = din("kv_tab", [5120 * 128, 512])
    pt4 = din("pt4", [4, 128], I32)
    bm4 = din("bm4", [32, 4])
    dq8 = din("dq8", [32, 8])
    pen32 = din("pen32", [32, 4, 8])
    oh_d = din("oh_d", [32, 256])
    dtb = din("dtb", [1, 16])
    y_out = dout("y_out", [NTT, 128, D])
    kvs_out = dout("kvs_out", [NTT, 128, 640])
    conv_out = dout("conv_out", [5, 3, 1536])
    ssm_out = dout("ssm_out", [5, 128, 1024])
    xsp = nc.dram_tensor("xsp", [NTT, 128, D], F32).ap()
    pre_k = nc.dram_tensor("pre_k", [128, 2, 1024], BF16).ap()
    pre_v = nc.dram_tensor("pre_v", [128, 8, 258], BF16).ap()
    pre_ki = nc.dram_tensor("pre_ki", [128, 1024], BF16).ap()
    pre_h = nc.dram_tensor("pre_h", [128, 1024], F32).ap()
    pre_halo = nc.dram_tensor("pre_halo", [128, 36], F32).ap()
    bsc_t = nc.dram_tensor("bsc", [8, 128, 256], F32)
    bsc = bsc_t.ap()

    OX = 0
    OH = 73728
    OW = OH + 16 * NTOK * 2
    O_STAGE = OW
    O_WUP = O_STAGE + 3 * 8192
    O_W2G = O_WUP + 4 * 4096
    O_GT = O_W2G + 2 * G * D * 2
    GTB = G * NTOK * 2 + 8
    O_SIL = O_GT + 2 * GTB
    O_MISC = O_SIL + 2 * 2048
    xres = A.at(OX, [128, NTT, D], F32, "xres")
    hT = A.at(OH, [128, 16, NTOK], BF16, "hT")
    stage = [A.at(O_STAGE + i * 8192, [128, 16, 128], F32, f"stage{i}") for i in range(3)]
    wup = [A.at(O_WUP + i * 4096, [128, 16, 128], BF16, f"wup{i}") for i in range(4)]
    w2g = [A.at(O_W2G + i * G * D * 2, [128, G, D], BF16, f"w2g{i}") for i in range(2)]
    gT = [A.at(O_GT + i * GTB, [128, G, NTOK], BF16, f"gT{i}") for i in range(2)]
    sil = [A.at(O_SIL + i * 2048, [128, 512], F32, f"sil{i}") for i in range(2)]
    al = lambda v: (v + 31) // 32 * 32
    o = O_MISC
    ident_b = A.at(o, [128, 128], BF16, "ident_b"); o = al(o + 256)
    ident_f = A.at(o, [128, 128], F32, "ident_f"); o = al(o + 512)
    tri_f = A.at(o, [128, 128], F32, "tri_f"); o = al(o + 512)
    ones_f = A.at(o, [128, 128], F32, "ones_f"); o = al(o + 512)
    ones_b = A.at(o, [128, 128], BF16, "ones_b"); o = al(o + 256)
    stat = A.at(o, [128, 8], F32, "stat"); o = al(o + 32)
    flag_t = A.at(o, [128, 2], F32, "flag_t"); o = al(o + 8)
    cw_t = A.at(o, [128, 12, 4], F32, "cw_t"); o = al(o + 192)
    cb_t = A.at(o, [128, 12], F32, "cb_t"); o = al(o + 48)
    dcol_t = A.at(o, [128, 8], F32, "dcol_t"); o = al(o + 32)
    gssd_t = A.at(o, [128, 8], F32, "gssd_t"); o = al(o + 32)
    dtb_t = A.at(o, [128, 16], F32, "dtb_t"); o = al(o + 64)
    a_t = A.at(o, [128, 16], F32, "a_t"); o = al(o + 64)
    halo = A.at(o, [128, 12, 3], F32, "halo"); o = al(o + 144)
    sct = A.at(o, [128, 12, 4, 3], F32, "sct"); o = al(o + 576)
    tokst = [A.at(O_MISC - 2048 + i * 512, [128, 128], F32, f"tokst{i}") for i in range(2)]
    tails = [A.at(o + i * 2560, [3, 5, 128], F32, f"tails{i}") for i in range(1)]; o = al(o + 2560)
    gb = A.at(O_STAGE, [128, D], F32, "gb")
    xs = A.at(O_STAGE + 8192, [128, D], BF16, "xs")
    junk = A.at(O_STAGE + 8192 + 4096, [128, D], BF16, "junk")
    NTP = 1072
    NB = NTP * 2
    qT = A.at(OX, [128, 8, NTP], BF16, "qT")
    qiT = A.at(OX + 8 * NB, [128, 8, NTP], BF16, "qiT")
    szT = A.at(OX + 16 * NB, [128, 8, NTP], BF16, "szT")
    ssdT = szT
    attT = A.at(OX + 24 * NB, [128, 8, NTP], BF16, "attT")
    kT = A.at(OX + 32 * NB, [128, 2, NTP], BF16, "kT")
    o = O_W2G
    xcT = A.at(o, [128, 12, NTP], BF16, "xcT"); o = al(o + 12 * NB)
    vb1 = A.at(o, [128, NTT, 2, 129], BF16, "vb1"); o = al(o + NTT * 2 * 129 * 2 + 4)
    sm_tok = A.at(o, [128, NTT, 128], F32, "sm_tok"); o = al(o + NTT * 512)
    kiT2 = A.at(o, [128, NTOK + 1], BF16, "kiT2"); o = al(o + NB + 2)
    dtT = A.at(o, [16, NTOK], F32, "dtT"); o = al(o + NTOK * 4)
    rawx = A.at(o, [128, NTOK + 3], F32, "rawx"); o = al(o + (NTOK + 3) * 4)
    cacc = A.at(o, [128, 1024], F32, "cacc"); o = al(o + 4096)
    exts = A.at(o, [128, 4, 11], F32, "exts"); o = al(o + 176)
    assert o <= O_MISC - 2048, o - O_MISC
    o = OH
    kT_pre = A.at(o, [128, 2, 1024], BF16, "kT_pre"); o = al(o + 4096)
    vb1_pre = A.at(o, [128, 8, 2, 129], BF16, "vb1_pre"); o = al(o + 4128)
    kiT2_pre = A.at(o, [128, 1024], BF16, "kiT2_pre"); o = al(o + 2048)
    o = al(o + 16384)
    Hst = A.at(o, [128, 1024], F32, "Hst"); o = al(o + 4096)
    Hb = A.at(o, [128, 1024], BF16, "Hb"); o = al(o + 2048)
    assert o <= OW
    o = O_STAGE
    ysb = A.at(o, [128, 1024], F32, "ysb"); o = al(o + 4096)
    xd = A.at(o, [128, 1024], BF16, "xd"); o = al(o + 2048)
    xdd = A.at(o, [128, 1024], BF16, "xdd"); o = al(o + 2048)
    Btok = A.at(o, [128, 256], BF16, "Btok"); o = al(o + 512)
    Dm = A.at(o, [128, 8, 128], F32, "Dm"); o = al(o + 4096)
    CBm = A.at(o, [128, 2, 128], F32, "CBm"); o = al(o + 1024)
    t1 = A.at(o, [128, 128], F32, "t1"); o = al(o + 512)
    t2 = A.at(o, [128, 128], F32, "t2"); o = al(o + 512)
    Mh = A.at(o, [128, 128], BF16, "Mh"); o = al(o + 256)
    sm16 = [A.at(o + i * 64, [128, 16], F32, f"sm16_{i}") for i in range(8)]; o = al(o + 512)
    gbuf = A.at(o, [128, 8, 128], F32, "gbuf"); o = al(o + 4096)
    sq = A.at(o, [128, 8, 128], BF16, "sq"); o = al(o + 2048)
    rs = A.at(o, [128, 2, 128], F32, "rs"); o = al(o + 1024)
    assert o <= O_W2G

    psall = nc.alloc_psum_tensor("psall", [128, 4096], F32)
    ps = [psall[:, i * 512:(i + 1) * 512] for i in range(8)]

    import os
    KSTOP = int(os.environ.get("KSTOP", "99"))
    KSKIP = os.environ.get("KSKIP", "")
    nbar = [0]

    class _Stop(Exception):
        pass

    def bar():
        S.barrier(lambda e: e.memset(stat[:, 7:8], 0.0))
        nbar[0] += 1
        if nbar[0] >= KSTOP:
            raise _Stop()

    for t, nm in ((ident_b, "ident_b"), (ident_f, "ident_f")):
        S.op("pool", lambda e, t=t: e.memset(t[:], 1.0), w=[nm])
        S.op("pool", lambda e, t=t: e.affine_select(out=t[:], in_=t[:], pattern=[[-1, 128]], compare_op=ALU.is_equal,
                                                     fill=0.0, base=0, channel_multiplier=1), r=[nm], w=[nm])
    S.op("pool", lambda e: e.memset(tri_f[:], 1.0), w=["tri_f"])
    S.op("pool", lambda e: e.affine_select(out=tri_f[:], in_=tri_f[:], pattern=[[1, 128]], compare_op=ALU.is_ge,
                                           fill=0.0, base=0, channel_multiplier=-1), r=["tri_f"], w=["tri_f"])
    S.op("pool", lambda e: e.memset(ones_f[:], 1.0), w=["ones_f"])
    S.op("pool", lambda e: e.memset(ones_b[:], 1.0), w=["ones_b"])
    for i, (dst, src) in enumerate(((flag_t[:], flagc[:, :]), (cw_t[:], cwT[:, :, :]), (cb_t[:], cbT[:, :]), (dcol_t[:], dcol[:, :]),
                                    (gssd_t[:], gssd[:, :]), (sct[:], sconvT[:, :, :, :]),
                                    (dtb_t[:], dtb[0:1, :].partition_broadcast(128)), (a_t[:], alog[0:1, :].partition_broadcast(128)))):
        S.op("sp", lambda e, dst=dst, src=src: e.dma_start(out=dst, in_=src), w=[("cst", i)], dma="cst")
    S.dma_batch_end("cst")
    S.op("act", lambda e: e.activation(out=a_t[:], in_=a_t[:], func=AF.Exp), r=[("cst", 7)], w=[("cst", 7)])
    S.op("dve", lambda e: e.tensor_scalar(out=a_t[:], in0=a_t[:], scalar1=-1.0, scalar2=None, op0=ALU.mult), r=[("cst", 7)], w=[("cst", 7)])

    def load_x(src, ntiles):
        for t in range(ntiles):
            S.op("sp", lambda e, t=t: e.dma_start(out=xres[:, t, :], in_=src[t, :, :]), w=[("x", t)], dma="xld")
        S.dma_batch_end("xld")

    def norm_to_hT(gi, ntiles):
        S.op("sp", lambda e: e.dma_start(out=gb[:], in_=gvec[gi:gi + 1, :].partition_broadcast(128)),
             r=["stage0"], w=["gb", "stage0"], dma="gld")
        for t in range(ntiles):
            ncol = 128 if t < NPT else 35
            S.op("act", lambda e, t=t: e.activation(out=junk[:], in_=xres[:, t, :], func=AF.Square, accum_out=stat[:, 0:1]),
                 r=[("x", t)], w=["junk", "ss", "stage1"])
            S.op("dve", lambda e: e.tensor_scalar(out=stat[:, 1:2], in0=stat[:, 0:1], scalar1=1.0 / D, scalar2=EPS,
                                                  op0=ALU.mult, op1=ALU.add), r=["ss"], w=["ms"])
            S.op("act", lambda e: e.activation(out=stat[:, 2:3], in_=stat[:, 1:2], func=AF.Sqrt), r=["ms"], w=["sd"])
            S.op("dve", lambda e: e.reciprocal(out=stat[:, 3:4], in_=stat[:, 2:3]), r=["sd"], w=["rstd"])
            S.op("dve", lambda e, t=t: e.scalar_tensor_tensor(out=xs[:], in0=xres[:, t, :], scalar=stat[:, 3:4], in1=gb[:],
                                                              op0=ALU.mult, op1=ALU.mult),
                 r=[("x", t), "rstd", "gb", "stage0"], w=["xs", "stage1"])
            for half in range(2):
                pb = ps[2 * (t % 2) + half]
                pbv = pb.bitcast(BF16)
                pk = ("ps", 2 * (t % 2) + half)
                for j in range(8):
                    kc = half * 8 + j
                    S.op("pe", lambda e, pbv=pbv, j=j, kc=kc: e.transpose(out=pbv[:, j * 128:(j + 1) * 128],
                                                                          in_=xs[:, kc * 128:(kc + 1) * 128], identity=ident_b[:]),
                         r=["xs", "stage1", "ident_b"], w=[pk])
                tbk = ("hT", 0 if t < 4 else (1 if t < 8 else 2))
                c0 = t * 128
                if half == 0:
                    S.op("act", lambda e, pbv=pbv, half=half, c0=c0, ncol=ncol: e.activation(
                        out=hT[:, half * 8:(half + 1) * 8, c0:c0 + ncol],
                        in_=pbv.rearrange("p (j c) -> p j c", j=8)[:, :, 0:ncol], func=AF.Copy), r=[pk], w=[tbk])
                else:
                    S.op("dve", lambda e, pbv=pbv, half=half, c0=c0, ncol=ncol: e.tensor_copy(
                        out=hT[:, half * 8:(half + 1) * 8, c0:c0 + ncol],
                        in_=pbv.rearrange("p (j c) -> p j c", j=8)[:, :, 0:ncol]), r=[pk], w=[tbk])

    wctr = [0]
    suse = [0, 0, 0]

    def stage_slot():
        i = wctr[0]
        wctr[0] += 1
        k = i % 3
        suse[k] += 1
        return stage[k], f"stage{k}", f"stage{k}_{suse[k] // 100}", i

    def load_up_tile(src_ap, cast_eng):
        st, sk, sg, i = stage_slot()
        wb = wup[i % 4]
        wk = f"wup{i % 4}"
        S.op("sp", lambda e: e.dma_start(out=st[:], in_=src_ap), w=[sk], dma=sg)
        if cast_eng == "act":
            S.op("act", lambda e: e.activation(out=wb[:], in_=st[:], func=AF.Copy), r=[sk], w=[wk])
        else:
            S.op(cast_eng, lambda e: e.tensor_copy(out=wb[:], in_=st[:]), r=[sk], w=[wk])
        return wb, wk

    def up_mm(wb, wk, tb, pbank, pkey):
        c0, n = TB[tb]
        for kc in range(16):
            S.op("pe", lambda e, kc=kc: e.matmul(pbank[:, 0:n], lhsT=wb[:, kc, :], rhs=hT[:, kc, c0:c0 + n],
                                                 start=(kc == 0), stop=(kc == 15)),
                 r=[wk, ("hT", tb)], w=[pkey])

    def ffn(w, ntiles):
        w1, w3, w2 = w
        ntb = 3 if ntiles == NTT else 2
        pctr = 0
        for grp in range(NFF // G):
            gt = gT[grp % 2]
            w2t = w2g[grp % 2]
            for j in range(G):
                fc = grp * G + j
                wb1, wk1 = load_up_tile(w1[fc], "pool")
                wb3, wk3 = load_up_tile(w3[fc], "dve")
                st, sk, sg, i = stage_slot()
                S.op("sp", lambda e, st=st, fc=fc: e.dma_start(out=st[:].rearrange("p a b -> p (a b)"), in_=w2[fc]),
                     w=[sk], dma=sg)
                S.op("act", lambda e, st=st, w2t=w2t, j=j: e.activation(out=w2t[:, j, :], in_=st[:].rearrange("p a b -> p (a b)"),
                                                                        func=AF.Copy), r=[sk], w=[("w2g", grp % 2, j)])
                for tb in range(ntb):
                    c0, n = TB[tb]
                    pa, pbk = 2 * (pctr % 3), 2 * (pctr % 3) + 1
                    pctr += 1
                    up_mm(wb1, wk1, tb, ps[pa], ("ps", pa))
                    up_mm(wb3, wk3, tb, ps[pbk], ("ps", pbk))
                    sl = sil[pctr % 2]
                    slk = ("sil", pctr % 2)
                    S.op("act", lambda e, sl=sl, pa=pa, n=n: e.activation(out=sl[:, 0:n], in_=ps[pa][:, 0:n], func=AF.Silu),
                         r=[("ps", pa)], w=[slk])
                    S.op("dve", lambda e, sl=sl, pbk=pbk, n=n, c0=c0, gt=gt, j=j: e.tensor_tensor(
                        out=gt[:, j, c0:c0 + n], in0=sl[:, 0:n], in1=ps[pbk][:, 0:n], op=ALU.mult),
                         r=[slk, ("ps", pbk)], w=[("gT", grp % 2, j, tb)])
            for t in range(ntiles):
                m = 128 if t < NPT else 35
                tb = 0 if t < 4 else (1 if t < 8 else 2)
                for nb in range(4):
                    pd = 6 + (t * 4 + nb) % 2
                    for j in range(G):
                        S.op("pe", lambda e, t=t, j=j, nb=nb, pd=pd, m=m, gt=gt, w2t=w2t: e.matmul(
                            ps[pd][0:m, :], lhsT=gt[:, j, t * 128:t * 128 + m], rhs=w2t[:, j, nb * 512:(nb + 1) * 512],
                            start=(j == 0), stop=(j == G - 1)),
                             r=[("gT", grp % 2, j, tb), ("w2g", grp % 2, j)], w=[("ps", pd)])
                    S.op("dve", lambda e, t=t, nb=nb, pd=pd, m=m: e.scalar_tensor_tensor(
                        out=xres[0:m, t, nb * 512:(nb + 1) * 512], in0=ps[pd][0:m, :], scalar=0.5,
                        in1=xres[0:m, t, nb * 512:(nb + 1) * 512], op0=ALU.mult, op1=ALU.add),
                         r=[("ps", pd), ("x", t)], w=[("x", t)])

    QSCALE = 128.0 ** -0.5

    def inproj(main):
        ntiles = NTT if main else NPT
        ntb = 3 if main else 2
        chunks = list(range(NIN)) if main else [8, 9, 10, 11] + list(range(28, 40)) + [41, 42]
        if main and "q" in KSKIP:
            chunks = [c for c in chunks if not (c < 8 or 12 <= c < 28)]
        pctr = 0
        tctr = [0]
        if main:
            S.op("sp", lambda e: e.dma_start(out=halo[:], in_=pre_halo.rearrange("p (a b) -> p a b", b=3)),
                 w=["halo"], dma="pre")
            S.dma_batch_end("pre")
        else:
            S.op("pool", lambda e: e.memset(halo[:], 0.0), w=["halo"])
        S.op("pool", lambda e: e.memset(vb1[:, :, :, 128:129], 1.0), w=["vb1ones"])
        for c in chunks:
            wb, wk = load_up_tile(win[c], "pool" if c % 2 == 0 else "dve")
            is_q, is_k, is_v = c < 8, 8 <= c < 10, 10 <= c < 12
            is_qi, is_z, is_xbc = 12 <= c < 20, 20 <= c < 28, 28 <= c < 40
            is_sm, is_ki2, is_aux = c == 40, c == 41, c == 42
            if not is_v and not is_sm:
                for tb in range(ntb):
                    c0, n = TB[tb]
                    pa = pctr % 6
                    pctr += 1
                    up_mm(wb, wk, tb, ps[pa], ("ps", pa))
                    src = ps[pa][:, 0:n]
                    if is_q:
                        S.op("act", lambda e, src=src, c=c, c0=c0, n=n: e.activation(out=qT[:, c, c0:c0 + n], in_=src, func=AF.Copy, scale=QSCALE),
                             r=[("ps", pa)], w=[("qT", c, tb)])
                    elif is_k:
                        S.op("act", lambda e, src=src, c=c, c0=c0, n=n: e.activation(out=kT[:, c - 8, c0:c0 + n], in_=src, func=AF.Copy),
                             r=[("ps", pa)], w=[("kT", c - 8, tb)])
                    elif is_qi:
                        S.op("act", lambda e, src=src, c=c, c0=c0, n=n: e.activation(out=qiT[:, c - 12, c0:c0 + n], in_=src, func=AF.Copy),
                             r=[("ps", pa)], w=[("qiT", c - 12, tb)])
                    elif is_z:
                        S.op("act", lambda e, src=src, c=c, c0=c0, n=n: e.activation(out=szT[:, c - 20, c0:c0 + n], in_=src, func=AF.Silu),
                             r=[("ps", pa)], w=[("szT", c - 20, tb)])
                    elif is_ki2:
                        S.op("act", lambda e, src=src, c0=c0, n=n: e.activation(out=kiT2[:, c0:c0 + n], in_=src, func=AF.Copy),
                             r=[("ps", pa)], w=[("kiT2", tb)])
                    elif is_aux:
                        S.op("act", lambda e, pa=pa, c0=c0, n=n: e.activation(out=dtT[0:16, c0:c0 + n], in_=ps[pa][0:16, 0:n], func=AF.Copy),
                             r=[("ps", pa)], w=[("dtT", tb)])
                    elif is_xbc:
                        S.op("act", lambda e, src=src, c0=c0, n=n: e.activation(out=rawx[:, 3 + c0:3 + c0 + n], in_=src, func=AF.Copy),
                             r=[("ps", pa)], w=[("rawx", tb)])
            if is_xbc:
                xc = c - 28
                rk = [("rawx", tb) for tb in range(ntb)]
                S.op("dve", lambda e, xc=xc: e.tensor_copy(out=rawx[:, 0:3], in_=halo[:, xc, :]), r=["halo"], w=["rawxh"])
                if not main:
                    S.op("dve", lambda e, xc=xc: e.tensor_copy(out=halo[:, xc, :], in_=rawx[:, 3 + 1021:3 + 1024]),
                         r=rk + ["rawxh"], w=["halo"])
                S.op("dve", lambda e, xc=xc: e.tensor_scalar(out=cacc[:], in0=rawx[:, 0:1024], scalar1=cw_t[:, xc, 0:1], scalar2=None,
                                                             op0=ALU.mult), r=rk + ["rawxh", ("cst", 1)], w=["cacc"])
                for k in range(1, 4):
                    S.op("dve", lambda e, xc=xc, k=k: e.scalar_tensor_tensor(out=cacc[:], in0=rawx[:, k:k + 1024], scalar=cw_t[:, xc, k:k + 1],
                                                                            in1=cacc[:], op0=ALU.mult, op1=ALU.add),
                         r=rk + ["rawxh"], w=["cacc"])
                S.op("act", lambda e, xc=xc: e.activation(out=xcT[:, xc, 0:1024], in_=cacc[:], func=AF.Silu, bias=cb_t[:, xc:xc + 1]),
                     r=["cacc", ("cst", 2)], w=[("xcT", xc)])
                if main and "s" not in KSKIP:
                    S.op("dve", lambda e, xc=xc: e.tensor_copy(out=exts[:, :, 0:3], in_=sct[:, xc, :, :]), r=[("cst", 5)], w=["exts"])
                    S.op("dve", lambda e: e.tensor_copy(out=exts[:, :, 3:11], in_=rawx[:, 3 + 1024:3 + 1056].rearrange("p (b t) -> p b t", t=8)),
                         r=rk, w=["exts"])
                    S.op("dve", lambda e, xc=xc: e.tensor_scalar(out=cacc[:, 0:32].rearrange("p (b t) -> p b t", t=8), in0=exts[:, :, 0:8],
                                                                 scalar1=cw_t[:, xc, 0:1], scalar2=None, op0=ALU.mult),
                         r=["exts", ("xcT", xc)], w=["cacc"])
                    for k in range(1, 4):
                        S.op("dve", lambda e, xc=xc, k=k: e.scalar_tensor_tensor(
                            out=cacc[:, 0:32].rearrange("p (b t) -> p b t", t=8), in0=exts[:, :, k:k + 8], scalar=cw_t[:, xc, k:k + 1],
                            in1=cacc[:, 0:32].rearrange("p (b t) -> p b t", t=8), op0=ALU.mult, op1=ALU.add), r=["exts"], w=["cacc"])
                    S.op("act", lambda e, xc=xc: e.activation(out=xcT[:, xc, 1024:1056], in_=cacc[:, 0:32], func=AF.Silu, bias=cb_t[:, xc:xc + 1]),
                         r=["cacc"], w=[("xcT", xc)])
                if main and "t" not in KSKIP:
                    srcs = [1021] + [1024 + b * 8 + 5 for b in range(4)]
                    for si, s0 in enumerate(srcs):
                        pb, off = (7, si * 128) if si < 4 else (6, 0)
                        S.op("pe", lambda e, s0=s0, pb=pb, off=off: e.transpose(out=ps[pb][0:3, off:off + 128],
                                                                              in_=rawx[:, 3 + s0:3 + s0 + 3], identity=ident_f[:]),
                             r=rk + ["ident_f"], w=[("ps", pb)])
                    tl = tails[0]
                    tlk = ("tails", 0)
                    S.op("act", lambda e, tl=tl: e.activation(out=tl[0:3, 0:4, :], in_=ps[7][0:3, 0:512].rearrange("p (s c) -> p s c", s=4),
                                                              func=AF.Copy), r=[("ps", 7)], w=[tlk])
                    S.op("act", lambda e, tl=tl: e.activation(out=tl[0:3, 4, :], in_=ps[6][0:3, 0:128], func=AF.Copy), r=[("ps", 6)], w=[tlk])
                    S.op("sp", lambda e, tl=tl, xc=xc: e.dma_start(out=conv_out[:, :, xc * 128:(xc + 1) * 128].rearrange("s r c -> r s c"),
                                                                   in_=tl[0:3, :, :]), r=[tlk], w=[("conv_out", xc)], dma="tl0")
            if is_k or is_v or is_sm:
                if is_k and not main:
                    continue
                col = (c - 8) * 128 if (is_k or is_v) else 512
                for t in range(ntiles):
                    m = 128 if t < NPT else 35
                    tb = 0 if t < 4 else (1 if t < 8 else 2)
                    pd = 6 + t % 2
                    for kc in range(16):
                        S.op("pe", lambda e, t=t, kc=kc, pd=pd, m=m, wb=wb: e.matmul(
                            ps[pd][0:m, 0:128], lhsT=hT[:, kc, t * 128:t * 128 + m], rhs=wb[:, kc, :],
                            start=(kc == 0), stop=(kc == 15)), r=[wk, ("hT", tb)], w=[("ps", pd)])
                    if is_v:
                        S.op("dve", lambda e, t=t, pd=pd, c=c: e.tensor_copy(out=vb1[:, t, c - 10, 0:128], in_=ps[pd][:, 0:128]),
                             r=[("ps", pd)], w=[("vb1", t)])
                    if is_sm:
                        S.op("dve", lambda e, t=t, pd=pd: e.tensor_copy(out=sm_tok[:, t, :], in_=ps[pd][:, 0:128]),
                             r=[("ps", pd)], w=[("sm_tok", t)])
                    if main and "o" not in KSKIP:
                        ti = tctr[0] % 2
                        tctr[0] += 1
                        if "e" not in KSKIP:
                            S.op("dve", lambda e, ti=ti, pd=pd: e.tensor_copy(out=tokst[ti][:], in_=ps[pd][:, 0:128]),
                                 r=[("ps", pd)], w=[("tokst", ti)])
                        if "d" not in KSKIP:
                          S.op(os.environ.get("KTOKQ", "sp"), lambda e, t=t, ti=ti, col=col: e.dma_start(out=kvs_out[t, :, col:col + 128], in_=tokst[ti][:]),
                             r=[("tokst", ti)], w=[("kvs_out", t, col)], dma=f"tok{ti}")

    dtk, dta, acum, ea, dec, eal, scd, tmp16 = sm16

    def ssd_chunk(c0, L, full):
        cs0, cs1 = c0, c0 + L
        K = ["ssd"]

        def Q(eng, fn):
            S.op(eng, fn, w=K)

        Q("pe", lambda e: e.transpose(out=ps[6][0:L, 0:16], in_=dtT[0:16, cs0:cs1], identity=ident_f[0:16, 0:16]))
        Q("dve", lambda e: e.tensor_tensor(out=tmp16[0:L, :], in0=ps[6][0:L, 0:16], in1=dtb_t[0:L, :], op=ALU.add))
        Q("act", lambda e: e.activation(out=tmp16[0:L, :], in_=tmp16[0:L, :], func=AF.Exp))
        Q("act", lambda e: e.activation(out=dtk[0:L, :], in_=tmp16[0:L, :], func=AF.Ln, bias=1.0))
        Q("dve", lambda e: e.tensor_tensor(out=dta[0:L, :], in0=dtk[0:L, :], in1=a_t[0:L, :], op=ALU.mult))
        Q("pe", lambda e: e.matmul(ps[6][0:L, 16:32], lhsT=tri_f[0:L, 0:L], rhs=dta[0:L, :], start=True, stop=True))
        Q("pe", lambda e: e.matmul(ps[6][:, 32:48], lhsT=ones_f[0:L, :], rhs=dta[0:L, :], start=True, stop=True))
        Q("dve", lambda e: e.tensor_copy(out=acum[0:L, :], in_=ps[6][0:L, 16:32]))
        Q("act", lambda e: e.activation(out=ea[0:L, :], in_=acum[0:L, :], func=AF.Exp))
        Q("dve", lambda e: e.tensor_tensor(out=dec[0:L, :], in0=ps[6][0:L, 32:48], in1=acum[0:L, :], op=ALU.subtract))
        Q("act", lambda e: e.activation(out=dec[0:L, :], in_=dec[0:L, :], func=AF.Exp))
        Q("act", lambda e: e.activation(out=eal[:, :], in_=ps[6][:, 32:48], func=AF.Exp))
        Q("dve", lambda e: e.tensor_tensor(out=scd[0:L, :], in0=dtk[0:L, :], in1=dec[0:L, :], op=ALU.mult))
        pv = [ps[0].bitcast(BF16), ps[1].bitcast(BF16)]
        for j in range(10):
            dst = pv[0][0:L, j * 128:(j + 1) * 128] if j < 8 else pv[1][0:L, (j - 8) * 128:(j - 7) * 128]
            Q("pe", lambda e, j=j, dst=dst: e.transpose(out=dst, in_=xcT[:, j, cs0:cs1], identity=ident_b[:]))
        xv = pv[0][0:L, 0:1024].rearrange("p (h q) -> p h q", q=64)
        Q("dve", lambda e: e.tensor_tensor(out=xd[0:L, :].rearrange("p (h q) -> p h q", q=64), in0=xv,
                                           in1=dtk[0:L, :].rearrange("p (h o) -> p h o", o=1).to_broadcast([L, 16, 64]), op=ALU.mult))
        Q("dve", lambda e: e.tensor_tensor(out=xdd[0:L, :].rearrange("p (h q) -> p h q", q=64), in0=xv,
                                           in1=scd[0:L, :].rearrange("p (h o) -> p h o", o=1).to_broadcast([L, 16, 64]), op=ALU.mult))
        Q("act", lambda e: e.activation(out=Btok[0:L, :], in_=pv[1][0:L, 0:256], func=AF.Copy))
        if full:
            for g in range(2):
                Q("pe", lambda e, g=g: e.matmul(ps[6][0:L, 128 + g * 128:128 + g * 128 + L], lhsT=xcT[:, 8 + g, cs0:cs1],
                                               rhs=xcT[:, 10 + g, cs0:cs1], start=True, stop=True))
                Q("dve", lambda e, g=g: e.tensor_tensor(out=CBm[0:L, g, 0:L], in0=ps[6][0:L, 128 + g * 128:128 + g * 128 + L],
                                                        in1=tri_f[0:L, 0:L], op=ALU.mult))
            for half in range(2):
                Q("dve", lambda e, half=half: e.tensor_tensor(
                    out=Dm[0:L, :, 0:L], in0=ident_f[0:L, 0:L].rearrange("p (o i) -> p o i", o=1).to_broadcast([L, 8, L]),
                    in1=acum[0:L, half * 8:(half + 1) * 8].rearrange("p (h o) -> p h o", o=1).to_broadcast([L, 8, L]), op=ALU.mult))
                for q4 in range(2):
                    Q("pe", lambda e, q4=q4: e.matmul(ps[2 + q4][0:L, 0:4 * L].rearrange("p (h i) -> p h i", h=4), lhsT=ones_f[0:L, 0:L],
                                                     rhs=Dm[0:L, 4 * q4:4 * q4 + 4, 0:L], start=True, stop=True))
                for hh in range(8):
                    h = half * 8 + hh
                    bsrc = ps[2 + hh // 4][0:L, (hh % 4) * L:(hh % 4 + 1) * L]
                    Q("dve", lambda e, bsrc=bsrc, h=h: e.tensor_scalar(out=t1[0:L, 0:L], in0=bsrc, scalar1=acum[0:L, h:h + 1], scalar2=0.0,
                                                                       op0=ALU.subtract, op1=ALU.min))
                    Q("act", lambda e: e.activation(out=t2[0:L, 0:L], in_=t1[0:L, 0:L], func=AF.Exp))
                    Q("dve", lambda e, h=h: e.tensor_tensor(out=Mh[0:L, 0:L], in0=t2[0:L, 0:L], in1=CBm[0:L, h // 8, 0:L], op=ALU.mult))
                    Q("pe", lambda e, h=h: e.matmul(ps[h // 8][0:L, (h % 8) * 64:(h % 8 + 1) * 64], lhsT=Mh[0:L, 0:L],
                                                   rhs=xd[0:L, h * 64:(h + 1) * 64], start=True, stop=True))
            for g in range(2):
                Q("pe", lambda e, g=g: e.matmul(ps[4 + g][0:L, :], lhsT=xcT[:, 10 + g, cs0:cs1], rhs=Hb[:, g * 512:(g + 1) * 512],
                                               start=True, stop=True))
                Q("act", lambda e, g=g: e.activation(out=ysb[0:L, g * 512:(g + 1) * 512], in_=ps[g][0:L, :], func=AF.Copy))
            for h in range(16):
                Q("dve", lambda e, h=h: e.scalar_tensor_tensor(out=ysb[0:L, h * 64:(h + 1) * 64], in0=ps[4 + h // 8][0:L, (h % 8) * 64:(h % 8 + 1) * 64],
                                                               scalar=ea[0:L, h:h + 1], in1=ysb[0:L, h * 64:(h + 1) * 64], op0=ALU.mult, op1=ALU.add))
        for g in range(2):
            Q("pe", lambda e, g=g: e.matmul(ps[2 + g][:, :], lhsT=Btok[0:L, g * 128:(g + 1) * 128], rhs=xdd[0:L, g * 512:(g + 1) * 512],
                                           start=True, stop=True))
        Q("dve", lambda e: e.tensor_tensor(out=Hst[:].rearrange("p (h q) -> p h q", q=64), in0=Hst[:].rearrange("p (h q) -> p h q", q=64),
                                           in1=eal[:, :].rearrange("p (h o) -> p h o", o=1).to_broadcast([128, 16, 64]), op=ALU.mult))
        for g in range(2):
            Q("dve", lambda e, g=g: e.tensor_tensor(out=Hst[:, g * 512:(g + 1) * 512], in0=Hst[:, g * 512:(g + 1) * 512], in1=ps[2 + g][:, :],
                                                    op=ALU.add))
        Q("act", lambda e: e.activation(out=Hb[:], in_=Hst[:], func=AF.Copy))
        if full:
            for j in range(8):
                Q("pe", lambda e, j=j: e.transpose(out=ps[4 + j // 4][:, (j % 4) * L:(j % 4 + 1) * L], in_=ysb[0:L, j * 128:(j + 1) * 128],
                                                  identity=ident_f[0:L, 0:L]))
            for j in range(8):
                ysrc = ps[4 + j // 4][:, (j % 4) * L:(j % 4 + 1) * L]
                Q("dve", lambda e, j=j, ysrc=ysrc: e.scalar_tensor_tensor(out=gbuf[:, j, 0:L], in0=xcT[:, j, cs0:cs1], scalar=dcol_t[:, j:j + 1],
                                                                          in1=ysrc, op0=ALU.mult, op1=ALU.add))
                Q("dve", lambda e, j=j: e.tensor_tensor(out=gbuf[:, j, 0:L], in0=gbuf[:, j, 0:L], in1=szT[:, j, cs0:cs1], op=ALU.mult))
                Q("act", lambda e, j=j: e.activation(out=sq[:, j, 0:L], in_=gbuf[:, j, 0:L], func=AF.Square))
            for g in range(2):
                for jj in range(4):
                    Q("pe", lambda e, g=g, jj=jj: e.matmul(ps[7][:, g * 128:g * 128 + L], lhsT=ones_b[:, :], rhs=sq[:, 4 * g + jj, 0:L],
                                                          start=(jj == 0), stop=(jj == 3)))
                Q("dve", lambda e, g=g: e.tensor_scalar(out=rs[:, g, 0:L], in0=ps[7][:, g * 128:g * 128 + L], scalar1=1.0 / 512, scalar2=EPS,
                                                        op0=ALU.mult, op1=ALU.add))
                Q("act", lambda e, g=g: e.activation(out=rs[:, g, 0:L], in_=rs[:, g, 0:L], func=AF.Sqrt))
                Q("dve", lambda e, g=g: e.reciprocal(out=rs[:, g, 0:L], in_=rs[:, g, 0:L]))
            for j in range(8):
                Q("dve", lambda e, j=j: e.scalar_tensor_tensor(out=ssdT[:, j, cs0:cs1], in0=gbuf[:, j, 0:L], scalar=gssd_t[:, j:j + 1],
                                                               in1=rs[:, j // 4, 0:L], op0=ALU.mult, op1=ALU.mult))


    sc = A.at(OH + 10272, [128, 2048], F32, "sc")
    mk = A.at(OH + 10272 + 8192, [128, 2048], BF16, "mk")
    mkT = A.at(OH + 10272 + 12288, [128, 16, 128], BF16, "mkT")
    o = O_STAGE
    bT = A.at(o, [128, 2, 8, 128], F32, "bT"); o += 8192
    rtmp = A.at(o, [128, 2048], F32, "rtmp"); o += 8192
    pexp = A.at(o, [128, 512], F32, "pexp"); o += 2048
    pm = A.at(o, [128, 512], BF16, "pm"); o += 1024
    atok = A.at(o, [128, 1024], BF16, "atok"); o += 2048
    hw = A.at(o, [128, 32], F32, "hw"); o += 128
    p2 = A.at(o, [128, 32], F32, "p2"); o += 128
    wis = A.at(o, [128, 16], F32, "wis"); o += 64
    cfar = A.at(o, [128, 8], F32, "cfar"); o += 32
    rden = A.at(o, [128, 8], F32, "rden"); o += 32
    a1 = A.at(o, [128, 8], F32, "a1"); o += 32
    rb_t = A.at(o, [32, 8], F32, "rb_t"); o += 32
    oh_t = A.at(o, [32, 256], F32, "oh_t"); o += 1024
    rbb = A.at(o, [32, 8, 128], F32, "rbb"); o += 4096
    cfT = A.at(o, [128, 8, 128], F32, "cfT"); o += 4096
    rtmp2 = A.at(o, [128, 2048], F32, "rtmp2"); o += 8192
    assert o <= O_W2G
    NIT = 24

    atn = [0]

    def AQ(eng, fn, dma=None, r=(), w=()):
        if dma is not None:
            atn[0] += 1
            dma = f"at{atn[0] // 100}"
        S.op(eng, fn, r=list(r), w=["att"] + list(w), dma=dma)

    gcn = [0]

    def gather(dst, table, idx_t, j, key):
        gcn[0] += 1
        S.op("pool", lambda e: e.indirect_dma_start(out=dst, out_offset=None, in_=table[:, :],
                                                    in_offset=bass.IndirectOffsetOnAxis(ap=idx_t[:, j:j + 1], axis=0)),
             r=["att_idx"], w=["gch", key], dma=f"ag{gcn[0] // 100}")

    def att_setup():
        AQ("sp", lambda e: e.dma_start(out=rb_t[:], in_=relb[:, :]), dma="at")
        AQ("sp", lambda e: e.dma_start(out=oh_t[:], in_=oh_d[:, :]), dma="at")
        AQ("sp", lambda e: e.dma_start(out=cfar[:], in_=relb[31:32, :].partition_broadcast(128)), dma="at")
        AQ("dve", lambda e: e.tensor_copy(out=rbb[:], in_=rb_t[:].rearrange("p (h o) -> p h o", o=1).to_broadcast([32, 8, 128])))
        for h in range(8):
            AQ("pe", lambda e, h=h: e.matmul(ps[0][:, 0:256], lhsT=rbb[:, h, :], rhs=oh_t[:, :], start=True, stop=True))
            AQ("dve", lambda e: e.tensor_copy(out=rtmp[:, 0:256], in_=ps[0][:, 0:256]))
            AQ("sp", lambda e, h=h: e.dma_start(out=bsc[h, :, :], in_=rtmp[:, 0:256]), dma="at")
        for h in range(8):
            for ty in range(2):
                src = bass.AP(tensor=bsc_t, offset=h * 128 * 256 + 128 * ty, ap=[[255, 128], [1, 128]])
                AQ("sp", lambda e, h=h, ty=ty, src=src: e.dma_start(out=bT[:, ty, h, :], in_=src), dma="at")
        for k in range(NIT):
            AQ("pool", lambda e, k=k: e.memset(p2[:, k:k + 1], 2.0 ** -(k + 1)))
        AQ("dve", lambda e: e.tensor_copy(out=cfT[:], in_=cfar[:].rearrange("p (h o) -> p h o", o=1).to_broadcast([128, 8, 128])))

    KATT = int(os.environ.get("KATT", "9"))
    KQT = int(os.environ.get("KQT", "8"))

    def att_prompt_tile(qt):
        if KATT < 2 or qt >= KQT:
            return
        q0 = qt * 128
        nb = 9 + qt
        Sx = nb * 128
        segs = [(kiT2_pre, 0, 512), (kiT2_pre, 512, 512)]
        own = 128 * (qt + 1)
        c = 0
        while c < own:
            n = min(512, own - c)
            segs.append((kiT2, c, n))
            c += n
        AQ("dve", lambda e: e.tensor_scalar(out=wis[:], in0=sm_tok[:, qt, 64:80], scalar1=1024.0 ** -0.5, scalar2=None, op0=ALU.mult))
        for hi in range(16):
            pb = 64 * (hi % 2)
            par = hi % 2
            rt = rtmp if par == 0 else rtmp2
            for si, (src, c0, n) in enumerate(segs):
                S.op("pe", lambda e, si=si, src=src, c0=c0, n=n, pb=pb, hi=hi, par=par: e.matmul(
                    ps[4 * par + si][:, 0:n], lhsT=qiT[pb:pb + 64, hi // 2, q0:q0 + 128], rhs=src[pb:pb + 64, c0:c0 + n], start=True, stop=True),
                     r=["att"], w=[("ixp", par)])
            S.op("act", lambda e, par=par, rt=rt: e.activation(out=rt[:, 0:Sx], in_=psall[:, 2048 * par:2048 * par + Sx], func=AF.Relu),
                 r=[("ixp", par)], w=[("ixr", par)])
            if hi == 0:
                S.op("dve", lambda e, rt=rt: e.tensor_scalar(out=sc[:, 0:Sx], in0=rt[:, 0:Sx], scalar1=wis[:, 0:1], scalar2=None, op0=ALU.mult),
                     r=[("ixr", par), "att"], w=["sc"])
            else:
                S.op("dve", lambda e, hi=hi, rt=rt: e.scalar_tensor_tensor(out=sc[:, 0:Sx], in0=rt[:, 0:Sx], scalar=wis[:, hi:hi + 1], in1=sc[:, 0:Sx],
                                                                            op0=ALU.mult, op1=ALU.add), r=[("ixr", par)], w=["sc"])
        AQ("pool", lambda e: e.memset(stat[:, 5:6], 0.0), r=["sc", ("ixp", 0), ("ixp", 1), ("ixr", 0), ("ixr", 1)])
        if KATT < 3:
            return
        absm, lo, mid, cnt, tt, Wd = [a1[:, i:i + 1] for i in range(6)]
        AQ("dve", lambda e: e.tensor_reduce(out=absm, in_=sc[:, 0:Sx], axis=AX.X, op=ALU.max, apply_absolute_value=True))
        AQ("dve", lambda e: e.tensor_scalar(out=sc[:, 0:1024], in0=sc[:, 0:1024], scalar1=flag_t[:, 1:2], scalar2=None, op0=ALU.add))
        AQ("pool", lambda e: e.affine_select(out=sc[:, Sx - 128:Sx], in_=sc[:, Sx - 128:Sx], pattern=[[-1, 128]], compare_op=ALU.is_ge,
                                             fill=-1e30, base=0, channel_multiplier=1))
        AQ("dve", lambda e: e.tensor_scalar(out=lo, in0=absm, scalar1=-1.0, scalar2=-1.0, op0=ALU.mult, op1=ALU.add))
        AQ("dve", lambda e: e.tensor_scalar(out=Wd, in0=absm, scalar1=2.0, scalar2=2.0, op0=ALU.mult, op1=ALU.add))
        AQ("dve", lambda e: e.tensor_scalar(out=hw[:, 0:NIT], in0=p2[:, 0:NIT], scalar1=Wd, scalar2=None, op0=ALU.mult))
        for k in range(NIT):
            AQ("dve", lambda e, k=k: e.tensor_tensor(out=mid, in0=lo, in1=hw[:, k:k + 1], op=ALU.add))
            AQ("dve", lambda e: e.tensor_scalar(out=mk[:, 0:Sx], in0=sc[:, 0:Sx], scalar1=mid, scalar2=0.0, op0=ALU.is_ge, op1=ALU.add, accum_out=cnt))
            AQ("dve", lambda e, k=k: e.tensor_scalar(out=tt, in0=cnt, scalar1=256.0, scalar2=hw[:, k:k + 1], op0=ALU.is_ge, op1=ALU.mult))
            AQ("dve", lambda e: e.tensor_tensor(out=lo, in0=lo, in1=tt, op=ALU.add))
        AQ("dve", lambda e: e.tensor_scalar(out=mk[:, 0:Sx], in0=sc[:, 0:Sx], scalar1=lo, scalar2=None, op0=ALU.is_ge))
        if KATT < 4:
            return
        pv0, pv1 = ps[0].bitcast(BF16), ps[1].bitcast(BF16)
        for kb in range(nb):
            dst = pv0[:, kb * 128:(kb + 1) * 128] if kb < 8 else pv1[:, (kb - 8) * 128:(kb - 7) * 128]
            AQ("pe", lambda e, kb=kb, dst=dst: e.transpose(out=dst, in_=mk[:, kb * 128:(kb + 1) * 128], identity=ident_b[:]))
        AQ("dve", lambda e: e.tensor_copy(out=mkT[:, 0:8, :], in_=pv0.rearrange("p (k c) -> p k c", c=128)))
        AQ("dve", lambda e: e.tensor_copy(out=mkT[:, 8:nb, :], in_=pv1[:, 0:(nb - 8) * 128].rearrange("p (k c) -> p k c", c=128)))

        if KATT < 5:
            return

        def pso(h):
            return ps[5 + h // 3][:, (h % 3) * 160:(h % 3) * 160 + 129]

        for kb in range(nb):
            pre = kb < 8
            j = kb if pre else kb - 8
            ksrc = kT_pre if pre else kT
            vsrc = vb1_pre if pre else vb1
            diff = (8 + qt) - kb
            for kv in range(2):
                AQ("pe", lambda e, kv=kv, ksrc=ksrc, j=j: e.matmul(ps[2 + kv].rearrange("p (h c) -> p h c", c=128), lhsT=ksrc[:, kv, j * 128:(j + 1) * 128],
                                                                  rhs=qT[:, 4 * kv:4 * kv + 4, q0:q0 + 128], start=True, stop=True))
                btile = cfT[:, 4 * kv:4 * kv + 4, :] if diff >= 2 else bT[:, diff, 4 * kv:4 * kv + 4, :]
                AQ("dve", lambda e, kv=kv, btile=btile: e.tensor_tensor(out=pexp[:].rearrange("p (h c) -> p h c", c=128),
                                                                        in0=ps[2 + kv].rearrange("p (h c) -> p h c", c=128), in1=btile, op=ALU.add))
                AQ("act", lambda e: e.activation(out=pexp[:], in_=pexp[:], func=AF.Exp))
                AQ("dve", lambda e, kb=kb: e.tensor_tensor(out=pm[:].rearrange("p (h c) -> p h c", c=128), in0=pexp[:].rearrange("p (h c) -> p h c", c=128),
                                                           in1=mkT[:, kb:kb + 1, :].to_broadcast([128, 4, 128]), op=ALU.mult))
                for hh in range(4):
                    h = 4 * kv + hh
                    AQ("pe", lambda e, h=h, hh=hh, vsrc=vsrc, j=j, kv=kv, kb=kb: e.matmul(pso(h), lhsT=pm[:, hh * 128:(hh + 1) * 128], rhs=vsrc[:, j, kv, :],
                                                                                       start=(kb == 0), stop=(kb == nb - 1)))
        if KATT < 6:
            return
        for b3 in range(3):
            nh = 3 if b3 < 2 else 2
            AQ("dve", lambda e, b3=b3, nh=nh: e.reciprocal(out=rden[:, 3 * b3:3 * b3 + nh],
                                                          in_=ps[5 + b3][:, 0:480].rearrange("p (h c) -> p h c", c=160)[:, 0:nh, 128]))
        for h in range(8):
            AQ("dve", lambda e, h=h: e.tensor_scalar(out=atok[:, h * 128:(h + 1) * 128], in0=pso(h)[:, 0:128], scalar1=rden[:, h:h + 1], scalar2=None,
                                                     op0=ALU.mult))
        for h in range(8):
            AQ("pe", lambda e, h=h: e.transpose(out=pv0[:, h * 128:(h + 1) * 128], in_=atok[:, h * 128:(h + 1) * 128], identity=ident_b[:]))
        AQ("dve", lambda e: e.tensor_copy(out=attT[:, :, q0:q0 + 128], in_=pv0.rearrange("p (k c) -> p k c", c=128)))
        S.op("pool", lambda e: e.memset(stat[:, 6:7], 0.0), r=["att"], w=["mixT"])


    def att_sample():
        o = O_W2G
        def T(shape, dt, nm):
            nonlocal o
            nb_ = int(np.prod(shape[1:])) * (2 if dt == BF16 else 4)
            t_ = A.at(o, shape, dt, nm)
            o = al(o + nb_)
            return t_
        pti = T([128, 128], I32, "s_pti"); ptf = T([128, 128], F32, "s_ptf"); idx_i = T([128, 128], I32, "s_idx")
        iota_c = T([128, 8], F32, "s_iota")
        kig2 = [T([128, 4, 64], F32, "s_kig0"), A.at(O_STAGE + 18432, [128, 4, 64], F32, "s_kig1")]; kiTs = T([64, 512], BF16, "s_kiTs")
        qiS = T([64, 16, 8], BF16, "s_qiS")
        wis32 = T([32, 16], F32, "s_wis32"); wd32 = T([32, 16, 8], F32, "s_wd32"); wdb = T([32, 128], F32, "s_wdb")
        wrow = T([128, 128], F32, "s_wrow")
        bm_t = T([32, 4], F32, "s_bm"); dq_t = T([32, 8], F32, "s_dq"); pen_t = T([32, 4, 8], F32, "s_pen")
        scS = T([128, 132, 8], F32, "s_scS"); ind = T([128, 132, 8], BF16, "s_ind"); mS = T([128, 132, 8], BF16, "s_mS")
        KVg2 = [A.at(O_STAGE, [128, 4, 512], F32, "s_KVg0"), A.at(O_STAGE + 10240, [128, 4, 512], F32, "s_KVg1")]
        kTs = T([128, 4, 2, 128], BF16, "s_kTs"); Vb = T([128, 4, 2, 129], BF16, "s_Vb")
        pS = T([128, 256], F32, "s_pS"); pmS = T([128, 256], BF16, "s_pmS")
        cfS = T([128, 8, 8], F32, "s_cfS"); bSl = T([128, 8, 8], F32, "s_bSl"); bSn = T([32, 8, 8], F32, "s_bSn")
        rw = [T([128, 8], F32, f"s_rw{i}") for i in range(8)]
        dg = T([8, 8], F32, "s_dg"); m2 = T([8, 8], F32, "s_m2")
        atS = T([32, 2, 128], BF16, "s_atS"); rdS = T([32, 8], F32, "s_rdS")
        assert o <= O_W2G + 12 * NB, (o - O_W2G, 12 * NB)
        absr, lor, midr, cntr, ttr, Wr, pc, hwr = rw
        NPG = 128

        AQ("pool", lambda e: e.iota(iota_c[:, 0:1], pattern=[[0, 1]], base=0, channel_multiplier=1, allow_small_or_imprecise_dtypes=True))
        AQ("sp", lambda e: e.dma_start(out=bm_t[:], in_=bm4[:, :]), dma="at")
        AQ("sp", lambda e: e.dma_start(out=dq_t[:], in_=dq8[:, :]), dma="at")
        AQ("sp", lambda e: e.dma_start(out=pen_t[:], in_=pen32[:, :, :]), dma="at")
        AQ("dve", lambda e: e.tensor_scalar(out=wis32[:], in0=sm_tok[0:32, 8, 64:80], scalar1=1024.0 ** -0.5, scalar2=None, op0=ALU.mult))
        AQ("dve", lambda e: e.tensor_tensor(out=wd32[:], in0=wis32[:].rearrange("p (h o) -> p h o", o=1).to_broadcast([32, 16, 8]),
                                            in1=dq_t[:].rearrange("p (o q) -> p o q", o=1).to_broadcast([32, 16, 8]), op=ALU.mult))
        AQ("dve", lambda e: e.tensor_copy(out=cfS[:], in_=cfar[:].rearrange("p (h o) -> p h o", o=1).to_broadcast([128, 8, 8])))
        for h in range(8):
            src = bass.AP(tensor=bsc_t, offset=h * 128 * 256 + 128, ap=[[255, 128], [1, 8]])
            AQ("sp", lambda e, h=h, src=src: e.dma_start(out=bSl[:, h, :], in_=src), dma="at")
            for tb in range(4):
                src2 = bass.AP(tensor=bsc_t, offset=h * 128 * 256, ap=[[255, 8], [1, 8]])
                AQ("sp", lambda e, h=h, tb=tb, src2=src2: e.dma_start(out=bSn[8 * tb:8 * tb + 8, h, :], in_=src2), dma="at")
        AQ("pool", lambda e: e.memset(Vb[:, :, :, 128:129], 1.0))

        for b in range(4):
            cb = 1024 + 8 * b
            AQ("sp", lambda e, b=b: e.dma_start(out=pti[:], in_=pt4[b:b + 1, :].partition_broadcast(128)), dma="at")
            AQ("dve", lambda e: e.tensor_copy(out=ptf[:], in_=pti[:]))
            AQ("dve", lambda e: e.tensor_scalar(out=ptf[:], in0=ptf[:], scalar1=128.0, scalar2=iota_c[:, 0:1], op0=ALU.mult, op1=ALU.add))
            AQ("dve", lambda e: e.tensor_copy(out=idx_i[:], in_=ptf[:]), w=["att_idx"])
            qv = qiS[:].rearrange("d (hc two) q -> d hc two q", two=2)
            AQ("sp", lambda e, cb=cb, qv=qv: e.dma_start(out=qv[:, :, 0, :], in_=qiT[0:64, :, cb:cb + 8]), dma="at")
            AQ("sp", lambda e, cb=cb, qv=qv: e.dma_start(out=qv[:, :, 1, :], in_=qiT[64:128, :, cb:cb + 8]), dma="at")
            qflat = qiS[:].rearrange("d h q -> d (h q)")
            AQ("dve", lambda e, b=b: e.tensor_scalar(out=wdb[:], in0=wd32[:].rearrange("p h q -> p (h q)"), scalar1=bm_t[:, b:b + 1], scalar2=None, op0=ALU.mult))
            AQ("pe", lambda e: e.matmul(ps[0][:, 0:128], lhsT=ones_f[0:32, :], rhs=wdb[:], start=True, stop=True))
            AQ("dve", lambda e: e.tensor_copy(out=wrow[:], in_=ps[0][:, 0:128]))
            AQ("pool", lambda e: e.memset(scS[:, 128, :], -1e30))
            AQ("pe", lambda e, qflat=qflat: e.matmul(ps[0][0:32, 0:128], lhsT=kiT2[0:64, 1024:1056], rhs=qflat, start=True, stop=True))
            AQ("act", lambda e: e.activation(out=rtmp[0:32, 0:128], in_=ps[0][0:32, 0:128], func=AF.Relu))
            AQ("dve", lambda e: e.tensor_tensor(out=rtmp[0:32, 0:128], in0=rtmp[0:32, 0:128], in1=wrow[0:32, :], op=ALU.mult))
            AQ("dve", lambda e: e.tensor_reduce(out=scS[0:32, 128, :], in_=rtmp[0:32, 0:128].rearrange("p (h q) -> p q h", q=8), axis=AX.X, op=ALU.add))
            AQ("dve", lambda e, b=b: e.tensor_tensor(out=scS[0:32, 128, :], in0=scS[0:32, 128, :], in1=pen_t[:, b, :], op=ALU.add))
            for st in range(NPG // 4):
                j0 = 4 * st
                kig = kig2[st % 2]
                for pg in range(4):
                    gather(kig[:, pg, :], kidx_tab, idx_i, j0 + pg, ("kig", st % 2, pg))
                for pg in range(4):
                    AQ("pe", lambda e, pg=pg, kig=kig: e.transpose(out=ps[0][0:64, pg * 128:(pg + 1) * 128], in_=kig[:, pg, :], identity=ident_f[:]),
                       r=[("kig", st % 2, pg)])
                AQ("dve", lambda e: e.tensor_copy(out=kiTs[:], in_=ps[0][0:64, :]))
                for pg in range(4):
                    AQ("pe", lambda e, pg=pg, qflat=qflat: e.matmul(ps[1][:, pg * 128:(pg + 1) * 128], lhsT=kiTs[:, pg * 128:(pg + 1) * 128], rhs=qflat,
                                                                   start=True, stop=True))
                AQ("act", lambda e: e.activation(out=rtmp[:, 0:512], in_=ps[1][:, :], func=AF.Relu))
                AQ("dve", lambda e: e.tensor_tensor(out=rtmp[:, 0:512].rearrange("p (g c) -> p g c", g=4), in0=rtmp[:, 0:512].rearrange("p (g c) -> p g c", g=4),
                                                    in1=wrow[:].rearrange("p (o c) -> p o c", o=1).to_broadcast([128, 4, 128]), op=ALU.mult))
                AQ("dve", lambda e, j0=j0: e.tensor_reduce(out=scS[:, j0:j0 + 4, :], in_=rtmp[:, 0:512].rearrange("p (g h q) -> p g q h", g=4, q=8),
                                                           axis=AX.X, op=ALU.add))
            AQ("dve", lambda e: e.tensor_reduce(out=pc[:], in_=scS[:, 0:128, :].rearrange("p k q -> p q k"), axis=AX.X, op=ALU.max, apply_absolute_value=True))
            AQ("pe", lambda e: e.transpose(out=ps[0][0:8, 0:128], in_=pc[:], identity=ident_f[:]))
            AQ("dve", lambda e: e.tensor_reduce(out=m2[:, 0:1], in_=ps[0][0:8, 0:128], axis=AX.X, op=ALU.max))
            AQ("dve", lambda e: e.tensor_scalar(out=dg[:], in0=ident_f[0:8, 0:8], scalar1=m2[:, 0:1], scalar2=None, op0=ALU.mult))
            AQ("pe", lambda e: e.matmul(ps[0][:, 0:8], lhsT=ones_f[0:8, :], rhs=dg[:], start=True, stop=True))
            AQ("dve", lambda e: e.tensor_copy(out=absr[:], in_=ps[0][:, 0:8]))
            AQ("dve", lambda e: e.tensor_scalar(out=lor[:], in0=absr[:], scalar1=-1.0, scalar2=-1.0, op0=ALU.mult, op1=ALU.add))
            AQ("dve", lambda e: e.tensor_scalar(out=Wr[:], in0=absr[:], scalar1=2.0, scalar2=2.0, op0=ALU.mult, op1=ALU.add))
            for k in range(NIT):
                AQ("dve", lambda e, k=k: e.tensor_scalar(out=hwr[:], in0=Wr[:], scalar1=2.0 ** -(k + 1), scalar2=None, op0=ALU.mult))
                AQ("dve", lambda e: e.tensor_tensor(out=midr[:], in0=lor[:], in1=hwr[:], op=ALU.add))
                AQ("dve", lambda e: e.tensor_tensor(out=ind[:, 0:129, :], in0=scS[:, 0:129, :],
                                                    in1=midr[:].rearrange("p (o q) -> p o q", o=1).to_broadcast([128, 129, 8]), op=ALU.is_ge))
                AQ("dve", lambda e: e.tensor_reduce(out=pc[:], in_=ind[:, 0:129, :].rearrange("p k q -> p q k"), axis=AX.X, op=ALU.add))
                AQ("pe", lambda e: e.matmul(ps[0][:, 0:8], lhsT=ones_f[:, :], rhs=pc[:], start=True, stop=True))
                AQ("dve", lambda e: e.tensor_scalar(out=ttr[:], in0=ps[0][:, 0:8], scalar1=256.0, scalar2=None, op0=ALU.is_ge))
                AQ("dve", lambda e: e.tensor_tensor(out=ttr[:], in0=ttr[:], in1=hwr[:], op=ALU.mult))
                AQ("dve", lambda e: e.tensor_tensor(out=lor[:], in0=lor[:], in1=ttr[:], op=ALU.add))
            AQ("dve", lambda e: e.tensor_tensor(out=mS[:, 0:129, :], in0=scS[:, 0:129, :],
                                                in1=lor[:].rearrange("p (o q) -> p o q", o=1).to_broadcast([128, 129, 8]), op=ALU.is_ge))
            def pso(kv):
                return ps[4][0:32, kv * 160:kv * 160 + 129]
            for st in range(NPG // 4):
                j0 = 4 * st
                KVg = KVg2[st % 2]
                for pg in range(4):
                    gather(KVg[:, pg, :], kv_tab, idx_i, j0 + pg, ("kvg", st % 2, pg))
                for pg in range(4):
                    for kv in range(2):
                        r_ = pg * 2 + kv
                        AQ("pe", lambda e, pg=pg, kv=kv, r_=r_, KVg=KVg: e.transpose(out=ps[r_ // 4][:, (r_ % 4) * 128:(r_ % 4 + 1) * 128],
                                                                                   in_=KVg[:, pg, kv * 128:(kv + 1) * 128], identity=ident_f[:]),
                           r=[("kvg", st % 2, pg)])
                AQ("dve", lambda e: e.tensor_copy(out=kTs[:, 0:2, :, :].rearrange("p g k s -> p (g k s)"), in_=ps[0][:, :]))
                AQ("act", lambda e: e.activation(out=kTs[:, 2:4, :, :].rearrange("p g k s -> p (g k s)"), in_=ps[1][:, :], func=AF.Copy))
                S.op("dve", lambda e, KVg=KVg: e.tensor_copy(out=Vb[:, :, :, 0:128], in_=KVg[:, :, 256:512].rearrange("p g (k d) -> p g k d", k=2)),
                     r=[("kvg", st % 2, pg) for pg in range(4)], w=["Vb"])
                for pg in range(4):
                    for kv in range(2):
                        r_ = pg * 2 + kv
                        AQ("pe", lambda e, pg=pg, kv=kv, r_=r_, cb=cb: e.matmul(ps[2][:, r_ * 32:(r_ + 1) * 32].rearrange("p (h q) -> p h q", q=8),
                                                                              lhsT=kTs[:, pg, kv, :], rhs=qT[:, 4 * kv:4 * kv + 4, cb:cb + 8], start=True, stop=True))
                AQ("dve", lambda e: e.tensor_tensor(out=pS[:].rearrange("p (g c) -> p g c", g=4), in0=ps[2][:, 0:256].rearrange("p (g c) -> p g c", g=4),
                                                    in1=cfS[:].rearrange("p h q -> p (h q)").rearrange("p (o c) -> p o c", o=1).to_broadcast([128, 4, 64]), op=ALU.add))
                if st == NPG // 4 - 1:
                    AQ("dve", lambda e: e.tensor_tensor(out=pS[:, 192:256], in0=ps[2][:, 192:256], in1=bSl[:].rearrange("p h q -> p (h q)"), op=ALU.add))
                AQ("act", lambda e: e.activation(out=pS[:], in_=pS[:], func=AF.Exp))
                AQ("dve", lambda e, j0=j0: e.tensor_tensor(out=pmS[:].rearrange("p (g h q) -> p g h q", g=4, q=8), in0=pS[:].rearrange("p (g h q) -> p g h q", g=4, q=8),
                                                           in1=mS[:, j0:j0 + 4, :].rearrange("p g (o q) -> p g o q", o=1).to_broadcast([128, 4, 8, 8]), op=ALU.mult))
                for pg in range(4):
                    for kv in range(2):
                        r_ = pg * 2 + kv
                        AQ("pe", lambda e, pg=pg, kv=kv, r_=r_, st=st: e.matmul(pso(kv), lhsT=pmS[:, r_ * 32:(r_ + 1) * 32], rhs=Vb[:, pg, kv, :],
                                                                              start=(st == 0 and pg == 0), stop=False), r=["Vb"])
            for kv in range(2):
                AQ("pe", lambda e, kv=kv, cb=cb: e.matmul(ps[2][0:32, kv * 32:(kv + 1) * 32].rearrange("p (h q) -> p h q", q=8), lhsT=kT[:, kv, 1024:1056],
                                                         rhs=qT[:, 4 * kv:4 * kv + 4, cb:cb + 8], start=True, stop=True))
            AQ("dve", lambda e: e.tensor_tensor(out=pS[0:32, 0:64], in0=ps[2][0:32, 0:64], in1=bSn[:].rearrange("p h q -> p (h q)"), op=ALU.add))
            AQ("act", lambda e: e.activation(out=pS[0:32, 0:64], in_=pS[0:32, 0:64], func=AF.Exp))
            AQ("dve", lambda e: e.tensor_tensor(out=pmS[0:32, 0:64].rearrange("p (h q) -> p h q", q=8), in0=pS[0:32, 0:64].rearrange("p (h q) -> p h q", q=8),
                                                in1=mS[0:32, 128:129, :].to_broadcast([32, 8, 8]), op=ALU.mult))
            for kv in range(2):
                AQ("pe", lambda e, kv=kv: e.matmul(pso(kv), lhsT=pmS[0:32, kv * 32:(kv + 1) * 32], rhs=vb1[0:32, 8, kv, :], start=False, stop=True))
            for kv in range(2):
                AQ("dve", lambda e, kv=kv: e.reciprocal(out=rdS[:, kv:kv + 1], in_=pso(kv)[:, 128:129]))
                AQ("dve", lambda e, kv=kv: e.tensor_scalar(out=atS[:, kv, :], in0=pso(kv)[:, 0:128], scalar1=rdS[:, kv:kv + 1], scalar2=None, op0=ALU.mult))
            pvb = ps[0].bitcast(BF16)
            for kv in range(2):
                AQ("pe", lambda e, kv=kv: e.transpose(out=pvb[:, kv * 32:(kv + 1) * 32], in_=atS[:, kv, :], identity=ident_b[0:32, 0:32]))
            AQ("dve", lambda e, cb=cb: e.tensor_copy(out=attT[:, :, cb:cb + 8], in_=pvb[:, 0:64].rearrange("p (h q) -> p h q", q=8)))
        S.op("pool", lambda e: e.memset(stat[:, 6:7], 0.0), r=["att"], w=["mixT"])

    xst = [A.at(OH + 10272 + i * 2048, [128, 512], F32, f"xst{i}") for i in range(4)]
    xctr = [0]

    def outproj():
        for nb in range(4):
            wt = w2g[nb % 2]
            wv = wt[:].rearrange("p g d -> p (g d)").rearrange("p (k c) -> p k c", c=512)
            for kg in range(4):
                st, sk, sg, i = stage_slot()
                S.op("sp", lambda e, st=st, nb=nb, kg=kg: e.dma_start(out=st[:].rearrange("p a b -> p (a b)"),
                                                                      in_=wout[nb, kg].rearrange("p k c -> p (k c)")), w=[sk], dma=sg)
                S.op("act", lambda e, st=st, wv=wv, kg=kg: e.activation(out=wv[:, kg * 4:(kg + 1) * 4, :],
                                                                        in_=st[:].rearrange("p a b -> p (a b)").rearrange("p (k c) -> p k c", c=512),
                                                                        func=AF.Copy), r=[sk], w=[("wo", nb % 2, kg)])
            for t in range(NTT):
                m = 128 if t < NPT else 35
                pd = 6 + t % 2
                xi = xctr[0] % 2
                xctr[0] += 1
                S.op("sp", lambda e, t=t, nb=nb, xi=xi: e.dma_start(out=xst[xi][:], in_=xsp[t, :, nb * 512:(nb + 1) * 512]),
                     r=[("xsp", t, nb)], w=[("xst", xi)], dma=f"xsi{xi}")
                for kc in range(16):
                    src = attT if kc < 8 else ssdT
                    S.op("pe", lambda e, t=t, kc=kc, pd=pd, m=m, src=src, wv=wv: e.matmul(
                        ps[pd][0:m, :], lhsT=src[:, kc % 8, t * 128:t * 128 + m], rhs=wv[:, kc, :], start=(kc == 0), stop=(kc == 15)),
                         r=[("wo", nb % 2, kc // 4), "mixT"], w=[("ps", pd)])
                S.op("dve", lambda e, xi=xi, pd=pd, m=m: e.tensor_tensor(out=xst[xi][0:m, :], in0=ps[pd][0:m, :], in1=xst[xi][0:m, :], op=ALU.add),
                     r=[("ps", pd)], w=[("xst", xi)])
                S.op("sp", lambda e, t=t, nb=nb, xi=xi: e.dma_start(out=xsp[t, :, nb * 512:(nb + 1) * 512], in_=xst[xi][:]),
                     r=[("xst", xi)], w=[("xsp", t, nb)], dma=f"xso{xi}")

    def final_norm():
        S.op("sp", lambda e: e.dma_start(out=gb[:], in_=gvec[3:4, :].partition_broadcast(128)),
             r=["stage0"], w=["gb", "stage0"], dma="gld")
        for t in range(NTT):
            S.op("act", lambda e, t=t: e.activation(out=junk[:], in_=xres[:, t, :], func=AF.Square, accum_out=stat[:, 0:1]),
                 r=[("x", t)], w=["junk", "ss", "stage1"])
            S.op("dve", lambda e: e.tensor_scalar(out=stat[:, 1:2], in0=stat[:, 0:1], scalar1=1.0 / D, scalar2=EPS,
                                                  op0=ALU.mult, op1=ALU.add), r=["ss"], w=["ms"])
            S.op("act", lambda e: e.activation(out=stat[:, 2:3], in_=stat[:, 1:2], func=AF.Sqrt), r=["ms"], w=["sd"])
            S.op("dve", lambda e: e.reciprocal(out=stat[:, 3:4], in_=stat[:, 2:3]), r=["sd"], w=["rstd"])
            S.op("dve", lambda e, t=t: e.scalar_tensor_tensor(out=xres[:, t, :], in0=xres[:, t, :], scalar=stat[:, 3:4], in1=gb[:],
                                                              op0=ALU.mult, op1=ALU.mult),
                 r=[("x", t), "rstd", "gb", "stage0"], w=[("x", t)])
            S.op("sp", lambda e, t=t: e.dma_start(out=y_out[t, :, :], in_=xres[:, t, :]), r=[("x", t)], w=[("y_out", t)], dma="out")

    try:
        load_x(xpre, NPT)
        norm_to_hT(0, NPT)
        ffn(wf1, NPT)
        norm_to_hT(1, NPT)
        bar()
        inproj(False)
        bar()
        S.op("pool", lambda e: e.memset(Hst[:], 0.0), w=["ssd"])
        for t in range(NPT):
            ssd_chunk(t * 128, 128, False)
        S.op("dve", lambda e: e.tensor_scalar(out=Hst[:], in0=Hst[:], scalar1=flag_t[:, 0:1], scalar2=None, op0=ALU.mult), r=[("cst", 0)], w=["ssd"])
        S.op("sp", lambda e: e.dma_start(out=pre_k[:, :, :], in_=kT[:, :, 0:1024]), w=["pre0"], dma="pre")
        S.op("sp", lambda e: e.dma_start(out=pre_v[:, :, :], in_=vb1[:, 0:8, :, :].rearrange("p t k d -> p t (k d)")),
             r=["vb1ones"], w=["pre1"], dma="pre")
        S.op("sp", lambda e: e.dma_start(out=pre_ki[:, :], in_=kiT2[:, 0:1024]), w=["pre2"], dma="pre")
        S.op("sp", lambda e: e.dma_start(out=pre_h[:, :], in_=Hst[:]), r=["ssd"], w=["pre3"], dma="pre")
        S.op("sp", lambda e: e.dma_start(out=pre_halo[:, :], in_=halo[:].rearrange("p a b -> p (a b)")), r=["halo"], w=["pre4"], dma="pre")
        S.dma_batch_end("pre")
        bar()
        load_x(xin, NTT)
        norm_to_hT(0, NTT)
        ffn(wf1, NTT)
        norm_to_hT(1, NTT)
        for t in range(NTT):
            S.op("sp", lambda e, t=t: e.dma_start(out=xsp[t, :, :], in_=xres[:, t, :]), r=[("x", t)], w=[("xsp", t, nb) for nb in range(4)], dma="xsp")
        bar()
        inproj(True)
        bar()
        S.op("sp", lambda e: e.dma_start(out=kT_pre[:], in_=pre_k[:, :, :]), w=["kT_pre"], dma="pre")
        S.op("sp", lambda e: e.dma_start(out=vb1_pre[:].rearrange("p t k d -> p t (k d)"), in_=pre_v[:, :, :]),
             w=["vb1_pre"], dma="pre")
        S.op("sp", lambda e: e.dma_start(out=kiT2_pre[:], in_=pre_ki[:, :]), w=["kiT2_pre"], dma="pre")
        S.op("sp", lambda e: e.dma_start(out=Hst[:], in_=pre_h[:, :]), w=["ssd"], dma="pre")
        S.dma_batch_end("pre")
        S.op("act", lambda e: e.activation(out=Hb[:], in_=Hst[:], func=AF.Copy), w=["ssd"])
        for t in range(NPT):
            ssd_chunk(t * 128, 128, True)
        S.op("sp", lambda e: e.dma_start(out=ssm_out[0, :, :], in_=Hst[:]), r=["ssd"], w=[("ssm_out", 0)], dma="hs")
        for b in range(4):
            S.op("sp", lambda e, b=b: e.dma_start(out=Hst[:], in_=sssmT[b]), r=[("ssm_out", b)], w=["ssd"], dma="hs")
            S.op("act", lambda e: e.activation(out=Hb[:], in_=Hst[:], func=AF.Copy), w=["ssd"])
            ssd_chunk(1024 + 8 * b, 8, True)
            S.op("sp", lambda e, b=b: e.dma_start(out=ssm_out[1 + b, :, :], in_=Hst[:]), r=["ssd"], w=[("ssm_out", 1 + b)], dma="hs")
        bar()
        S.op("pool", lambda e: e.memset(attT[:], 0.0), w=["mixT", "att"])
        att_setup()
        for qt in range(NPT):
            att_prompt_tile(qt)
        if "S" not in KSKIP:
            att_sample()
        S.op("pool", lambda e: e.memset(ssdT[:, :, 1056:NTOK], 0.0), r=["ssd"], w=["mixT", "ssd"])
        bar()
        outproj()
        bar()
        load_x(xsp, NTT)
        if "h" not in KSKIP:
            norm_to_hT(2, NTT)
        if "f" not in KSKIP:
            ffn(wf2, NTT)
        if "n" not in KSKIP:
            final_norm()
    except _Stop:
        pass
    fin = ["out"] + [f"tok{i}" for i in range(2)] + ["tl0"] + ["hs"]
    S.emit(final_dma_groups=fin)
    return nc


def _up_layout(W):
    K, N = W.shape
    assert K == D and N % 128 == 0
    return np.ascontiguousarray(W.reshape(16, 128, N // 128, 128).transpose(2, 1, 0, 3))


def _t5_onehot():
    n = np.arange(256)
    nf = np.maximum(n, 1).astype(np.float32)
    large = 16 + (np.log(nf / np.float32(16)) / np.float32(np.log(128 / 16)) * np.float32(16)).astype(np.int32)
    bucket = np.where(n < 16, n, np.minimum(large, 31))
    oh = np.zeros((32, 256), np.float32)
    oh[bucket, n] = 1.0
    return oh


_IN_OFF = dict(q=(0, 1024), k=(1024, 256), v=(1280, 256), qi=(1536, 1024), ki=(2560, 64), wi=(2624, 16),
               z=(2640, 1024), xbc=(3664, 1536), dt=(5200, 16))


def kernel(x_prompt, x_sample, cache_k, cache_v, cache_kidx, state_conv, state_ssm, page_table, rel_bias,
           g_ffn1, w1_ffn1, w3_ffn1, w2_ffn1, g_mix, w_in, conv_w, conv_b, a_log, dt_bias, d_skip, g_ssd,
           w_out, g_ffn2, w1_ffn2, w3_ffn2, w2_ffn2, g_final):
    f = lambda a: np.asarray(a, dtype=np.float32)
    xp, xs_ = f(x_prompt), f(x_sample)
    win_full = f(w_in)[0]
    cols = []
    for nm in ("q", "k", "v", "qi", "z", "xbc", "ki", "wi", "dt"):
        o, n = _IN_OFF[nm]
        cols.append(win_full[:, o:o + n])
    cols.append(np.zeros((D, 32), np.float32))
    ko, kn = _IN_OFF["ki"]
    cols += [win_full[:, ko:ko + kn], win_full[:, ko:ko + kn]]
    do, dn = _IN_OFF["dt"]
    cols += [win_full[:, do:do + dn], np.zeros((D, 112), np.float32)]
    win_r = _up_layout(np.concatenate(cols, axis=1))
    shared = dict(
        gvec=np.stack([f(g_ffn1)[0], f(g_mix)[0], f(g_ffn2)[0], f(g_final)]),
        w1a=_up_layout(f(w1_ffn1)[0]), w3a=_up_layout(f(w3_ffn1)[0]), w2a=np.ascontiguousarray(f(w2_ffn1)[0].reshape(NFF, 128, D)),
        w1b=_up_layout(f(w1_ffn2)[0]), w3b=_up_layout(f(w3_ffn2)[0]), w2b=np.ascontiguousarray(f(w2_ffn2)[0].reshape(NFF, 128, D)),
        win=win_r,
        wout=np.ascontiguousarray(f(w_out)[0].reshape(4, 4, 128, 4, 512).transpose(3, 0, 2, 1, 4)),
        cwT=np.ascontiguousarray(f(conv_w)[0].reshape(4, 12, 128).transpose(2, 1, 0)),
        cbT=np.ascontiguousarray(f(conv_b)[0].reshape(12, 128).T),
        dcol=np.ascontiguousarray(np.repeat(f(d_skip)[0], 64).reshape(8, 128).T),
        gssd=np.ascontiguousarray(f(g_ssd)[0].reshape(8, 128).T),
        alog=f(a_log), dtb=f(dt_bias), relb=f(rel_bias), oh_d=_t5_onehot(),
    )
    ck = np.ascontiguousarray(f(cache_k)[0].reshape(5120 * 128, 256))
    cv = np.ascontiguousarray(f(cache_v)[0].reshape(5120 * 128, 256))
    cki = np.ascontiguousarray(f(cache_kidx)[0].reshape(5120 * 128, 64))
    ptab = np.asarray(page_table, dtype=np.int32)
    tt_ = np.arange(32)
    bm4 = (tt_[:, None] // 8 == np.arange(4)[None]).astype(np.float32)
    dq8 = (tt_[:, None] % 8 == np.arange(8)[None]).astype(np.float32)
    pen32 = np.where((tt_[:, None, None] // 8 == np.arange(4)[None, :, None]) & (tt_[:, None, None] % 8 <= np.arange(8)[None, None, :]), 0.0, -1e30).astype(np.float32)
    shared.update(kidx_tab=cki, kv_tab=np.concatenate([ck, cv], axis=1), bm4=bm4, dq8=dq8, pen32=pen32)
    sconv_all = f(state_conv)[0]
    sssm_all = f(state_ssm)[0]
    in_maps = []
    for c in range(8):
        b, half = c // 2, c % 2
        xin = np.zeros((NTT, 128, D), np.float32)
        xin[:NPT] = xp[b, half * 1024:(half + 1) * 1024].reshape(NPT, 128, D)
        xin[8, 0:32] = xs_[4 * c:4 * c + 4].reshape(32, D)
        if half == 1:
            xin[8, 32:35] = xp[b, 1021:1024]
        m = dict(shared)
        m["xin"] = xin
        m["pt4"] = np.ascontiguousarray(ptab[4 * c:4 * c + 4])
        m["xpre"] = np.ascontiguousarray(xp[b, 0:1024].reshape(NPT, 128, D)) if half == 1 else np.zeros((NPT, 128, D), np.float32)
        fl = np.zeros((128, 2), np.float32)
        fl[:, 0] = float(half)
        fl[:, 1] = (float(half) - 1.0) * 1e30
        m["flagc"] = fl
        m["sconvT"] = np.ascontiguousarray(sconv_all[4 * c:4 * c + 4].reshape(4, 3, 12, 128).transpose(3, 2, 0, 1))
        m["sssmT"] = np.ascontiguousarray(sssm_all[4 * c:4 * c + 4].reshape(4, 1024, 128).transpose(0, 2, 1))
        in_maps.append(m)
    nc = build()
    res = run_bass_kernel_spmd(nc, in_maps, core_ids=list(range(8))).results
    _DBG["res"] = res

    y_prompt = np.zeros((4, 2048, D), np.float32)
    y_sample = np.zeros((32, 8, D), np.float32)
    k_prompt = np.zeros((1, 4, 2048, 2, 128), np.float32)
    v_prompt = np.zeros_like(k_prompt)
    kidx_prompt = np.zeros((1, 4, 2048, 64), np.float32)
    conv_prompt = np.zeros((1, 4, 3, 1536), np.float32)
    ssm_prompt = np.zeros((1, 4, 16, 64, 128), np.float32)
    k_sample = np.zeros((1, 32, 8, 2, 128), np.float32)
    v_sample = np.zeros_like(k_sample)
    kidx_sample = np.zeros((1, 32, 8, 64), np.float32)
    conv_sample = np.zeros((1, 32, 3, 1536), np.float32)
    ssm_sample = np.zeros((1, 32, 16, 64, 128), np.float32)
    for c in range(8):
        b, half = c // 2, c % 2
        r = res[c]
        sl = slice(half * 1024, (half + 1) * 1024)
        yo = r["y_out"]
        y_prompt[b, sl] = yo[:NPT].reshape(1024, D)
        y_sample[4 * c:4 * c + 4] = yo[8, 0:32].reshape(4, 8, D)
        kv = r["kvs_out"]
        kvp = kv[:NPT].reshape(1024, 640)
        k_prompt[0, b, sl] = kvp[:, 0:256].reshape(1024, 2, 128)
        v_prompt[0, b, sl] = kvp[:, 256:512].reshape(1024, 2, 128)
        kidx_prompt[0, b, sl] = kvp[:, 512:576]
        kvs_ = kv[8, 0:32]
        k_sample[0, 4 * c:4 * c + 4] = kvs_[:, 0:256].reshape(4, 8, 2, 128)
        v_sample[0, 4 * c:4 * c + 4] = kvs_[:, 256:512].reshape(4, 8, 2, 128)
        kidx_sample[0, 4 * c:4 * c + 4] = kvs_[:, 512:576].reshape(4, 8, 64)
        so = r["ssm_out"].reshape(5, 128, 16, 64).transpose(0, 2, 3, 1)
        if half == 1:
            ssm_prompt[0, b] = so[0]
        ssm_sample[0, 4 * c:4 * c + 4] = so[1:5]
        co = r["conv_out"]
        if half == 1:
            conv_prompt[0, b] = co[0]
        conv_sample[0, 4 * c:4 * c + 4] = co[1:5]
    return (y_prompt, y_sample, k_prompt, v_prompt, kidx_prompt, conv_prompt, ssm_prompt,
            k_sample, v_sample, kidx_sample, conv_sample, ssm_sample)
```
